# Optimizing a Trainium2 kernel written in Bass

```python
import math
import jax, jax.numpy as jnp
from jax import lax
import numpy as np

D_MODEL = 2048
BATCH = 4
SEQ = 2048
DEPTH = 2
DEC_BATCH = 128
DEC_SEQ = 4
PAST_LEN = 16384
PAGE_SIZE = 128

N_MIXERS = 4
GROUP_W = D_MODEL // N_MIXERS
D_MIX = N_MIXERS * GROUP_W
N_IN_SLICES = 11
D_IN = N_IN_SLICES * GROUP_W
RET_HEADS = 4
RET_DK = GROUP_W // RET_HEADS
RET_DV = GROUP_W // RET_HEADS
RET_CHUNK = 128
ROPE_THETA = 10000.0
CONV_W = 31
SC_W = 3
SGU_GROUPS = 4
SGU_CH = GROUP_W // SGU_GROUPS
SGU_CHUNK = 128
D_FF = 5632
N_EXPERTS = 8
TOP_K = 2
MOE_BLOCK = 128
N_DENSE = (DEPTH + 1) // 2
N_MOE = DEPTH // 2
EPS = 1e-6

kernel_name = 'hybrid_retention_conv_sgu_moe_step'

F32 = jnp.float32


def rmsnorm(x, g):
    xf = x.astype(F32)
    y = xf * lax.rsqrt(jnp.mean(xf * xf, axis=-1, keepdims=True) + EPS)
    return (y * g.astype(F32)).astype(x.dtype)


def layernorm(x, g, b):
    xf = x.astype(F32)
    mu = jnp.mean(xf, axis=-1, keepdims=True)
    xc = xf - mu
    y = xc * lax.rsqrt(jnp.mean(xc * xc, axis=-1, keepdims=True) + EPS)
    return (y * g.astype(F32) + b.astype(F32)).astype(x.dtype)


def rotary(x, pos):
    half = x.shape[-1] // 2
    inv = ROPE_THETA ** (-jnp.arange(half, dtype=F32) / half)
    ang = pos[:, None] * inv[None, :]
    cos = jnp.cos(ang)[None, :, None, :]
    sin = jnp.sin(ang)[None, :, None, :]
    x1, x2 = x[..., :half], x[..., half:]
    return jnp.concatenate([x1 * cos - x2 * sin, x1 * sin + x2 * cos], axis=-1)


def retention(q, k, v, s0):
    Bt, L, H, _ = q.shape
    C = math.gcd(L, RET_CHUNK)
    n = L // C
    log_g = jnp.log1p(-jnp.exp2(-5.0 - jnp.arange(H, dtype=F32)))
    idx = jnp.arange(C, dtype=F32)
    rel = idx[:, None] - idx[None, :]
    decay_in = jnp.where(rel >= 0, jnp.exp(log_g[:, None, None] * jnp.maximum(rel, 0.0)), 0.0)
    decay_q = jnp.exp(log_g[:, None] * (idx + 1.0)[None, :]).T[None, :, :, None]
    decay_k = jnp.exp(log_g[:, None] * (C - 1.0 - idx)[None, :]).T[None, :, :, None]
    decay_c = jnp.exp(log_g * C)[None, :, None, None]

    def chunk_step(S, qkv):
        qc, kc, vc = qkv
        scores = jnp.einsum('bihd,bjhd->bhij', qc, kc) * decay_in[None]
        inner = jnp.einsum('bhij,bjhe->bihe', scores, vc)
        cross = jnp.einsum('bihd,bhde->bihe', qc, S) * decay_q
        S_new = S * decay_c + jnp.einsum('bjhd,bjhe->bhde', kc * decay_k, vc)
        return S_new, inner + cross

    def split(t):
        return jnp.moveaxis(t.reshape(Bt, n, C, H, t.shape[-1]), 1, 0)

    S, out = lax.scan(chunk_step, s0, (split(q), split(k), split(v)))
    out = jnp.moveaxis(out, 0, 1).reshape(Bt, L, H, v.shape[-1])
    return out, S


def causal_dwconv(ext, w):
    C = w.shape[-1]
    return lax.conv_general_dilated(
        ext, w.astype(ext.dtype)[:, None, :], window_strides=(1,), padding='VALID',
        dimension_numbers=('NWC', 'WIO', 'NWC'), feature_group_count=C)


def mixer_layer(h, s_ret, buf31, buf3, pos0, w_in, ret_norm_g, conv31_w, conv31_b,
                conv_ln_g, conv_ln_b, sconv_w, sgu_ln_g, sgu_ln_b, sgu_w, sgu_b, w_out):
    Bt, L, _ = h.shape
    z = h @ w_in
    (q, k, v, g, b_lin, b_gate, c_b, c_c, c_h, d_u, d_v) = jnp.split(z, N_IN_SLICES, axis=-1)

    pos = jnp.arange(L, dtype=F32) + pos0
    qh = rotary(q.reshape(Bt, L, RET_HEADS, RET_DK).astype(F32), pos)
    kh = rotary(k.reshape(Bt, L, RET_HEADS, RET_DK).astype(F32), pos) * (RET_DK ** -0.5)
    vh = v.reshape(Bt, L, RET_HEADS, RET_DV).astype(F32)
    r, s_new = retention(qh, kh, vh, s_ret.astype(F32))
    r = r * lax.rsqrt(jnp.mean(r * r, axis=-1, keepdims=True) + EPS)
    r = r.reshape(Bt, L, GROUP_W) * ret_norm_g.astype(F32)
    out_a = jax.nn.silu(g) * r.astype(h.dtype)

    glu = b_lin * jax.nn.sigmoid(b_gate)
    ext31 = jnp.concatenate([buf31.astype(glu.dtype), glu], axis=1)
    conv = causal_dwconv(ext31, conv31_w) + conv31_b
    out_b = jax.nn.silu(layernorm(conv, conv_ln_g, conv_ln_b))
    new_buf31 = ext31[:, -(CONV_W - 1):]

    gated = c_c * c_h
    ext3 = jnp.concatenate([buf3.astype(gated.dtype), gated], axis=1)
    out_c = c_b * causal_dwconv(ext3, sconv_w)
    new_buf3 = ext3[:, -(SC_W - 1):]

    vn = layernorm(d_v, sgu_ln_g, sgu_ln_b)
    Cs = min(L, SGU_CHUNK)
    nc = L // Cs
    wm = jnp.tril(sgu_w[:, :Cs, :Cs])
    vg = vn.reshape(Bt, nc, Cs, SGU_GROUPS, SGU_CH)
    mixed = jnp.einsum('gts,bnsgc->bntgc', wm, vg) + sgu_b[:, :Cs].T[None, None, :, :, None]
    out_d = d_u * mixed.reshape(Bt, L, GROUP_W)

    y = jnp.concatenate([out_a, out_b, out_c, out_d], axis=-1) @ w_out
    return y, s_new.astype(s_ret.dtype), new_buf31, new_buf3, vn


def swiglu(x, w1, w3, w2):
    return (jax.nn.silu(x @ w1) * (x @ w3)) @ w2


def moe_swiglu(x, router_w, w1, w3, w2):
    Bt, L, D = x.shape
    xt = x.reshape(-1, D)
    T = xt.shape[0]
    logits = (xt @ router_w).astype(F32)
    top_l, top_e = lax.top_k(logits, TOP_K)
    gates = jax.nn.softmax(top_l, axis=-1).astype(x.dtype)
    S = T * TOP_K
    flat_e = top_e.reshape(-1)
    flat_t = jnp.repeat(jnp.arange(T, dtype=jnp.int32), TOP_K, total_repeat_length=S)
    flat_g = gates.reshape(-1)
    order = jnp.argsort(flat_e)
    se = flat_e[order]
    counts = jnp.bincount(flat_e, length=N_EXPERTS)
    padded = (counts + MOE_BLOCK - 1) // MOE_BLOCK * MOE_BLOCK
    pad_end = jnp.cumsum(padded)
    pad_start = pad_end - padded
    start = jnp.cumsum(counts) - counts
    dest = pad_start[se] + jnp.arange(S, dtype=jnp.int32) - start[se]
    n_blocks = -(-S // MOE_BLOCK) + N_EXPERTS
    P = n_blocks * MOE_BLOCK
    slot_t = jnp.zeros((P,), jnp.int32).at[dest].set(flat_t[order])
    slot_g = jnp.zeros((P,), x.dtype).at[dest].set(flat_g[order])
    block_start = jnp.arange(n_blocks, dtype=jnp.int32) * MOE_BLOCK
    block_e = jnp.minimum(jnp.sum(block_start[:, None] >= pad_end[None, :], axis=-1), N_EXPERTS - 1)
    xb = xt[slot_t].reshape(n_blocks, MOE_BLOCK, D)

    def expert_block(args):
        xe, e = args
        return swiglu(xe, w1[e], w3[e], w2[e])

    yb = lax.map(expert_block, (xb, block_e)).reshape(P, D) * slot_g[:, None]
    out = jnp.zeros((T, D), yb.dtype).at[slot_t].add(yb)
    return out.reshape(Bt, L, D)


def setup_inputs(seed: int = 0) -> dict:
    key = jax.random.key(seed)
    ks = iter(jax.random.split(key, 40))
    nrm = lambda shape, scale: jax.random.normal(next(ks), shape, F32) * scale
    gain = lambda shape: 1.0 + nrm(shape, 0.01)
    return {
        'x_prompt': nrm((BATCH, SEQ, D_MODEL), 1.0),
        'x_sample': nrm((DEC_BATCH, DEC_SEQ, D_MODEL), 1.0),
        'state_ret': nrm((DEPTH, DEC_BATCH, RET_HEADS, RET_DK, RET_DV), 0.5),
        'state_conv31': nrm((DEPTH, DEC_BATCH, CONV_W - 1, GROUP_W), 0.5),
        'state_conv3': nrm((DEPTH, DEC_BATCH, SC_W - 1, GROUP_W), 0.5),
        'mix_norm_g': gain((DEPTH, D_MODEL)),
        'w_in': nrm((DEPTH, D_MODEL, D_IN), D_MODEL ** -0.5),
        'ret_norm_g': gain((DEPTH, GROUP_W)),
        'conv31_w': nrm((DEPTH, CONV_W, GROUP_W), CONV_W ** -0.5),
        'conv31_b': nrm((DEPTH, GROUP_W), 0.02),
        'conv_ln_g': gain((DEPTH, GROUP_W)),
        'conv_ln_b': nrm((DEPTH, GROUP_W), 0.02),
        'sconv_w': nrm((DEPTH, SC_W, GROUP_W), SC_W ** -0.5),
        'sgu_ln_g': gain((DEPTH, GROUP_W)),
        'sgu_ln_b': nrm((DEPTH, GROUP_W), 0.02),
        'sgu_w': nrm((DEPTH, SGU_GROUPS, SGU_CHUNK, SGU_CHUNK), SGU_CHUNK ** -0.5),
        'sgu_b': gain((DEPTH, SGU_GROUPS, SGU_CHUNK)),
        'w_out': nrm((DEPTH, D_MIX, D_MODEL), D_MIX ** -0.5),
        'ffn_norm_g': gain((DEPTH, D_MODEL)),
        'dense_w1': nrm((N_DENSE, D_MODEL, D_FF), D_MODEL ** -0.5),
        'dense_w3': nrm((N_DENSE, D_MODEL, D_FF), D_MODEL ** -0.5),
        'dense_w2': nrm((N_DENSE, D_FF, D_MODEL), D_FF ** -0.5),
        'router_w': nrm((N_MOE, D_MODEL, N_EXPERTS), D_MODEL ** -0.5),
        'moe_w1': nrm((N_MOE, N_EXPERTS, D_MODEL, D_FF), D_MODEL ** -0.5),
        'moe_w3': nrm((N_MOE, N_EXPERTS, D_MODEL, D_FF), D_MODEL ** -0.5),
        'moe_w2': nrm((N_MOE, N_EXPERTS, D_FF, D_MODEL), D_FF ** -0.5),
        'final_norm_g': gain((D_MODEL,)),
    }


def reference(x_prompt, x_sample, state_ret, state_conv31, state_conv3, mix_norm_g, w_in,
              ret_norm_g, conv31_w, conv31_b, conv_ln_g, conv_ln_b, sconv_w, sgu_ln_g, sgu_ln_b,
              sgu_w, sgu_b, w_out, ffn_norm_g, dense_w1, dense_w3, dense_w2, router_w,
              moe_w1, moe_w3, moe_w2, final_norm_g):
    def run(x, s_ret, b31, b3, pos0):
        rets, c31s, c3s, vrows = [], [], [], []
        for l in range(DEPTH):
            h = rmsnorm(x, mix_norm_g[l])
            y, s_new, nb31, nb3, vn = mixer_layer(
                h, s_ret[l], b31[l], b3[l], pos0, w_in[l], ret_norm_g[l], conv31_w[l], conv31_b[l],
                conv_ln_g[l], conv_ln_b[l], sconv_w[l], sgu_ln_g[l], sgu_ln_b[l], sgu_w[l], sgu_b[l],
                w_out[l])
            x = x + y
            h = rmsnorm(x, ffn_norm_g[l])
            j = l // 2
            if l % 2 == 0:
                x = x + swiglu(h, dense_w1[j], dense_w3[j], dense_w2[j])
            else:
                x = x + moe_swiglu(h, router_w[j], moe_w1[j], moe_w3[j], moe_w2[j])
            rets.append(s_new)
            c31s.append(nb31)
            c3s.append(nb3)
            vrows.append(vn)
        return (rmsnorm(x, final_norm_g), jnp.stack(rets), jnp.stack(c31s), jnp.stack(c3s),
                jnp.stack(vrows))

    dt = x_prompt.dtype
    zero_ret = jnp.zeros((DEPTH, BATCH, RET_HEADS, RET_DK, RET_DV), dt)
    zero_31 = jnp.zeros((DEPTH, BATCH, CONV_W - 1, GROUP_W), dt)
    zero_3 = jnp.zeros((DEPTH, BATCH, SC_W - 1, GROUP_W), dt)
    y_prompt, ret_p, c31_p, c3_p, _ = run(x_prompt, zero_ret, zero_31, zero_3, 0)
    y_sample, ret_s, c31_s, c3_s, v_s = run(x_sample, state_ret, state_conv31, state_conv3, PAST_LEN)
    return (y_prompt, y_sample, ret_p, ret_s, c31_p, c31_s, c3_p, c3_s, v_s)
```

```python
import math
from contextlib import ExitStack

import numpy as np
import concourse.bass as bass
import concourse.mybir as mybir
from concourse.bass_utils import run_bass_kernel_spmd

F32 = mybir.dt.float32
BF16 = mybir.dt.bfloat16
ALU = mybir.AluOpType
AF = mybir.ActivationFunctionType
AX = mybir.AxisListType

NCORES = 8
D = 2048
DIN = 5632
DFF = 5632
NPT = 1024
NST = 64
T = NPT + NST
TT = [(0, 512), (512, 512), (1024, 64)]
NB = 16
EPS = 1e-6
NBUF = 4
SLOT = 4096
NEXP = 8
GAM = [1.0 - 2.0 ** (-5.0 - h) for h in range(4)]
SAME_SYNC = True
DBG = None
LITE = False
XCH = True


class StopEmit(Exception):
    pass


def dbg_stop(tag):
    if DBG == tag:
        raise StopEmit()


STAGE = 0


class Sync:
    def __init__(self, nc, es):
        self.nc = nc
        self.plan = False
        self.engs = {}
        for name, obj in [("pe", nc.tensor), ("dve", nc.vector), ("act", nc.scalar),
                          ("pool", nc.gpsimd), ("sp", nc.sync)]:
            self.engs[name] = dict(obj=obj, sem=es.enter_context(nc.semaphore("sem_" + name)), cnt=0, waited={})
        self.dpools = {}
        for q, n in [("sp", 8), ("pool", 6), ("act", 4)]:
            self.dpools[q] = dict(i=0, sems=[dict(sem=es.enter_context(nc.semaphore(f"d_{q}{i}")), cnt=0)
                                             for i in range(n)])
        self.lastw = {}
        self.readers = {}
        self.cc = [dict(sem=es.enter_context(nc.semaphore(f"cc{i}")), cnt=0) for i in range(2)]

    def coll(self, i, fn, reads=(), writes=()):
        if self.plan:
            return
        reads, writes = self._x(reads), self._x(writes)
        self._need("pool", self._deps(reads, writes))
        ins = fn()
        self.cc[i]["cnt"] += 1
        ins.then_inc(self.cc[i]["sem"])
        self._mark(reads, writes, (("cc", i), self.cc[i]["cnt"]))

    def reset(self):
        for c in self.cc:
            c["cnt"] = 0
        for e in self.engs.values():
            e["cnt"] = 0
            e["waited"] = {}
        for p in self.dpools.values():
            p["i"] = 0
            for s in p["sems"]:
                s["cnt"] = 0
        self.lastw = {}
        self.readers = {}

    def semh(self, sk):
        if isinstance(sk, str):
            return self.engs[sk]["sem"]
        if sk[0] == "cc":
            return self.cc[sk[1]]["sem"]
        return self.dpools[sk[0]]["sems"][sk[1]]["sem"]

    def _need(self, eng, deps):
        e = self.engs[eng]
        best = {}
        for sk, v in deps:
            if sk == eng and not SAME_SYNC:
                continue
            if v > best.get(sk, 0):
                best[sk] = v
        for sk, v in best.items():
            if e["waited"].get(sk, 0) >= v:
                continue
            e["obj"].wait_ge(self.semh(sk), v)
            e["waited"][sk] = v

    PSUM_KEYS = ("pb0", "pb1", "pb2", "pb3", "pb4", "pb5", "psS", "psB")

    def _deps(self, reads, writes):
        d = []
        for k in reads:
            if k in self.lastw:
                d.append(self.lastw[k])
            if k in self.PSUM_KEYS:
                d.extend(self.readers.get(k, {}).items())
        for k in writes:
            if k in self.lastw:
                d.append(self.lastw[k])
            d.extend(self.readers.get(k, {}).items())
        return d

    def _mark(self, reads, writes, dep):
        for k in reads:
            r = self.readers.setdefault(k, {})
            r[dep[0]] = max(r.get(dep[0], 0), dep[1])
        for k in writes:
            self.lastw[k] = dep
            self.readers[k] = {}

    ALIAS = {"RA": ("pb0", "pb1", "pb2"), "RB": ("pb3", "pb4", "pb5")}
    ALIAS.update({("xT", c): ("xT",) for c in range(16)})
    ALIAS.update({("hT", c): ("hT",) for c in range(16)})

    def _x(self, keys):
        out = []
        for k in keys:
            out.extend(self.ALIAS.get(k, (k,)))
        return out

    def op(self, eng, fns, reads=(), writes=()):
        if self.plan:
            return
        reads, writes = self._x(reads), self._x(writes)
        self._need(eng, self._deps(reads, writes))
        e = self.engs[eng]
        if callable(fns):
            fns = [fns]
        for f in fns[:-1]:
            f()
        ins = fns[-1]()
        e["cnt"] += 1
        ins.then_inc(e["sem"], 1)
        self._mark(reads, writes, (eng, e["cnt"]))

    def dma(self, q, out, in_, reads=(), writes=()):
        if self.plan:
            return
        reads, writes = self._x(reads), self._x(writes)
        p = self.dpools[q]
        idx = p["i"] % len(p["sems"])
        p["i"] += 1
        ds = p["sems"][idx]
        deps = self._deps(reads, writes)
        if ds["cnt"] > 0:
            deps.append(((q, idx), 16 * ds["cnt"]))
        self._need(q, deps)
        ins = self.engs[q]["obj"].dma_start(out=out, in_=in_)
        ds["cnt"] += 1
        ins.then_inc(ds["sem"], 16)
        self._mark(reads, writes, ((q, idx), 16 * ds["cnt"]))

    def barrier(self):
        if self.plan:
            return
        deps = [(n, e["cnt"]) for n, e in self.engs.items() if e["cnt"] > 0]
        for q, p in self.dpools.items():
            for i, s in enumerate(p["sems"]):
                if s["cnt"] > 0:
                    deps.append(((q, i), 16 * s["cnt"]))
        for i, c in enumerate(self.cc):
            if c["cnt"] > 0:
                deps.append((("cc", i), c["cnt"]))
        for n in self.engs:
            self._need(n, [d for d in deps if d[0] != n])

    def finish(self):
        self.barrier()


def build_program():
    nc = bass.Bass("TRN2", target_bir_lowering=False)

    def din(name, shape):
        if LITE and name in ("w_in", "w_out", "dense_w1", "dense_w3", "dense_w2", "moe_w1", "moe_w3", "moe_w2"):
            return nc.dram_tensor(name, [1] * (len(shape) - 2) + list(shape[-2:]), F32, kind="ExternalInput").ap()
        return nc.dram_tensor(name, list(shape), F32, kind="ExternalInput").ap()

    def dout(name, shape):
        return nc.dram_tensor(name, list(shape), F32, kind="ExternalOutput").ap()

    xin = din("xin", [T, D])
    st_ret = din("st_ret", [2, NB, 4, 128, 128])
    st_c31 = din("st_c31", [2, NB, 30, 512])
    st_c3 = din("st_c3", [2, NB, 2, 512])
    mix_norm_g = din("mix_norm_g", [2, D])
    w_in = din("w_in", [2, D, DIN])
    ret_norm_g = din("ret_norm_g", [2, 512])
    conv31_w = din("conv31_w", [2, 31, 512])
    conv31_b = din("conv31_b", [2, 512])
    conv_ln_g = din("conv_ln_g", [2, 512])
    conv_ln_b = din("conv_ln_b", [2, 512])
    sconv_w = din("sconv_w", [2, 3, 512])
    sgu_ln_g = din("sgu_ln_g", [2, 512])
    sgu_ln_b = din("sgu_ln_b", [2, 512])
    sgu_w = din("sgu_w", [2, 4, 128, 128])
    sgu_b = din("sgu_b", [2, 512])
    w_out = din("w_out", [2, D, D])
    ffn_norm_g = din("ffn_norm_g", [2, D])
    dense_w1 = din("dense_w1", [1, D, DFF])
    dense_w3 = din("dense_w3", [1, D, DFF])
    dense_w2 = din("dense_w2", [1, DFF, D])
    router_w = din("router_w", [D, NEXP])
    moe_w1 = din("moe_w1", [NEXP, D, DFF])
    moe_w3 = din("moe_w3", [NEXP, D, DFF])
    moe_w2 = din("moe_w2", [NEXP, DFF, D])
    final_norm_g = din("final_norm_g", [D])
    c_ident = din("c_ident", [128, 128])
    c_rope_c = din("c_rope_c", [128, T])
    c_rope_s = din("c_rope_s", [128, T])
    c_dmask = din("c_dmask", [128, 512])
    c_dmask_s = din("c_dmask_s", [64, 256])
    c_dq = din("c_dq", [128, 512])
    c_dq_s = din("c_dq_s", [128, 256])
    c_dkv = din("c_dkv", [128, 8])
    c_ind = din("c_ind", [64, NB])
    c_triu = din("c_triu", [128, 128])
    c_bm = din("c_bm", [64, 64])
    c_R = din("c_R", [4, 64])
    c_esel = din("c_esel", [8, 8 * 128])
    c_s0mask = din("c_s0mask", [128, 1])

    y_out = dout("y_out", [T, D])
    ret_p = dout("ret_p", [2, 4, 128, 128])
    ret_s = dout("ret_s", [2, NB, 4, 128, 128])
    c31_p = dout("c31_p", [2, 30, 512])
    c31_s = dout("c31_s", [2, NB, 30, 512])
    c3_p = dout("c3_p", [2, 2, 512])
    c3_s = dout("c3_s", [2, NB, 2, 512])
    vs_out = dout("vs_out", [2, NST, 512])

    xi = [nc.dram_tensor(f"xch_in{l}", [1024, 128], F32).ap() for l in range(2)]
    xg = [nc.dram_tensor(f"xch_all{l}", [2048, 128], F32).ap() for l in range(2)]

    es = ExitStack()
    with es:
        def sb(name, shape, dt=F32):
            return es.enter_context(nc.sbuf_tensor(name, list(shape), dt))

        xT = sb("xT", [128, 16, T])
        hT = sb("hT", [128, 16, T], BF16)
        mixT = sb("mixT", [128, 4, T], BF16)
        wsl = sb("wsl", [128, NBUF, SLOT], BF16)
        identf = sb("identf", [128, 128])
        identb = sb("identb", [128, 128], BF16)
        onesb = sb("onesb", [128, 128], BF16)
        rope_c = sb("rope_c", [128, T])
        rope_s = sb("rope_s", [128, T])
        dmask = sb("dmask", [128, 512])
        dmask_s = sb("dmask_s", [64, 256])
        dq = sb("dq", [128, 512])
        dq_s = sb("dq_s", [128, 256])
        dkv = sb("dkv", [128, 8])
        ind = sb("ind", [64, NB])
        triu = sb("triu", [128, 128])
        bm = sb("bm", [64, 64])
        Rm = sb("Rm", [4, 64])
        esel = sb("esel", [8, 8 * 128])
        s0mask = sb("s0mask", [128, 1])
        colT = sb("colT", [128, 512])
        epsc = sb("epsc", [128, 1])
        hx31 = sb("hx31", [128, 4, 30])
        hx3 = sb("hx3", [128, 4, 2])
        S0all = sb("S0all", [128, 4, 128])
        ARENA = 9728
        arena = sb("arena", [128, ARENA])
        arena_b = arena[:].bitcast(BF16)

        psA = es.enter_context(nc.psum_tensor("psA", [128, 3072], F32))
        psS = es.enter_context(nc.psum_tensor("psS", [128, 512], F32))
        psB = es.enter_context(nc.psum_tensor("psB", [128, 1024], BF16))

        S = Sync(nc, es)
        ROWBASE = {"RA": 0, "RB": 1536}

        def fa(off, n):
            assert off + n <= ARENA
            return arena[:, off:off + n]

        def ba(off_words, n):
            assert off_words * 2 + n <= 2 * ARENA
            return arena_b[:, off_words * 2: off_words * 2 + n]

        class WStream:
            def __init__(self):
                self.blocks = []
                self.i = 0
                self.issued = 0

            def reset(self):
                self.i = 0
                self.issued = 0

            def _issue(self, j):
                slot = j % NBUF
                for (a, b, c0, n, src) in self.blocks[j]:
                    dst = wsl[:, slot, 0:a * b].rearrange("p (a b) -> p a b", a=a)[:, :, c0:c0 + n]
                    S.dma("pool", dst, src, reads=[], writes=[("w", slot)])

            def get(self, parts):
                if S.plan:
                    self.blocks.append(parts)
                    self.i += 1
                    return (self.i - 1) % NBUF
                j = self.i
                while self.issued < min(len(self.blocks), j + NBUF - 1):
                    self._issue(self.issued)
                    self.issued += 1
                self.i += 1
                return j % NBUF

        W = WStream()

        def wview(slot, off, a, b):
            return wsl[:, slot, off:off + a * b].rearrange("p (a b) -> p a b", a=a)

        def win_cols(l, c0, n):
            return w_in[l, :, c0:c0 + n].rearrange("(kc p) n -> p kc n", p=128)

        def proj_fm(slot, coff, row, ncols=128, kdim=16, act=None):
            base = ROWBASE[row]
            src = act if act is not None else hT
            wv = wview(slot, 0, 16, 256) if kdim == 16 else None
            fns = []
            for kc in range(kdim):
                for (t0, tn) in TT:
                    fns.append(lambda kc=kc, t0=t0, tn=tn: nc.tensor.matmul(
                        psA[0:ncols, base + t0: base + t0 + tn], lhsT=wv[:, kc, coff:coff + ncols],
                        rhs=src[:, kc, t0:t0 + tn], start=(kc == 0), stop=(kc == kdim - 1)))
            S.op("pe", fns, reads=[("w", slot), "hT"], writes=[row])

        def prow(row, n=T, p=128):
            base = ROWBASE[row]
            return psA[0:p, base:base + n]

        def stat_bcast(src_bf_rows, keys, row):
            base = ROWBASE[row]
            fns = []
            n = len(src_bf_rows)
            for k, r in enumerate(src_bf_rows):
                for (t0, tn) in TT:
                    fns.append(lambda k=k, r=r, t0=t0, tn=tn: nc.tensor.matmul(
                        psA[:, base + t0: base + t0 + tn], lhsT=onesb[:], rhs=r[:, t0:t0 + tn],
                        start=(k == 0), stop=(k == n - 1)))
            S.op("pe", fns, reads=list(keys), writes=[row])

        def rsqrt_row(dst, src, scale, key_dst, key_src):
            S.op("act", lambda: nc.scalar.activation(out=dst, in_=src, func=AF.Sqrt, bias=epsc[:], scale=scale),
                 reads=[key_src], writes=[key_dst])
            S.op("dve", lambda: nc.vector.reciprocal(out=dst, in_=dst), reads=[key_dst], writes=[key_dst])

        def load_consts():
            for dst, src in [(identf, c_ident), (rope_c, c_rope_c), (rope_s, c_rope_s), (dmask, c_dmask),
                             (dmask_s, c_dmask_s), (dq, c_dq), (dq_s, c_dq_s), (dkv, c_dkv), (ind, c_ind),
                             (triu, c_triu), (bm, c_bm), (Rm, c_R), (esel, c_esel), (s0mask, c_s0mask)]:
                S.dma("sp", dst[:], src, writes=["const"])
            S.op("dve", lambda: nc.vector.tensor_copy(out=identb[:], in_=identf[:]), reads=["const"], writes=["identb"])
            S.op("dve", lambda: nc.vector.memset(onesb[:], 1.0), writes=["onesb"])
            S.op("dve", lambda: nc.vector.memset(epsc[:], EPS), writes=["epsc"])
            stg = fa(0, 512).rearrange("p (a b) -> p a b", a=4)
            S.op("dve", lambda: nc.vector.memset(fa(0, 512), 0.0), writes=["stg"])
            rows = {}

            def put(tile, r0, name, src_rows):
                n = src_rows.shape[0]
                S.dma("sp", stg[r0:r0 + n, tile, :], src_rows, reads=[], writes=["stg"])
                rows[name] = tile * 128 + r0
                return r0 + n

            r = 0
            for l in range(2):
                tile = l
                r = 0
                r = put(tile, r, ("mixg", l), mix_norm_g[l].rearrange("(c p) -> c p", p=128))
                r = put(tile, r, ("ffng", l), ffn_norm_g[l].rearrange("(c p) -> c p", p=128))
                r = put(tile, r, ("retg", l), ret_norm_g[l].rearrange("(c p) -> c p", p=128))
                r = put(tile, r, ("c31b", l), conv31_b[l].rearrange("(c p) -> c p", p=128))
                r = put(tile, r, ("clng", l), conv_ln_g[l].rearrange("(c p) -> c p", p=128))
                r = put(tile, r, ("clnb", l), conv_ln_b[l].rearrange("(c p) -> c p", p=128))
                r = put(tile, r, ("scw", l), sconv_w[l].rearrange("j (c p) -> (j c) p", p=128))
                if l == 0:
                    r = put(tile, r, ("fing",), final_norm_g.rearrange("(c p) -> c p", p=128))
            for l in range(2):
                put(2 + l, 0, ("c31w", l), conv31_w[l].rearrange("j (c p) -> (j c) p", p=128))
            fns = [lambda t=t: nc.tensor.transpose(psS[:, t * 128:(t + 1) * 128], stg[:, t, :], identf[:])
                   for t in range(4)]
            S.op("pe", fns, reads=["stg", "const"], writes=["psS"])
            S.op("dve", lambda: nc.vector.tensor_copy(out=colT[:], in_=psS[:]), reads=["psS"], writes=["colT"])
            return rows

        def col(rows, name, i=0):
            j = rows[name] + i
            return colT[:, j:j + 1]

        def load_x():
            for i in range(9):
                n = 128 if i < 8 else 64
                stage = fa(512 + (i % 2) * 2048, 2048)
                S.dma("sp", stage[0:n, :], xin[i * 128:i * 128 + n, :], writes=[("xst", i % 2)])
                for g in range(4):
                    bank = ROWBASE["RA"] + (g % 2) * 512
                    key = "pb%d" % (g % 2)
                    fns = [lambda j=j, g=g: nc.tensor.transpose(
                        psA[:, bank + j * 128: bank + j * 128 + n], stage[0:n, (g * 4 + j) * 128:(g * 4 + j + 1) * 128],
                        identf[0:n, 0:n]) for j in range(4)]
                    S.op("pe", fns, reads=[("xst", i % 2), "const"], writes=[key])
                    src = psA[:, bank:bank + 512].rearrange("p (a b) -> p a b", a=4)[:, :, 0:n]
                    S.op("dve" if g % 2 == 0 else "act",
                         (lambda src=src, g=g: nc.vector.tensor_copy(out=xT[:, g * 4:(g + 1) * 4, i * 128:i * 128 + n], in_=src))
                         if g % 2 == 0 else
                         (lambda src=src, g=g: nc.scalar.copy(out=xT[:, g * 4:(g + 1) * 4, i * 128:i * 128 + n], in_=src)),
                         reads=[key], writes=["xT"])

        def rmsnorm_to_hT(gname, rows, gi=0):
            sq = [ba(0, T), ba(T // 2, T)]
            rstd = fa(T, T)
            for c in range(16):
                b = sq[c % 2]
                S.op("act", lambda c=c, b=b: nc.scalar.activation(out=b, in_=xT[:, c, :], func=AF.Square),
                     reads=[("xT", c)], writes=[("sq", c % 2)])
                base = ROWBASE["RA"]
                fns = [lambda c=c, b=b, t0=t0, tn=tn: nc.tensor.matmul(
                    psA[:, base + t0: base + t0 + tn], lhsT=onesb[:], rhs=b[:, t0:t0 + tn],
                    start=(c == 0), stop=(c == 15)) for (t0, tn) in TT]
                S.op("pe", fns, reads=[("sq", c % 2), "onesb"], writes=["RA"])
            dbg_stop("n_a")
            rsqrt_row(rstd, prow("RA"), 1.0 / D, "rstd", "RA")
            dbg_stop("n_b")
            for c in range(16):
                S.op("dve", lambda c=c: nc.vector.scalar_tensor_tensor(
                    out=hT[:, c, :], in0=xT[:, c, :], scalar=col(rows, gname, gi + c), in1=rstd,
                    op0=ALU.mult, op1=ALU.mult), reads=[("xT", c), "rstd", "colT"], writes=[("hT", c)])

        def accum_out(slots, src_rows, src_key, kdim_per_slot=2):
            nk = len(slots) * kdim_per_slot
            for dm in range(16):
                row = "RA" if dm % 2 == 0 else "RB"
                base = ROWBASE[row]
                fns = []
                for k in range(nk):
                    wv = wview(slots[k // kdim_per_slot], 0, kdim_per_slot, 2048)
                    for (t0, tn) in TT:
                        fns.append(lambda k=k, wv=wv, t0=t0, tn=tn, dm=dm, base=base: nc.tensor.matmul(
                            psA[:, base + t0: base + t0 + tn],
                            lhsT=wv[:, k % kdim_per_slot, dm * 128:(dm + 1) * 128],
                            rhs=src_rows[k][:, t0:t0 + tn], start=(k == 0), stop=(k == nk - 1)))
                wkeys = [("w", s) for s in slots]
                if isinstance(src_key, list) and dm == 0:
                    nsplit = 3 * (nk - 1)
                    S.op("pe", fns[:nsplit], reads=wkeys + src_key[:nk - 1], writes=[row])
                    S.op("pe", fns[nsplit:], reads=wkeys + [src_key[nk - 1]], writes=[row])
                else:
                    S.op("pe", fns, reads=wkeys + (src_key if isinstance(src_key, list) else [src_key]), writes=[row])
                S.op("dve", lambda dm=dm, row=row: nc.vector.tensor_tensor(
                    out=xT[:, dm, :], in0=prow(row), in1=xT[:, dm, :], op=ALU.add),
                    reads=[row, ("xT", dm)], writes=[("xT", dm)])

        def wout_slots(l, m):
            sl = []
            for s in range(2):
                r0 = m * 512 + s * 256
                src = w_out[l, r0:r0 + 256, :].rearrange("(kc p) n -> p kc n", p=128)
                sl.append(W.get([(2, 2048, 0, 2048, src)]))
            return sl

        def rope_proj(slot, dst, dkey, f1, oT):
            wall = wview(slot, 0, 16, 256)
            wx = wall[:, :, 0:128]
            wlo = wall[:, :, 128:192]
            whi = wall[:, :, 192:256]
            for row, parts in (("RA", [(wx, 0, 128)]), ("RB", [(wlo, 0, 64), (whi, 64, 64)])):
                base = ROWBASE[row]
                fns = []
                for (wv, m0, mn) in parts:
                    for kc in range(16):
                        for (t0, tn) in TT:
                            fns.append(lambda wv=wv, m0=m0, mn=mn, kc=kc, t0=t0, tn=tn, base=base: nc.tensor.matmul(
                                psA[m0:m0 + mn, base + t0: base + t0 + tn], lhsT=wv[:, kc, :],
                                rhs=hT[:, kc, t0:t0 + tn], start=(kc == 0), stop=(kc == 15)))
                S.op("pe", fns, reads=[("w", slot), "hT"], writes=[row])
            S.op("dve", lambda: nc.vector.tensor_tensor(out=f1, in0=prow("RA"), in1=rope_c[:], op=ALU.mult),
                 reads=["RA", "const"], writes=["f1"])
            S.op("dve", lambda: nc.vector.tensor_tensor(out=oT, in0=prow("RB"), in1=rope_s[:], op=ALU.mult),
                 reads=["RB", "const"], writes=["oTtmp"])
            S.op("dve", lambda: nc.vector.tensor_tensor(out=dst, in0=f1, in1=oT, op=ALU.add),
                 reads=["f1", "oTtmp"], writes=[dkey])

        def pre_pass(l):
            f1 = fa(0, T)
            oT = fa(T, T)
            kT = ba(2 * T, T)
            ktok = ba(2 * T + T // 2, 1152).rearrange("p (a b) -> p a b", a=9)
            vtt = ba(2 * T + T // 2 + 576, 1152).rearrange("p (a b) -> p a b", a=9)
            o_sm = 2 * T + T // 2 + 1152
            Sx = fa(o_sm, 128)
            gl = fa(o_sm + 128, 128)
            gd = fa(o_sm + 256, 128)
            t1 = fa(o_sm + 384, 128)
            t2 = fa(o_sm + 512, 128)
            S.barrier()
            for h in range(4):
                kc_ = 1 * 512 + h * 128
                vc = 2 * 512 + h * 128
                s_k = W.get([(16, 256, 0, 128, win_cols(l, kc_, 128)),
                             (16, 256, 128, 64, win_cols(l, kc_ + 64, 64)),
                             (16, 256, 192, 64, win_cols(l, kc_, 64))])
                rope_proj(s_k, kT, "kT", f1, oT)
                s_v = W.get([(16, 256, 0, 128, win_cols(l, vc, 128))])
                wv_v = wview(s_v, 0, 16, 256)
                for grp in range(2):
                    base = ROWBASE["RB"]
                    fns = []
                    for j in range(4):
                        i = grp * 4 + j
                        for kc in range(16):
                            fns.append(lambda j=j, i=i, kc=kc: nc.tensor.matmul(
                                psA[:, base + j * 128: base + (j + 1) * 128], lhsT=hT[:, kc, i * 128:(i + 1) * 128],
                                rhs=wv_v[:, kc, 0:128], start=(kc == 0), stop=(kc == 15)))
                    S.op("pe", fns, reads=[("w", s_v), "hT"], writes=["RB"])
                    for j in range(4):
                        i = grp * 4 + j
                        S.op("dve", lambda j=j, i=i: nc.vector.tensor_scalar(
                            out=vtt[:, i, :], in0=psA[:, base + j * 128: base + (j + 1) * 128], scalar1=dkv[:, h:h + 1],
                            scalar2=float(GAM[h] ** (128 * (7 - i))), op0=ALU.mult, op1=ALU.mult),
                            reads=["RB", "const"], writes=["vtt"])
                    fns = [lambda j=j, grp=grp: nc.tensor.transpose(
                        psB[:, j * 128:(j + 1) * 128], kT[:, (grp * 4 + j) * 128:(grp * 4 + j + 1) * 128], identb[:])
                        for j in range(4)]
                    S.op("pe", fns, reads=["kT", "identb"], writes=["psB"])
                    S.op("act", lambda grp=grp: nc.scalar.copy(
                        out=ktok[:, grp * 4:grp * 4 + 4, :], in_=psB[:, 0:512].rearrange("p (a b) -> p a b", a=4)),
                        reads=["psB"], writes=["ktok"])
                fns = [lambda c=c: nc.tensor.matmul(psS[:, 0:128], lhsT=ktok[:, c, :], rhs=vtt[:, c, :],
                                                    start=(c == 0), stop=(c == 7)) for c in range(8)]
                S.op("pe", fns, reads=["ktok", "vtt"], writes=["psS"])
                S.op("act", lambda: nc.scalar.copy(out=Sx, in_=psS[:, 0:128]), reads=["psS"], writes=["Sx"])
                S.dma("sp", xi[l][h * 128:(h + 1) * 128, :], Sx, reads=["Sx"], writes=[("xi", l)])
            lc = slice(NPT - 128, NPT)
            for ct in range(4):
                s_b = W.get([(16, 256, 0, 128, win_cols(l, 2048 + ct * 128, 128)),
                             (16, 256, 128, 128, win_cols(l, 2560 + ct * 128, 128))])
                s_c = W.get([(16, 256, 0, 128, win_cols(l, 3584 + ct * 128, 128)),
                             (16, 256, 128, 128, win_cols(l, 4096 + ct * 128, 128))])
                fns = []
                for q_, sl_ in enumerate((s_b, s_b, s_c, s_c)):
                    wv = wview(sl_, 0, 16, 256)
                    for kc in range(16):
                        fns.append(lambda q_=q_, wv=wv, kc=kc: nc.tensor.matmul(
                            psS[:, q_ * 128:(q_ + 1) * 128], lhsT=wv[:, kc, (q_ % 2) * 128:(q_ % 2) * 128 + 128],
                            rhs=hT[:, kc, lc], start=(kc == 0), stop=(kc == 15)))
                S.op("pe", fns, reads=[("w", s_b), ("w", s_c), "hT"], writes=["psS"])
                S.op("act", lambda: nc.scalar.activation(out=t1, in_=psS[:, 128:256], func=AF.Sigmoid), reads=["psS"], writes=["t1"])
                S.op("act", lambda: nc.scalar.copy(out=t2, in_=psS[:, 256:384]), reads=["psS"], writes=["t2"])
                S.op("dve", lambda: nc.vector.tensor_tensor(out=gl, in0=psS[:, 0:128], in1=t1, op=ALU.mult),
                     reads=["psS", "t1"], writes=["gl"])
                S.op("dve", lambda: nc.vector.tensor_tensor(out=gd, in0=psS[:, 384:512], in1=t2, op=ALU.mult),
                     reads=["psS", "t2"], writes=["gd"])
                S.dma("sp", xi[l][512 + ct * 128:512 + (ct + 1) * 128, 0:30], gl[:, 98:128], reads=["gl"], writes=[("xi", l)])
                S.dma("sp", xi[l][512 + ct * 128:512 + (ct + 1) * 128, 32:34], gd[:, 126:128], reads=["gd"], writes=[("xi", l)])
            S.coll(l, lambda: nc.gpsimd.collective_compute(
                "AllGather", ALU.bypass, replica_groups=[[0, 1], [2, 3], [4, 5], [6, 7]], ins=[xi[l]], outs=[xg[l]]),
                reads=[("xi", l)], writes=[("xg", l)])

        def load_xch(l):
            if XCH:
                S.dma("sp", S0all[:], xg[l][0:512, :].rearrange("(h p) e -> p h e", p=128), reads=[("xg", l)], writes=["S0all"])
                S.dma("sp", hx31[:], xg[l][512:1024, 0:30].rearrange("(c p) r -> p c r", p=128), reads=[("xg", l)], writes=["hx31"])
                S.dma("sp", hx3[:], xg[l][512:1024, 32:34].rearrange("(c p) r -> p c r", p=128), reads=[("xg", l)], writes=["hx3"])
                for t_, k_ in ((S0all, "S0all"), (hx31, "hx31"), (hx3, "hx3")):
                    S.op("dve", lambda t_=t_: nc.vector.tensor_scalar(out=t_[:], in0=t_[:], scalar1=s0mask[:, 0:1], scalar2=None, op0=ALU.mult),
                         reads=[k_, "const"], writes=[k_])
            else:
                for t_, k_ in ((S0all, "S0all"), (hx31, "hx31"), (hx3, "hx3")):
                    S.op("dve", lambda t_=t_: nc.vector.memset(t_[:], 0.0), writes=[k_])

        def mixer_A(l, rows):
            o_f1 = 0
            o_oT = T
            o_q = 2 * T
            o_k = o_q + T // 2
            o_g = o_k + T // 2
            o_kt = o_g + T // 2
            o_v = o_kt + 576
            o_vt = o_v + 576
            o_sm = o_vt + 576
            f1 = fa(o_f1, T)
            oT = fa(o_oT, T)
            qT = ba(o_q, T)
            kT = ba(o_k, T)
            gS = ba(o_g, T)
            ktok = ba(o_kt, 1152).rearrange("p (a b) -> p a b", a=9)
            vtk = ba(o_v, 1152).rearrange("p (a b) -> p a b", a=9)
            vtt = ba(o_vt, 1152).rearrange("p (a b) -> p a b", a=9)
            PT = ba(o_sm, 128)
            Sb = ba(o_sm + 64, 128)
            Sf = fa(o_sm + 128, 128)
            crt = fa(o_sm + 256, 128)
            Ss16 = fa(o_sm + 384, 2048).rearrange("p (a b) -> p a b", a=16)
            Ssb = ba(o_sm + 2432, 512).rearrange("p (a b) -> p a b", a=4)
            Sso2 = [fa(o_sm + 2688 + k * 512, 512).rearrange("p (a b) -> p a b", a=4) for k in range(2)]
            vm = ba(o_sm + 3712, 512).rearrange("p (a b) -> p a b", a=4)
            assert o_sm + 3968 <= ARENA
            S.barrier()
            for h in range(4):
                qc = 0 * 512 + h * 128
                kc_ = 1 * 512 + h * 128
                vc = 2 * 512 + h * 128
                gc = 3 * 512 + h * 128
                for bg in range(4):
                    S.dma("sp", Ss16[:, bg * 4:(bg + 1) * 4, :], st_ret[l, bg * 4:(bg + 1) * 4, h].rearrange("b d e -> d b e"),
                          writes=[("Ss", bg)])

                def proj_qk(slot, dst, dkey):
                    wall = wview(slot, 0, 16, 256)
                    wx = wall[:, :, 0:128]
                    wlo = wall[:, :, 128:192]
                    whi = wall[:, :, 192:256]
                    for row, parts in (("RA", [(wx, 0, 128)]), ("RB", [(wlo, 0, 64), (whi, 64, 64)])):
                        base = ROWBASE[row]
                        fns = []
                        for (wv, m0, mn) in parts:
                            for kc in range(16):
                                for (t0, tn) in TT:
                                    fns.append(lambda wv=wv, m0=m0, mn=mn, kc=kc, t0=t0, tn=tn, base=base: nc.tensor.matmul(
                                        psA[m0:m0 + mn, base + t0: base + t0 + tn], lhsT=wv[:, kc, :],
                                        rhs=hT[:, kc, t0:t0 + tn], start=(kc == 0), stop=(kc == 15)))
                        S.op("pe", fns, reads=[("w", slot), "hT"], writes=[row])
                    S.op("dve", lambda: nc.vector.tensor_tensor(out=f1, in0=prow("RA"), in1=rope_c[:], op=ALU.mult),
                         reads=["RA", "const"], writes=["f1"])
                    S.op("dve", lambda: nc.vector.tensor_tensor(out=oT, in0=prow("RB"), in1=rope_s[:], op=ALU.mult),
                         reads=["RB", "const"], writes=["oTtmp"])
                    S.op("dve", lambda: nc.vector.tensor_tensor(out=dst, in0=f1, in1=oT, op=ALU.add),
                         reads=["f1", "oTtmp"], writes=[dkey])

                s_q = W.get([(16, 256, 0, 128, win_cols(l, qc, 128)),
                             (16, 256, 128, 64, win_cols(l, qc + 64, 64)),
                             (16, 256, 192, 64, win_cols(l, qc, 64))])
                proj_qk(s_q, qT, "qT")
                s_k = W.get([(16, 256, 0, 128, win_cols(l, kc_, 128)),
                             (16, 256, 128, 64, win_cols(l, kc_ + 64, 64)),
                             (16, 256, 192, 64, win_cols(l, kc_, 64))])
                proj_qk(s_k, kT, "kT")
                s_vg = W.get([(16, 256, 0, 128, win_cols(l, vc, 128)), (16, 256, 128, 128, win_cols(l, gc, 128))])
                wv_vg = wview(s_vg, 0, 16, 256)
                base = ROWBASE["RA"]
                fns = []
                for kc in range(16):
                    for (t0, tn) in TT:
                        fns.append(lambda kc=kc, t0=t0, tn=tn: nc.tensor.matmul(
                            psA[:, base + t0: base + t0 + tn], lhsT=wv_vg[:, kc, 128:256],
                            rhs=hT[:, kc, t0:t0 + tn], start=(kc == 0), stop=(kc == 15)))
                S.op("pe", fns, reads=[("w", s_vg), "hT"], writes=["RA"])
                S.op("act", lambda: nc.scalar.activation(out=gS, in_=prow("RA"), func=AF.Silu),
                     reads=["RA"], writes=["gS"])
                for grp in range(3):
                    tiles = [i for i in range(grp * 4, min(9, grp * 4 + 4))]
                    base = ROWBASE["RB"]
                    fns = []
                    for j, i in enumerate(tiles):
                        n = 128 if i < 8 else 64
                        for kc in range(16):
                            fns.append(lambda j=j, i=i, n=n, kc=kc: nc.tensor.matmul(
                                psA[0:n, base + j * 128: base + (j + 1) * 128], lhsT=hT[:, kc, i * 128:i * 128 + n],
                                rhs=wv_vg[:, kc, 0:128], start=(kc == 0), stop=(kc == 15)))
                    S.op("pe", fns, reads=[("w", s_vg), "hT"], writes=["RB"])
                    nt = len(tiles)
                    n = 128 if grp < 2 else 64
                    src = psA[0:n, base:base + nt * 128].rearrange("p (a b) -> p a b", a=nt)
                    S.op("act", lambda src=src, n=n, grp=grp, nt=nt: nc.scalar.copy(
                        out=vtk[0:n, grp * 4:grp * 4 + nt, :], in_=src), reads=["RB"], writes=["vtk"])
                    dcol = dkv[0:n, h:h + 1] if grp < 2 else dkv[0:n, 4 + h:5 + h]
                    S.op("dve", lambda src=src, n=n, grp=grp, nt=nt, dcol=dcol: nc.vector.tensor_scalar(
                        out=vtt[0:n, grp * 4:grp * 4 + nt, :], in0=src, scalar1=dcol, scalar2=None, op0=ALU.mult),
                        reads=["RB", "const"], writes=["vtt"])
                for grp in range(3):
                    tiles = [i for i in range(grp * 4, min(9, grp * 4 + 4))]
                    n = 128 if grp < 2 else 64
                    fns = [lambda j=j, i=i, n=n: nc.tensor.transpose(
                        psB[0:n, j * 128:(j + 1) * 128], kT[:, i * 128:i * 128 + n], identb[:])
                        for j, i in enumerate(tiles)]
                    S.op("pe", fns, reads=["kT", "identb"], writes=["psB"])
                    nt = len(tiles)
                    src = psB[0:n, 0:nt * 128].rearrange("p (a b) -> p a b", a=nt)
                    S.op("act", lambda src=src, n=n, grp=grp, nt=nt: nc.scalar.copy(
                        out=ktok[0:n, grp * 4:grp * 4 + nt, :], in_=src), reads=["psB"], writes=["ktok"])
                dbg_stop("A_proj")
                S.op("dve", lambda: nc.vector.tensor_copy(out=Sf, in_=S0all[:, h, :]), reads=["S0all"], writes=["Sf"])
                S.op("act", lambda: nc.scalar.copy(out=Sb, in_=Sf), reads=["Sf"], writes=["Sb"])
                gC = GAM[h] ** 128
                for c in range(8):
                    cs = slice(c * 128, (c + 1) * 128)
                    S.op("pe", lambda cs=cs: nc.tensor.matmul(psS[:, 0:128], lhsT=kT[:, cs], rhs=qT[:, cs],
                                                              start=True, stop=True),
                         reads=["kT", "qT"], writes=["psS"])
                    S.op("dve", lambda: nc.vector.tensor_tensor(out=PT, in0=psS[:, 0:128], in1=dmask[:, h * 128:(h + 1) * 128],
                                                                op=ALU.mult), reads=["psS", "const"], writes=["PT"])
                    S.op("pe", lambda c=c: nc.tensor.matmul(psA[:, 1536:1664], lhsT=vtk[:, c, :], rhs=PT, start=True, stop=True),
                         reads=["vtk", "PT"], writes=["pb3"])
                    S.op("pe", lambda cs=cs: nc.tensor.matmul(psA[:, 2048:2176], lhsT=Sb, rhs=qT[:, cs], start=True, stop=True),
                         reads=["Sb", "qT"], writes=["pb4"])
                    S.op("pe", lambda c=c: nc.tensor.matmul(psA[:, 2560:2688], lhsT=ktok[:, c, :], rhs=vtt[:, c, :],
                                                            start=True, stop=True),
                         reads=["ktok", "vtt"], writes=["pb5"])
                    S.op("dve", lambda: nc.vector.tensor_tensor(out=crt, in0=psA[:, 2048:2176], in1=dq[:, h * 128:(h + 1) * 128],
                                                                op=ALU.mult), reads=["pb4", "const"], writes=["crt"])
                    S.op("dve", lambda cs=cs: nc.vector.tensor_tensor(out=oT[:, cs], in0=psA[:, 1536:1664], in1=crt, op=ALU.add),
                         reads=["pb3", "crt", "oTtmp"], writes=["oT"])
                    S.op("dve", lambda: nc.vector.scalar_tensor_tensor(out=Sf, in0=Sf, scalar=float(gC), in1=psA[:, 2560:2688],
                                                                       op0=ALU.mult, op1=ALU.add),
                         reads=["pb5", "Sf"], writes=["Sf"])
                    S.op("act", lambda: nc.scalar.copy(out=Sb, in_=Sf), reads=["Sf"], writes=["Sb"])
                S.dma("sp", ret_p[l, h], Sf, reads=["Sf"], writes=[("o_retp", l, h)])
                sc = slice(NPT, T)
                S.op("pe", lambda: nc.tensor.matmul(psS[0:64, 0:64], lhsT=kT[:, sc], rhs=qT[:, sc], start=True, stop=True),
                     reads=["kT", "qT"], writes=["psS"])
                S.op("dve", lambda: nc.vector.tensor_tensor(out=PT[0:64, 0:64], in0=psS[0:64, 0:64],
                                                            in1=dmask_s[:, h * 64:(h + 1) * 64], op=ALU.mult),
                     reads=["psS", "const"], writes=["PT"])
                S.op("pe", lambda: nc.tensor.matmul(psA[:, 1536:1600], lhsT=vtk[0:64, 8, :], rhs=PT[0:64, 0:64],
                                                    start=True, stop=True), reads=["vtk", "PT"], writes=["pb3"])
                g4 = GAM[h] ** 4
                for bg in range(4):
                    Ss = Ss16[:, bg * 4:(bg + 1) * 4, :]
                    Sso = Sso2[bg % 2]
                    S.op("act", lambda Ss=Ss: nc.scalar.copy(out=Ssb[:], in_=Ss), reads=[("Ss", bg)], writes=["Ssb"])
                    fns = [lambda j=j, bg=bg: nc.tensor.matmul(
                        psA[:, 2048 + (bg * 4 + j) * 4: 2048 + (bg * 4 + j) * 4 + 4], lhsT=Ssb[:, j, :],
                        rhs=qT[:, NPT + (bg * 4 + j) * 4: NPT + (bg * 4 + j) * 4 + 4], start=True, stop=True)
                        for j in range(4)]
                    S.op("pe", fns, reads=["Ssb", "qT"], writes=["pb4"])
                    for j in range(4):
                        b = bg * 4 + j
                        S.op("dve", lambda j=j, b=b: nc.vector.tensor_scalar(
                            out=vm[0:64, j, :], in0=vtt[0:64, 8, :], scalar1=ind[:, b:b + 1], scalar2=None, op0=ALU.mult),
                            reads=["vtt", "const"], writes=["vm"])
                    base = ROWBASE["RA"]
                    fns = [lambda j=j: nc.tensor.matmul(psA[:, base + j * 128: base + (j + 1) * 128],
                                                        lhsT=ktok[0:64, 8, :], rhs=vm[0:64, j, :], start=True, stop=True)
                           for j in range(4)]
                    S.op("pe", fns, reads=["ktok", "vm"], writes=["RA"])
                    S.op("dve", lambda Ss=Ss, Sso=Sso: nc.vector.scalar_tensor_tensor(
                        out=Sso[:], in0=Ss, scalar=float(g4),
                        in1=psA[:, base:base + 512].rearrange("p (a b) -> p a b", a=4), op0=ALU.mult, op1=ALU.add),
                        reads=["RA", ("Ss", bg)], writes=[("Sso", bg % 2)])
                    S.dma("sp", ret_s[l, bg * 4:(bg + 1) * 4, h].rearrange("b d e -> d b e"), Sso[:],
                          reads=[("Sso", bg % 2)], writes=[("o_rets", l, h, bg)])
                S.op("dve", lambda: nc.vector.tensor_tensor(out=crt[:, 0:64], in0=psA[:, 2048:2112],
                                                            in1=dq_s[:, h * 64:(h + 1) * 64], op=ALU.mult),
                     reads=["pb4", "const"], writes=["crt"])
                S.op("dve", lambda: nc.vector.tensor_tensor(out=oT[:, sc], in0=psA[:, 1536:1600], in1=crt[:, 0:64], op=ALU.add),
                     reads=["pb3", "crt"], writes=["oT"])
                dbg_stop("A_chunks")
                osq = qT
                S.op("act", lambda: nc.scalar.activation(out=osq, in_=oT, func=AF.Square), reads=["oT", "qT"], writes=["qT"])
                stat_bcast([osq], ["qT", "onesb"], "RB")
                rsqrt_row(f1, prow("RB"), 1.0 / 128, "f1", "RB")
                S.op("dve", lambda: nc.vector.scalar_tensor_tensor(out=oT, in0=oT, scalar=col(rows, ("retg", l), h), in1=f1,
                                                                   op0=ALU.mult, op1=ALU.mult),
                     reads=["oT", "f1", "colT"], writes=["oT"])
                S.op("dve", lambda: nc.vector.tensor_tensor(out=mixT[:, h, :], in0=oT, in1=gS, op=ALU.mult),
                     reads=["oT", "gS"], writes=["mixT", "oTtmp"])
            sl = wout_slots(l, 0)
            accum_out(sl, [mixT[:, k, :] for k in range(4)], "mixT")

        def tm_rows(srcT_fn, ct, tstage, key):
            for which, (c0, n) in enumerate([(NPT - 128, 128), (NPT, 64)]):
                S.op("pe", lambda c0=c0, n=n, which=which: nc.tensor.transpose(
                    psS[0:n, which * 128: which * 128 + 128], srcT_fn(c0, n), identf[:]),
                    reads=[key, "const"], writes=["psS"])
                S.op("act", lambda n=n, which=which: nc.scalar.copy(
                    out=tstage[0:n, which, ct * 128:(ct + 1) * 128], in_=psS[0:n, which * 128: which * 128 + 128]),
                    reads=["psS"], writes=["tstage"])

        def mixer_B(l, rows):
            EXT = 30 + NPT + NB * 34
            o_ext = 0
            o_conv = 1600
            o_f1 = o_conv + 4 * T
            o_b1 = o_f1 + T
            o_hst = o_b1 + T
            ext = fa(o_ext, EXT)
            conv = fa(o_conv, 4 * T).rearrange("p (a b) -> p a b", a=4)
            f1 = fa(o_f1, T)
            b1 = ba(o_b1, T)
            b2 = ba(o_b1 + T // 2, T)
            hst = fa(o_hst, 512)
            tstage = fa(o_hst + 512, 1024).rearrange("p (a b) -> p a b", a=2)
            assert o_hst + 1536 <= ARENA
            S.barrier()
            ext_p = ext[:, 0:30 + NPT]
            ext_s = ext[:, 30 + NPT:EXT].rearrange("p (b r) -> p b r", b=NB)
            S.dma("act", c31_s[l, :, 0:26, :], st_c31[l, :, 4:30, :], writes=[("o_c31s_h", l)])
            for ct in range(4):
                s_b = W.get([(16, 256, 0, 128, win_cols(l, 2048 + ct * 128, 128)),
                             (16, 256, 128, 128, win_cols(l, 2560 + ct * 128, 128))])
                proj_fm(s_b, 0, "RA")
                proj_fm(s_b, 128, "RB")
                S.op("act", lambda: nc.scalar.activation(out=f1, in_=prow("RB"), func=AF.Sigmoid), reads=["RB"], writes=["f1"])
                S.op("dve", lambda: nc.vector.tensor_copy(out=ext[:, 0:30], in_=hx31[:, ct, :]), reads=["hx31"], writes=["ext"])
                hst4 = hst.rearrange("p (a b) -> p a b", a=4)
                for q4 in range(4):
                    S.dma("sp", hst4[0:120, q4, :],
                          st_c31[l, q4 * 4:(q4 + 1) * 4].rearrange("b r c -> (b r) c")[:, ct * 128:(ct + 1) * 128],
                          writes=[("hst", q4)])
                for q4 in range(4):
                    S.op("pe", lambda q4=q4: nc.tensor.transpose(psS[:, 256:376], hst4[0:120, q4, :], identf[0:120, 0:120]),
                         reads=[("hst", q4), "const"], writes=["psS"])
                    S.op("act", lambda q4=q4: nc.scalar.copy(
                        out=ext_s[:, q4 * 4:(q4 + 1) * 4, 0:30], in_=psS[:, 256:376].rearrange("p (b r) -> p b r", b=4)),
                        reads=["psS"], writes=["ext"])
                S.op("dve", lambda: nc.vector.tensor_tensor(out=ext_p[:, 30:30 + NPT], in0=prow("RA", NPT), in1=f1[:, 0:NPT], op=ALU.mult),
                     reads=["RA", "f1"], writes=["ext"])
                S.op("dve", lambda: nc.vector.tensor_tensor(
                    out=ext_s[:, :, 30:34], in0=psA[:, NPT:T].rearrange("p (b r) -> p b r", b=NB),
                    in1=f1[:, NPT:T].rearrange("p (b r) -> p b r", b=NB), op=ALU.mult),
                    reads=["RA", "f1"], writes=["ext"])
                def srcT(c0, n):
                    if c0 < NPT:
                        return ext_p[:, 30 + c0:30 + c0 + n]
                    return ext_s[:, :, 30:34]
                S.op("dve", lambda: nc.vector.tensor_copy(out=f1[:, 0:64].rearrange("p (b r) -> p b r", b=NB), in_=ext_s[:, :, 30:34]),
                     reads=["ext", "f1"], writes=["f1"])
                tm_rows(lambda c0, n: (ext_p[:, 30 + c0:30 + c0 + n] if c0 < NPT else f1[:, 0:64]), ct, tstage, "f1")
                wbase = rows[("c31w", l)]
                accp = [conv[:, ct, 0:NPT], f1[:, 0:NPT]]
                accs = [conv[:, ct, NPT:T].rearrange("p (b r) -> p b r", b=NB), f1[:, NPT:T].rearrange("p (b r) -> p b r", b=NB)]
                for j in range(31):
                    wc = colT[:, wbase + j * 4 + ct: wbase + j * 4 + ct + 1]
                    a_ = j % 2
                    kp, ks = ("cv", a_, "p"), ("cv", a_, "s")
                    if j == 0:
                        S.op("dve", lambda wc=wc: nc.vector.tensor_scalar(
                            out=accp[0], in0=ext_p[:, 0:NPT], scalar1=wc, scalar2=col(rows, ("c31b", l), ct),
                            op0=ALU.mult, op1=ALU.add), reads=["ext", "colT"], writes=[kp, "conv"])
                        S.op("dve", lambda wc=wc: nc.vector.tensor_scalar(
                            out=accs[0], in0=ext_s[:, :, 0:4], scalar1=wc,
                            scalar2=col(rows, ("c31b", l), ct), op0=ALU.mult, op1=ALU.add),
                            reads=["ext", "colT"], writes=[ks])
                    elif j == 1:
                        S.op("dve", lambda wc=wc: nc.vector.tensor_scalar(
                            out=accp[1], in0=ext_p[:, 1:1 + NPT], scalar1=wc, scalar2=None, op0=ALU.mult),
                            reads=["ext", "colT"], writes=[kp, "f1"])
                        S.op("dve", lambda wc=wc: nc.vector.tensor_scalar(
                            out=accs[1], in0=ext_s[:, :, 1:5], scalar1=wc, scalar2=None, op0=ALU.mult),
                            reads=["ext", "colT"], writes=[ks])
                    else:
                        S.op("dve", lambda wc=wc, j=j, a_=a_: nc.vector.scalar_tensor_tensor(
                            out=accp[a_], in0=ext_p[:, j:j + NPT], scalar=wc, in1=accp[a_],
                            op0=ALU.mult, op1=ALU.add), reads=["ext", "colT", kp], writes=[kp])
                        S.op("dve", lambda wc=wc, j=j, a_=a_: nc.vector.scalar_tensor_tensor(
                            out=accs[a_], in0=ext_s[:, :, j:j + 4], scalar=wc, in1=accs[a_],
                            op0=ALU.mult, op1=ALU.add), reads=["ext", "colT", ks], writes=[ks])
                S.op("dve", lambda: nc.vector.tensor_tensor(out=conv[:, ct, :], in0=conv[:, ct, :], in1=f1, op=ALU.add),
                     reads=[("cv", 0, "p"), ("cv", 0, "s"), ("cv", 1, "p"), ("cv", 1, "s"), "f1", "conv"],
                     writes=["conv", ("cv", 0, "p"), ("cv", 0, "s")])
            S.dma("sp", c31_p[l], tstage[98:128, 0, :], reads=["tstage"], writes=[("o_c31p", l)])
            for s in range(4):
                S.dma("sp", c31_s[l, :, 26 + s, :], tstage[s:64:4, 1, :], reads=["tstage"], writes=[("o_c31s", l, s)])
            for ct in range(4):
                b = b1 if ct % 2 == 0 else b2
                S.op("act", lambda ct=ct, b=b: nc.scalar.copy(out=b, in_=conv[:, ct, :]), reads=["conv"], writes=[("b12", ct % 2)])
                base = ROWBASE["RA"]
                fns = [lambda ct=ct, b=b, t0=t0, tn=tn: nc.tensor.matmul(
                    psA[:, base + t0: base + t0 + tn], lhsT=onesb[:], rhs=b[:, t0:t0 + tn], start=(ct == 0), stop=(ct == 3))
                    for (t0, tn) in TT]
                S.op("pe", fns, reads=[("b12", ct % 2), "onesb"], writes=["RA"])
            S.op("act", lambda: nc.scalar.mul(out=f1, in_=prow("RA"), mul=1.0 / 512), reads=["RA"], writes=["f1"])
            for ct in range(4):
                S.op("dve", lambda ct=ct: nc.vector.tensor_tensor(out=conv[:, ct, :], in0=conv[:, ct, :], in1=f1, op=ALU.subtract),
                     reads=["conv", "f1"], writes=["conv"])
            for ct in range(4):
                b = b1 if ct % 2 == 0 else b2
                S.op("act", lambda ct=ct, b=b: nc.scalar.activation(out=b, in_=conv[:, ct, :], func=AF.Square),
                     reads=["conv"], writes=[("b12", ct % 2)])
                base = ROWBASE["RB"]
                fns = [lambda ct=ct, b=b, t0=t0, tn=tn: nc.tensor.matmul(
                    psA[:, base + t0: base + t0 + tn], lhsT=onesb[:], rhs=b[:, t0:t0 + tn], start=(ct == 0), stop=(ct == 3))
                    for (t0, tn) in TT]
                S.op("pe", fns, reads=[("b12", ct % 2), "onesb"], writes=["RB"])
            rsqrt_row(f1, prow("RB"), 1.0 / 512, "f1", "RB")
            for ct in range(4):
                S.op("dve", lambda ct=ct: nc.vector.scalar_tensor_tensor(
                    out=conv[:, ct, :], in0=conv[:, ct, :], scalar=col(rows, ("clng", l), ct), in1=f1,
                    op0=ALU.mult, op1=ALU.mult), reads=["conv", "f1", "colT"], writes=["conv"])
                S.op("act", lambda ct=ct: nc.scalar.activation(out=mixT[:, ct, :], in_=conv[:, ct, :], func=AF.Silu,
                                                              bias=col(rows, ("clnb", l), ct), scale=1.0),
                     reads=["conv", "colT"], writes=["mixT"])
            sl = wout_slots(l, 1)
            accum_out(sl, [mixT[:, k, :] for k in range(4)], "mixT")

        def mixer_C(l, rows):
            EXT = 2 + NPT + NB * 6
            o_ext = 0
            o_f1 = 1124
            o_cv = o_f1 + T
            o_hst = o_cv + T
            ext = fa(o_ext, EXT)
            f1 = fa(o_f1, T)
            cv = fa(o_cv, T)
            hst = fa(o_hst, 512)
            tstage = fa(o_hst + 512, 1024).rearrange("p (a b) -> p a b", a=2)
            S.barrier()
            ext_p = ext[:, 0:2 + NPT]
            ext_s = ext[:, 2 + NPT:EXT].rearrange("p (b r) -> p b r", b=NB)
            S.dma("sp", hst[0:32, :], st_c3[l].rearrange("b r c -> (b r) c"), writes=["hst"])
            for ct in range(4):
                s_c = W.get([(16, 256, 0, 128, win_cols(l, 3584 + ct * 128, 128)),
                             (16, 256, 128, 128, win_cols(l, 4096 + ct * 128, 128))])
                proj_fm(s_c, 0, "RA")
                proj_fm(s_c, 128, "RB")
                S.op("act", lambda: nc.scalar.copy(out=f1, in_=prow("RA")), reads=["RA"], writes=["f1"])
                S.op("dve", lambda: nc.vector.tensor_copy(out=ext[:, 0:2], in_=hx3[:, ct, :]), reads=["hx3"], writes=["ext"])
                S.op("pe", lambda: nc.tensor.transpose(psS[:, 256:288], hst[0:32, ct * 128:(ct + 1) * 128], identf[0:32, 0:32]),
                     reads=["hst", "const"], writes=["psS"])
                S.op("act", lambda: nc.scalar.copy(out=ext_s[:, :, 0:2], in_=psS[:, 256:288].rearrange("p (b r) -> p b r", b=NB)),
                     reads=["psS"], writes=["ext"])
                S.op("dve", lambda: nc.vector.tensor_tensor(out=ext_p[:, 2:2 + NPT], in0=prow("RB", NPT), in1=f1[:, 0:NPT], op=ALU.mult),
                     reads=["RB", "f1"], writes=["ext"])
                S.op("dve", lambda: nc.vector.tensor_tensor(
                    out=ext_s[:, :, 2:6], in0=psA[:, 1536 + NPT:1536 + T].rearrange("p (b r) -> p b r", b=NB),
                    in1=f1[:, NPT:T].rearrange("p (b r) -> p b r", b=NB), op=ALU.mult), reads=["RB", "f1"], writes=["ext"])
                S.op("dve", lambda: nc.vector.tensor_copy(out=f1[:, 0:64].rearrange("p (b r) -> p b r", b=NB), in_=ext_s[:, :, 2:6]),
                     reads=["ext", "f1"], writes=["f1"])
                tm_rows(lambda c0, n: (ext_p[:, 2 + c0:2 + c0 + n] if c0 < NPT else f1[:, 0:64]), ct, tstage, "f1")
                wbase = rows[("scw", l)]
                for j in range(3):
                    wc = colT[:, wbase + j * 4 + ct: wbase + j * 4 + ct + 1]
                    if j == 0:
                        S.op("dve", lambda wc=wc: nc.vector.tensor_scalar(out=cv[:, 0:NPT], in0=ext_p[:, 0:NPT], scalar1=wc, scalar2=None,
                                                                          op0=ALU.mult), reads=["ext", "colT"], writes=["cv"])
                        S.op("dve", lambda wc=wc: nc.vector.tensor_scalar(
                            out=cv[:, NPT:T].rearrange("p (b r) -> p b r", b=NB), in0=ext_s[:, :, 0:4], scalar1=wc, scalar2=None,
                            op0=ALU.mult), reads=["ext", "colT"], writes=["cv"])
                    else:
                        S.op("dve", lambda wc=wc, j=j: nc.vector.scalar_tensor_tensor(
                            out=cv[:, 0:NPT], in0=ext_p[:, j:j + NPT], scalar=wc, in1=cv[:, 0:NPT], op0=ALU.mult, op1=ALU.add),
                            reads=["ext", "colT", "cv"], writes=["cv"])
                        S.op("dve", lambda wc=wc, j=j: nc.vector.scalar_tensor_tensor(
                            out=cv[:, NPT:T].rearrange("p (b r) -> p b r", b=NB), in0=ext_s[:, :, j:j + 4], scalar=wc,
                            in1=cv[:, NPT:T].rearrange("p (b r) -> p b r", b=NB), op0=ALU.mult, op1=ALU.add),
                            reads=["ext", "colT", "cv"], writes=["cv"])
                if ct % 2 == 0:
                    s_cb = W.get([(16, 256, 0, 256, win_cols(l, 3072 + ct * 128, 256))])
                proj_fm(s_cb, (ct % 2) * 128, "RA")
                S.op("dve", lambda ct=ct: nc.vector.tensor_tensor(out=mixT[:, ct, :], in0=prow("RA"), in1=cv, op=ALU.mult),
                     reads=["RA", "cv"], writes=["mixT"])
            S.dma("sp", c3_p[l], tstage[126:128, 0, :], reads=["tstage"], writes=[("o_c3p", l)])
            for s in range(2):
                S.dma("sp", c3_s[l, :, s, :], tstage[2 + s:64:4, 1, :], reads=["tstage"], writes=[("o_c3s", l, s)])
            sl = wout_slots(l, 2)
            accum_out(sl, [mixT[:, k, :] for k in range(4)], "mixT")

        def mixer_D(l, rows):
            o_lng = 0
            o_lnb = 512
            o_sgb = 1024
            o_vn = 1536
            o_sq = 2048
            o_vb = 2560
            o_wm = o_vb + 2304
            o_wms = o_wm + 256
            o_wst = o_wms + 128
            o_sm = o_wst + 512
            o_u = o_sm + 8
            o_mb = o_u + 64
            lng = fa(o_lng, 512)
            lnb = fa(o_lnb, 512)
            sgb = fa(o_sgb, 512)
            vn = fa(o_vn, 512)
            sqs = fa(o_sq, 512)
            vb = ba(o_vb, 9 * 512).rearrange("p (a b) -> p a b", a=9)
            wm = ba(o_wm, 512).rearrange("p (a b) -> p a b", a=4)
            wms = ba(o_wms, 256).rearrange("p (a b) -> p a b", a=4)
            wst4 = [fa(o_wst + k * 128, 128) for k in range(4)]
            sm = fa(o_sm, 8)
            U = fa(o_u, 64)
            mb = fa(o_mb, T)
            vn2 = [vn, fa(o_mb + T, 512)]
            assert o_mb + T + 512 <= ARENA
            S.barrier()
            S.dma("sp", lng, sgu_ln_g[l].partition_broadcast(128), writes=["lng"])
            S.dma("sp", lnb, sgu_ln_b[l].partition_broadcast(128), writes=["lnb"])
            S.dma("sp", sgb, sgu_b[l].partition_broadcast(128), writes=["sgb"])
            for g in range(4):
                S.dma("sp", wst4[g], sgu_w[l, g], writes=[("wst", g)])
            for g in range(4):
                wst = wst4[g]
                S.op("pe", lambda wst=wst: nc.tensor.transpose(psS[:, 0:128], wst, identf[:]), reads=[("wst", g), "const"], writes=["psS"])
                S.op("dve", lambda g=g: nc.vector.tensor_tensor(out=wm[:, g, :], in0=psS[:, 0:128], in1=triu[:], op=ALU.mult),
                     reads=["psS", "const"], writes=["wm"])
                S.op("pe", lambda wst=wst: nc.tensor.matmul(psS[0:4, 128:192], lhsT=wst[0:4, 0:4], rhs=Rm[:], start=True, stop=True),
                     reads=[("wst", g), "const"], writes=["psS"])
                S.op("act", lambda: nc.scalar.copy(out=U[0:4, :], in_=psS[0:4, 128:192]), reads=["psS"], writes=["U"])
                S.op("pe", lambda: nc.tensor.matmul(psS[0:64, 256:320], lhsT=Rm[:], rhs=U[0:4, :], start=True, stop=True),
                     reads=["U", "const"], writes=["psS"])
                S.op("dve", lambda g=g: nc.vector.tensor_tensor(out=wms[0:64, g, :], in0=psS[0:64, 256:320], in1=bm[:], op=ALU.mult),
                     reads=["psS", "const"], writes=["wms"])
            dbg_stop("D_w")
            s_v0 = W.get([(16, 256, 0, 256, win_cols(l, 5120, 256))])
            s_v1 = W.get([(16, 256, 0, 256, win_cols(l, 5376, 256))])
            for i in range(9):
                n = 128 if i < 8 else 64
                row = "RA" if i % 2 == 0 else "RB"
                base = ROWBASE[row]
                fns = []
                for half, sl_ in enumerate((s_v0, s_v1)):
                    wv = wview(sl_, 0, 16, 256)
                    for kc in range(16):
                        fns.append(lambda half=half, wv=wv, kc=kc, i=i, n=n, base=base: nc.tensor.matmul(
                            psA[0:n, base + half * 256: base + half * 256 + 256], lhsT=hT[:, kc, i * 128:i * 128 + n],
                            rhs=wv[:, kc, :], start=(kc == 0), stop=(kc == 15)))
                S.op("pe", fns, reads=[("w", s_v0), ("w", s_v1), "hT"], writes=[row])
                dv = psA[0:n, base:base + 512]
                vn = vn2[i % 2]
                kv = ("vn", i % 2)
                S.op("act", lambda dv=dv, n=n, vn=vn: nc.scalar.copy(out=vn[0:n, :], in_=dv), reads=[row], writes=[kv])
                S.op("dve", lambda n=n, vn=vn: nc.vector.tensor_reduce(out=sm[0:n, 0:1], in_=vn[0:n, :], axis=AX.X, op=ALU.add),
                     reads=[kv], writes=["sm0"])
                S.op("dve", lambda n=n, vn=vn: nc.vector.tensor_tensor(out=sqs[0:n, :], in0=vn[0:n, :], in1=vn[0:n, :], op=ALU.mult),
                     reads=[kv], writes=["sqs"])
                S.op("dve", lambda n=n: nc.vector.tensor_reduce(out=sm[0:n, 1:2], in_=sqs[0:n, :], axis=AX.X, op=ALU.add),
                     reads=["sqs"], writes=["sm1"])
                S.op("dve", lambda n=n: nc.vector.tensor_scalar(out=sm[0:n, 2:3], in0=sm[0:n, 0:1], scalar1=1.0 / 512, scalar2=None, op0=ALU.mult),
                     reads=["sm0"], writes=["sm2"])
                S.op("dve", lambda n=n: nc.vector.tensor_tensor(out=sm[0:n, 3:4], in0=sm[0:n, 2:3], in1=sm[0:n, 2:3], op=ALU.mult),
                     reads=["sm2"], writes=["sm3"])
                S.op("dve", lambda n=n: nc.vector.scalar_tensor_tensor(out=sm[0:n, 4:5], in0=sm[0:n, 1:2], scalar=1.0 / 512, in1=sm[0:n, 3:4],
                                                                       op0=ALU.mult, op1=ALU.subtract), reads=["sm1", "sm3"], writes=["sm4"])
                S.op("act", lambda n=n: nc.scalar.activation(out=sm[0:n, 5:6], in_=sm[0:n, 4:5], func=AF.Sqrt, bias=epsc[0:n, :], scale=1.0),
                     reads=["sm4", "epsc"], writes=["sm5"])
                S.op("dve", lambda n=n: nc.vector.reciprocal(out=sm[0:n, 6:7], in_=sm[0:n, 5:6]), reads=["sm5"], writes=["sm6"])
                S.op("dve", lambda n=n, vn=vn: nc.vector.tensor_scalar(out=vn[0:n, :], in0=vn[0:n, :], scalar1=sm[0:n, 2:3], scalar2=sm[0:n, 6:7],
                                                                       op0=ALU.subtract, op1=ALU.mult),
                     reads=[kv, "sm2", "sm6"], writes=[kv])
                S.op("dve", lambda n=n, vn=vn: nc.vector.tensor_tensor(out=vn[0:n, :], in0=vn[0:n, :], in1=lng[0:n, :], op=ALU.mult),
                     reads=[kv, "lng"], writes=[kv])
                S.op("dve", lambda n=n, vn=vn: nc.vector.tensor_tensor(out=vn[0:n, :], in0=vn[0:n, :], in1=lnb[0:n, :], op=ALU.add),
                     reads=[kv, "lnb"], writes=[kv])
                S.op("act", lambda n=n, i=i, vn=vn: nc.scalar.copy(out=vb[0:n, i, :], in_=vn[0:n, :]), reads=[kv], writes=["vb"])
                if i == 8:
                    S.dma("sp", vs_out[l], vn[0:64, :], reads=[kv], writes=[("o_vs", l)])
            dbg_stop("D_ln")
            for g in range(4):
                if g % 2 == 0:
                    s_u = W.get([(16, 256, 0, 256, win_cols(l, 4608 + g * 128, 256))])
                base = ROWBASE["RA"]
                fns = [lambda c=c, g=g: nc.tensor.matmul(psA[:, base + c * 128: base + (c + 1) * 128],
                                                         lhsT=vb[:, c, g * 128:(g + 1) * 128], rhs=wm[:, g, :], start=True, stop=True)
                       for c in range(8)]
                fns.append(lambda g=g: nc.tensor.matmul(psA[:, base + NPT: base + T], lhsT=vb[0:64, 8, g * 128:(g + 1) * 128],
                                                        rhs=wms[0:64, g, :], start=True, stop=True))
                S.op("pe", fns, reads=["vb", "wm", "wms"], writes=["RA"])
                S.op("dve", lambda g=g: nc.vector.tensor_tensor(
                    out=mb[:, 0:NPT].rearrange("p (c t) -> p c t", c=8), in0=psA[:, base:base + NPT].rearrange("p (c t) -> p c t", c=8),
                    in1=sgb[:, g * 128:(g + 1) * 128].unsqueeze(1).to_broadcast([128, 8, 128]), op=ALU.add),
                    reads=["RA", "sgb"], writes=["mb"])
                S.op("dve", lambda g=g: nc.vector.tensor_tensor(
                    out=mb[:, NPT:T].rearrange("p (b r) -> p b r", b=NB), in0=psA[:, base + NPT:base + T].rearrange("p (b r) -> p b r", b=NB),
                    in1=sgb[:, g * 128:g * 128 + 4].unsqueeze(1).to_broadcast([128, NB, 4]), op=ALU.add),
                    reads=["RA", "sgb"], writes=["mb"])
                proj_fm(s_u, (g % 2) * 128, "RB")
                S.op("dve", lambda g=g: nc.vector.tensor_tensor(out=mixT[:, g, :], in0=prow("RB"), in1=mb, op=ALU.mult),
                     reads=["RB", "mb"], writes=["mixT"])
            dbg_stop("D_mix")
            sl = wout_slots(l, 3)
            accum_out(sl, [mixT[:, k, :] for k in range(4)], "mixT")

        def ffn_pass(w1, w3, w2, gm=None):
            o_act = 0
            o_s = 2 * T
            act = ba(o_act, 4 * T).rearrange("p (a b) -> p a b", a=4)
            sbuf = [fa(o_s, T), fa(o_s + T, T)]
            for grp in range(DFF // 512):
                f0 = grp * 512
                for half in range(2):
                    c0 = f0 + half * 256
                    s1 = W.get([(16, 256, 0, 256, w1[:, c0:c0 + 256].rearrange("(kc p) n -> p kc n", p=128))])
                    s3 = W.get([(16, 256, 0, 256, w3[:, c0:c0 + 256].rearrange("(kc p) n -> p kc n", p=128))])
                    for t in range(2):
                        ti = half * 2 + t
                        sb_ = sbuf[ti % 2]
                        skey = ("fs", ti % 2)
                        proj_fm(s1, t * 128, "RA")
                        S.op("act", lambda sb_=sb_: nc.scalar.activation(out=sb_, in_=prow("RA"), func=AF.Silu),
                             reads=["RA"], writes=[skey])
                        proj_fm(s3, t * 128, "RB")
                        if gm is not None:
                            S.op("dve", lambda sb_=sb_: nc.vector.tensor_tensor(out=sb_, in0=sb_, in1=gm, op=ALU.mult),
                                 reads=[skey, "gm"], writes=[skey])
                        S.op("dve", lambda sb_=sb_, ti=ti: nc.vector.tensor_tensor(out=act[:, ti, :], in0=prow("RB"), in1=sb_, op=ALU.mult),
                             reads=["RB", skey], writes=[("actT", ti)])
                sl = []
                for half in range(2):
                    r0 = f0 + half * 256
                    sl.append(W.get([(2, 2048, 0, 2048, w2[r0:r0 + 256, :].rearrange("(kc p) n -> p kc n", p=128))]))
                accum_out(sl, [act[:, k, :] for k in range(4)], [("actT", k) for k in range(4)])

        def moe(rows):
            o_gm = 4 * T
            o_rw = o_gm + T
            o_lg = o_rw + 128
            gm = fa(o_gm, T)
            rw = fa(o_rw, 128).rearrange("p (a b) -> p a b", a=16)
            lg = fa(o_lg, 72).rearrange("p (a b) -> p a b", a=9)
            m1 = fa(o_lg + 72, 9)
            m2 = fa(o_lg + 84, 9)
            oh1 = fa(o_lg + 96, 72).rearrange("p (a b) -> p a b", a=9)
            oh2 = fa(o_lg + 168, 72).rearrange("p (a b) -> p a b", a=9)
            tmp = fa(o_lg + 240, 72).rearrange("p (a b) -> p a b", a=9)
            g1 = fa(o_lg + 312, 9)
            g2 = fa(o_lg + 324, 9)
            G = fa(o_lg + 336, 72).rearrange("p (a b) -> p a b", a=9)
            GT = fa(o_lg + 408, T)
            assert o_lg + 408 + T <= ARENA
            S.barrier()
            S.op("dve", lambda: nc.vector.memset(lg, -1e30), writes=["lg"])
            S.dma("sp", rw, router_w.rearrange("(c p) e -> p c e", p=128), writes=["rw"])
            for c in range(16):
                S.op("dve", lambda c=c: nc.vector.tensor_scalar(out=rw[:, c, :], in0=rw[:, c, :], scalar1=col(rows, ("ffng", 1), c),
                                                                scalar2=None, op0=ALU.mult), reads=["rw", "colT"], writes=["rw"])
            for i in range(9):
                n = 128 if i < 8 else 64
                fns = [lambda c=c, i=i, n=n: nc.tensor.matmul(psS[0:n, i * 8:(i + 1) * 8], lhsT=xT[:, c, i * 128:i * 128 + n],
                                                              rhs=rw[:, c, :], start=(c == 0), stop=(c == 15)) for c in range(16)]
                S.op("pe", fns, reads=["xT", "rw"], writes=["psS"])
            rstd_row = fa(T, T)
            onesf = fa(o_lg + 348, 1)
            S.op("dve", lambda: nc.vector.memset(onesf, 1.0), writes=["onesf"])
            fns = [lambda i=i: nc.tensor.matmul(psS[0:(128 if i < 8 else 64), 128 + i:129 + i],
                                                lhsT=rstd_row[0:1, i * 128:i * 128 + (128 if i < 8 else 64)], rhs=onesf[0:1, 0:1],
                                                start=True, stop=True) for i in range(9)]
            S.op("pe", fns, reads=["rstd", "onesf"], writes=["psS"])
            S.op("act", lambda: nc.scalar.copy(out=g2, in_=psS[:, 128:137]), reads=["psS"], writes=["g2"])
            for i in range(9):
                n = 128 if i < 8 else 64
                S.op("dve", lambda i=i, n=n: nc.vector.tensor_scalar(out=lg[0:n, i, :], in0=psS[0:n, i * 8:(i + 1) * 8], scalar1=g2[0:n, i:i + 1],
                                                                     scalar2=None, op0=ALU.mult), reads=["psS", "g2", "lg"], writes=["lg"])
            S.op("dve", lambda: nc.vector.tensor_reduce(out=m1, in_=lg, axis=AX.X, op=ALU.max), reads=["lg"], writes=["m1"])
            S.op("dve", lambda: nc.vector.tensor_tensor(out=oh1, in0=lg, in1=m1.unsqueeze(2).to_broadcast([128, 9, 8]), op=ALU.is_equal),
                 reads=["lg", "m1"], writes=["oh1"])
            S.op("dve", lambda: nc.vector.scalar_tensor_tensor(out=tmp, in0=oh1, scalar=-1e30, in1=lg, op0=ALU.mult, op1=ALU.add),
                 reads=["oh1", "lg"], writes=["tmp"])
            S.op("dve", lambda: nc.vector.tensor_reduce(out=m2, in_=tmp, axis=AX.X, op=ALU.max), reads=["tmp"], writes=["m2"])
            S.op("dve", lambda: nc.vector.tensor_tensor(out=oh2, in0=tmp, in1=m2.unsqueeze(2).to_broadcast([128, 9, 8]), op=ALU.is_equal),
                 reads=["tmp", "m2"], writes=["oh2"])
            S.op("dve", lambda: nc.vector.tensor_tensor(out=g1, in0=m1, in1=m2, op=ALU.subtract), reads=["m1", "m2"], writes=["g1"])
            S.op("act", lambda: nc.scalar.activation(out=g1, in_=g1, func=AF.Sigmoid), reads=["g1"], writes=["g1"])
            S.op("dve", lambda: nc.vector.tensor_scalar(out=g2, in0=g1, scalar1=-1.0, scalar2=1.0, op0=ALU.mult, op1=ALU.add),
                 reads=["g1", "g2"], writes=["g2"])
            S.op("dve", lambda: nc.vector.tensor_tensor(out=G, in0=oh1, in1=g1.unsqueeze(2).to_broadcast([128, 9, 8]), op=ALU.mult),
                 reads=["oh1", "g1"], writes=["G"])
            S.op("dve", lambda: nc.vector.tensor_tensor(out=tmp, in0=oh2, in1=g2.unsqueeze(2).to_broadcast([128, 9, 8]), op=ALU.mult),
                 reads=["oh2", "g2", "tmp"], writes=["tmp"])
            S.op("dve", lambda: nc.vector.tensor_tensor(out=G, in0=G, in1=tmp, op=ALU.add), reads=["G", "tmp"], writes=["G"])
            for i in range(9):
                n = 128 if i < 8 else 64
                S.op("pe", lambda i=i, n=n: nc.tensor.transpose(psA[0:8, i * 128:i * 128 + n], G[0:n, i, :], identf[0:n, 0:n]),
                     reads=["G", "const"], writes=["RA"])
            S.op("act", lambda: nc.scalar.copy(out=GT[0:8, :], in_=psA[0:8, 0:T]), reads=["RA"], writes=["GT"])
            for e in range(NEXP):
                base = ROWBASE["RB"]
                fns = [lambda e=e, t0=t0, tn=tn: nc.tensor.matmul(psA[:, base + t0: base + t0 + tn], lhsT=esel[:, e * 128:(e + 1) * 128],
                                                                  rhs=GT[0:8, t0:t0 + tn], start=True, stop=True) for (t0, tn) in TT]
                S.op("pe", fns, reads=["GT", "const"], writes=["RB"])
                S.op("act", lambda: nc.scalar.copy(out=gm, in_=prow("RB")), reads=["RB"], writes=["gm"])
                ffn_pass(moe_w1[e], moe_w3[e], moe_w2[e], gm=gm)

        def final_out(rows, norm=True):
            S.barrier()
            rstd = fa(0, T)
            sq = [ba(T, T), ba(T + T // 2, T)]
            for c in range(16 if norm else 0):
                b = sq[c % 2]
                S.op("act", lambda c=c, b=b: nc.scalar.activation(out=b, in_=xT[:, c, :], func=AF.Square),
                     reads=["xT"], writes=[("sq", c % 2)])
                base = ROWBASE["RA"]
                fns = [lambda c=c, b=b, t0=t0, tn=tn: nc.tensor.matmul(
                    psA[:, base + t0: base + t0 + tn], lhsT=onesb[:], rhs=b[:, t0:t0 + tn], start=(c == 0), stop=(c == 15))
                    for (t0, tn) in TT]
                S.op("pe", fns, reads=[("sq", c % 2), "onesb"], writes=["RA"])
            if norm:
                rsqrt_row(rstd, prow("RA"), 1.0 / D, "rstd", "RA")
            for c in range(16 if norm else 0):
                S.op("dve", lambda c=c: nc.vector.scalar_tensor_tensor(
                    out=xT[:, c, :], in0=xT[:, c, :], scalar=col(rows, ("fing",), c), in1=rstd, op0=ALU.mult, op1=ALU.mult),
                    reads=["xT", "rstd", "colT"], writes=["xT"])
            ost = [fa(2 * T, 2048), fa(2 * T + 2048, 2048)]
            for i in range(9):
                n = 128 if i < 8 else 64
                o = ost[i % 2]
                for g in range(4):
                    bank = ROWBASE["RA"] + (g % 2) * 512 if g < 2 else ROWBASE["RB"] + (g % 2) * 512
                    key = "pb%d" % (bank // 512)
                    fns = [lambda j=j, g=g, bank=bank: nc.tensor.transpose(
                        psA[0:n, bank + j * 128: bank + (j + 1) * 128], xT[:, g * 4 + j, i * 128:i * 128 + n], identf[:])
                        for j in range(4)]
                    S.op("pe", fns, reads=["xT", "const"], writes=[key])
                    if g % 2 == 0:
                        S.op("dve", lambda g=g, bank=bank, o=o: nc.vector.tensor_copy(out=o[0:n, g * 512:(g + 1) * 512], in_=psA[0:n, bank:bank + 512]),
                             reads=[key], writes=[("ost", i % 2)])
                    else:
                        S.op("act", lambda g=g, bank=bank, o=o: nc.scalar.copy(out=o[0:n, g * 512:(g + 1) * 512], in_=psA[0:n, bank:bank + 512]),
                             reads=[key], writes=[("ost", i % 2)])
                S.dma("sp", y_out[i * 128:i * 128 + n, :], o[0:n, :], reads=[("ost", i % 2)], writes=[("o_y", i)])

        def emit_all():
            st = [0]

            def cut():
                st[0] += 1
                if STAGE == st[0]:
                    final_out(rows, norm=False)
                    S.finish()
                    return True
                return False

            rows = load_consts()
            load_x()
            if cut():
                return
            for l in range(2):
                rmsnorm_to_hT(("mixg", l), rows)
                dbg_stop("norm0")
                if XCH:
                    pre_pass(l)
                mixer_D(l, rows)
                load_xch(l)
                if cut():
                    return
                mixer_A(l, rows)
                if cut():
                    return
                mixer_B(l, rows)
                if cut():
                    return
                mixer_C(l, rows)
                if cut():
                    return
                S.barrier()
                rmsnorm_to_hT(("ffng", l), rows)
                if l == 0:
                    ffn_pass(dense_w1[0], dense_w3[0], dense_w2[0])
                else:
                    moe(rows)
                if cut():
                    return
            final_out(rows)
            S.finish()

        S.plan = True
        try:
            emit_all()
        except StopEmit:
            pass
        S.plan = False
        S.reset()
        W.reset()
        try:
            emit_all()
        except StopEmit:
            S.finish()
    return nc


def _consts(half):
    c = {}
    c["c_ident"] = np.eye(128, dtype=np.float32)
    pos = np.concatenate([np.arange(NPT, dtype=np.float32) + np.float32(half * NPT),
                          np.tile(np.arange(4, dtype=np.float32) + np.float32(16384.0), NB)]).astype(np.float32)
    inv = (np.float32(10000.0) ** (-np.arange(64, dtype=np.float32) / np.float32(64))).astype(np.float32)
    ang = (pos[None, :] * inv[:, None]).astype(np.float32)
    cs = np.cos(ang).astype(np.float32)
    sn = np.sin(ang).astype(np.float32)
    c["c_rope_c"] = np.concatenate([cs, cs], 0)
    c["c_rope_s"] = np.concatenate([-sn, sn], 0)
    scale = 128.0 ** -0.5
    logg = [math.log1p(-2.0 ** (-5.0 - h)) for h in range(4)]
    j = np.arange(128)[:, None]
    i = np.arange(128)[None, :]
    dm = np.zeros((128, 4, 128), np.float64)
    dms = np.zeros((64, 4, 64), np.float64)
    dqv = np.zeros((128, 4, 128), np.float64)
    dqs = np.zeros((128, 4, 64), np.float64)
    dkv = np.zeros((128, 8), np.float64)
    js = np.arange(64)[:, None]
    is_ = np.arange(64)[None, :]
    for h in range(4):
        dm[:, h, :] = np.where(i >= j, np.exp(logg[h] * np.maximum(i - j, 0)), 0.0) * scale
        same = (js // 4) == (is_ // 4)
        dms[:, h, :] = np.where(same & (is_ >= js), np.exp(logg[h] * np.maximum(is_ - js, 0)), 0.0) * scale
        dqv[:, h, :] = np.exp(logg[h] * (np.arange(128) + 1.0))[None, :]
        dqs[:, h, :] = np.exp(logg[h] * ((np.arange(64) % 4) + 1.0))[None, :]
        dkv[:, h] = np.exp(logg[h] * (127.0 - np.arange(128))) * scale
        dkv[:64, 4 + h] = np.exp(logg[h] * (3.0 - (np.arange(64) % 4))) * scale
    c["c_dmask"] = dm.reshape(128, 512).astype(np.float32)
    c["c_dmask_s"] = dms.reshape(64, 256).astype(np.float32)
    c["c_dq"] = dqv.reshape(128, 512).astype(np.float32)
    c["c_dq_s"] = dqs.reshape(128, 256).astype(np.float32)
    c["c_dkv"] = dkv.astype(np.float32)
    c["c_ind"] = ((np.arange(64)[:, None] // 4) == np.arange(NB)[None, :]).astype(np.float32)
    c["c_triu"] = (j <= i).astype(np.float32)
    c["c_bm"] = (((js // 4) == (is_ // 4)) & (js <= is_)).astype(np.float32)
    c["c_R"] = ((np.arange(64)[None, :] % 4) == np.arange(4)[:, None]).astype(np.float32)
    es = np.zeros((8, 8, 128), np.float32)
    for e in range(8):
        es[e, e, :] = 1.0
    c["c_esel"] = es.reshape(8, 1024)
    c["c_s0mask"] = np.full((128, 1), float(half), np.float32)
    return c


_NC_CACHE = {}


def kernel(**inputs):
    f = lambda k: np.ascontiguousarray(np.asarray(inputs[k], dtype=np.float32))
    x_prompt = f("x_prompt")
    x_sample = f("x_sample")
    shared = {
        "mix_norm_g": f("mix_norm_g"), "w_in": f("w_in"), "ret_norm_g": f("ret_norm_g"), "conv31_w": f("conv31_w"),
        "conv31_b": f("conv31_b"), "conv_ln_g": f("conv_ln_g"), "conv_ln_b": f("conv_ln_b"), "sconv_w": f("sconv_w"),
        "sgu_ln_g": f("sgu_ln_g"), "sgu_ln_b": f("sgu_ln_b"), "sgu_w": f("sgu_w"),
        "sgu_b": f("sgu_b").reshape(2, 512), "w_out": f("w_out"), "ffn_norm_g": f("ffn_norm_g"),
        "dense_w1": f("dense_w1"), "dense_w3": f("dense_w3"), "dense_w2": f("dense_w2"),
        "router_w": f("router_w")[0], "moe_w1": f("moe_w1")[0], "moe_w3": f("moe_w3")[0], "moe_w2": f("moe_w2")[0],
        "final_norm_g": f("final_norm_g"),
    }
    st_ret = f("state_ret")
    st_c31 = f("state_conv31")
    st_c3 = f("state_conv3")
    in_maps = []
    for c in range(NCORES):
        b, half = c // 2, c % 2
        m = dict(shared)
        m["xin"] = np.ascontiguousarray(np.concatenate(
            [x_prompt[b, half * NPT:(half + 1) * NPT], x_sample[c * NB:(c + 1) * NB].reshape(NST, D)], 0))
        m["st_ret"] = np.ascontiguousarray(st_ret[:, c * NB:(c + 1) * NB])
        m["st_c31"] = np.ascontiguousarray(st_c31[:, c * NB:(c + 1) * NB])
        m["st_c3"] = np.ascontiguousarray(st_c3[:, c * NB:(c + 1) * NB])
        m.update(_consts(half))
        in_maps.append(m)
    if "nc" not in _NC_CACHE:
        _NC_CACHE["nc"] = build_program()
    res = run_bass_kernel_spmd(_NC_CACHE["nc"], in_maps, core_ids=list(range(NCORES)))
    R = res.results
    y_prompt = np.zeros((4, 2048, D), np.float32)
    y_sample = np.zeros((128, 4, D), np.float32)
    ret_p = np.zeros((2, 4, 4, 128, 128), np.float32)
    ret_s = np.zeros((2, 128, 4, 128, 128), np.float32)
    c31_p = np.zeros((2, 4, 30, 512), np.float32)
    c31_s = np.zeros((2, 128, 30, 512), np.float32)
    c3_p = np.zeros((2, 4, 2, 512), np.float32)
    c3_s = np.zeros((2, 128, 2, 512), np.float32)
    vs = np.zeros((2, 128, 4, 512), np.float32)
    for c in range(NCORES):
        b, half = c // 2, c % 2
        r = R[c]
        y_prompt[b, half * NPT:(half + 1) * NPT] = r["y_out"][:NPT]
        y_sample[c * NB:(c + 1) * NB] = r["y_out"][NPT:].reshape(NB, 4, D)
        ret_s[:, c * NB:(c + 1) * NB] = r["ret_s"]
        c31_s[:, c * NB:(c + 1) * NB] = r["c31_s"]
        c3_s[:, c * NB:(c + 1) * NB] = r["c3_s"]
        vs[:, c * NB:(c + 1) * NB] = r["vs_out"].reshape(2, NB, 4, 512)
        if half == 1:
            ret_p[:, b] = r["ret_p"]
            c31_p[:, b] = r["c31_p"]
            c3_p[:, b] = r["c3_p"]
    return (y_prompt, y_sample, ret_p, ret_s, c31_p, c31_s, c3_p, c3_s, vs)
```

```python
import math
from contextlib import ExitStack

import numpy as np
import concourse.bass as bass
import concourse.mybir as mybir
from concourse.bass_utils import run_bass_kernel_spmd

F32 = mybir.dt.float32
BF16 = mybir.dt.bfloat16
ALU = mybir.AluOpType
AF = mybir.ActivationFunctionType
AX = mybir.AxisListType

NCORES = 8
D = 2048
DIN = 5632
DFF = 5632
NPT = 1024
NST = 64
T = NPT + NST
TT = [(0, 512), (512, 512), (1024, 64)]
NB = 16
EPS = 1e-6
NBUF = 4
SLOT = 4096
NEXP = 8
GAM = [1.0 - 2.0 ** (-5.0 - h) for h in range(4)]
SAME_SYNC = True
DBG = None
LITE = False
XCH = True


class StopEmit(Exception):
    pass


def dbg_stop(tag):
    if DBG == tag:
        raise StopEmit()


STAGE = 0


class Sync:
    def __init__(self, nc, es):
        self.nc = nc
        self.plan = False
        self.engs = {}
        for name, obj in [("pe", nc.tensor), ("dve", nc.vector), ("act", nc.scalar),
                          ("pool", nc.gpsimd), ("sp", nc.sync)]:
            self.engs[name] = dict(obj=obj, sem=es.enter_context(nc.semaphore("sem_" + name)), cnt=0, waited={})
        self.dpools = {}
        for q, n in [("sp", 8), ("pool", 6), ("act", 4)]:
            self.dpools[q] = dict(i=0, sems=[dict(sem=es.enter_context(nc.semaphore(f"d_{q}{i}")), cnt=0)
                                             for i in range(n)])
        self.lastw = {}
        self.readers = {}
        self.cc = [dict(sem=es.enter_context(nc.semaphore(f"cc{i}")), cnt=0) for i in range(2)]

    def coll(self, i, fn, reads=(), writes=()):
        if self.plan:
            return
        reads, writes = self._x(reads), self._x(writes)
        self._need("pool", self._deps(reads, writes))
        ins = fn()
        self.cc[i]["cnt"] += 1
        ins.then_inc(self.cc[i]["sem"])
        self._mark(reads, writes, (("cc", i), self.cc[i]["cnt"]))

    def reset(self):
        for c in self.cc:
            c["cnt"] = 0
        for e in self.engs.values():
            e["cnt"] = 0
            e["waited"] = {}
        for p in self.dpools.values():
            p["i"] = 0
            for s in p["sems"]:
                s["cnt"] = 0
        self.lastw = {}
        self.readers = {}

    def semh(self, sk):
        if isinstance(sk, str):
            return self.engs[sk]["sem"]
        if sk[0] == "cc":
            return self.cc[sk[1]]["sem"]
        return self.dpools[sk[0]]["sems"][sk[1]]["sem"]

    def _need(self, eng, deps):
        e = self.engs[eng]
        best = {}
        for sk, v in deps:
            if sk == eng and not SAME_SYNC:
                continue
            if v > best.get(sk, 0):
                best[sk] = v
        for sk, v in best.items():
            if e["waited"].get(sk, 0) >= v:
                continue
            e["obj"].wait_ge(self.semh(sk), v)
            e["waited"][sk] = v

    PSUM_KEYS = ("pb0", "pb1", "pb2", "pb3", "pb4", "pb5", "psS", "psB")

    def _deps(self, reads, writes):
        d = []
        for k in reads:
            if k in self.lastw:
                d.append(self.lastw[k])
            if k in self.PSUM_KEYS:
                d.extend(self.readers.get(k, {}).items())
        for k in writes:
            if k in self.lastw:
                d.append(self.lastw[k])
            d.extend(self.readers.get(k, {}).items())
        return d

    def _mark(self, reads, writes, dep):
        for k in reads:
            r = self.readers.setdefault(k, {})
            r[dep[0]] = max(r.get(dep[0], 0), dep[1])
        for k in writes:
            self.lastw[k] = dep
            self.readers[k] = {}

    ALIAS = {"RA": ("pb0", "pb1", "pb2"), "RB": ("pb3", "pb4", "pb5")}
    ALIAS.update({("xT", c): ("xT",) for c in range(16)})
    ALIAS.update({("hT", c): ("hT",) for c in range(16)})

    def _x(self, keys):
        out = []
        for k in keys:
            out.extend(self.ALIAS.get(k, (k,)))
        return out

    def op(self, eng, fns, reads=(), writes=()):
        if self.plan:
            return
        reads, writes = self._x(reads), self._x(writes)
        self._need(eng, self._deps(reads, writes))
        e = self.engs[eng]
        if callable(fns):
            fns = [fns]
        for f in fns[:-1]:
            f()
        ins = fns[-1]()
        e["cnt"] += 1
        ins.then_inc(e["sem"], 1)
        self._mark(reads, writes, (eng, e["cnt"]))

    def dma(self, q, out, in_, reads=(), writes=()):
        if self.plan:
            return
        reads, writes = self._x(reads), self._x(writes)
        p = self.dpools[q]
        idx = p["i"] % len(p["sems"])
        p["i"] += 1
        ds = p["sems"][idx]
        deps = self._deps(reads, writes)
        if ds["cnt"] > 0:
            deps.append(((q, idx), 16 * ds["cnt"]))
        self._need(q, deps)
        ins = self.engs[q]["obj"].dma_start(out=out, in_=in_)
        ds["cnt"] += 1
        ins.then_inc(ds["sem"], 16)
        self._mark(reads, writes, ((q, idx), 16 * ds["cnt"]))

    def barrier(self, full=False):
        if self.plan:
            return
        deps = [(n, e["cnt"]) for n, e in self.engs.items() if e["cnt"] > 0]
        for q, p in self.dpools.items():
            if q == "pool" and not full:
                continue
            for i, s in enumerate(p["sems"]):
                if s["cnt"] > 0:
                    deps.append(((q, i), 16 * s["cnt"]))
        if full:
            for i, c in enumerate(self.cc):
                if c["cnt"] > 0:
                    deps.append((("cc", i), c["cnt"]))
        for n in self.engs:
            if n == "pool" and not full:
                continue
            self._need(n, [d for d in deps if d[0] != n])

    def finish(self):
        self.barrier(full=True)


def build_program():
    nc = bass.Bass("TRN2", target_bir_lowering=False)

    def din(name, shape):
        if LITE and name in ("w_in", "w_out", "dense_w1", "dense_w3", "dense_w2", "moe_w1", "moe_w3", "moe_w2"):
            return nc.dram_tensor(name, [1] * (len(shape) - 2) + list(shape[-2:]), F32, kind="ExternalInput").ap()
        return nc.dram_tensor(name, list(shape), F32, kind="ExternalInput").ap()

    def dout(name, shape):
        return nc.dram_tensor(name, list(shape), F32, kind="ExternalOutput").ap()

    xin = din("xin", [T, D])
    st_ret = din("st_ret", [2, NB, 4, 128, 128])
    st_c31 = din("st_c31", [2, NB, 30, 512])
    st_c3 = din("st_c3", [2, NB, 2, 512])
    mix_norm_g = din("mix_norm_g", [2, D])
    w_in = din("w_in", [2, D, DIN])
    ret_norm_g = din("ret_norm_g", [2, 512])
    conv31_w = din("conv31_w", [2, 31, 512])
    conv31_b = din("conv31_b", [2, 512])
    conv_ln_g = din("conv_ln_g", [2, 512])
    conv_ln_b = din("conv_ln_b", [2, 512])
    sconv_w = din("sconv_w", [2, 3, 512])
    sgu_ln_g = din("sgu_ln_g", [2, 512])
    sgu_ln_b = din("sgu_ln_b", [2, 512])
    sgu_w = din("sgu_w", [2, 4, 128, 128])
    sgu_b = din("sgu_b", [2, 512])
    w_out = din("w_out", [2, D, D])
    ffn_norm_g = din("ffn_norm_g", [2, D])
    dense_w1 = din("dense_w1", [1, D, DFF])
    dense_w3 = din("dense_w3", [1, D, DFF])
    dense_w2 = din("dense_w2", [1, DFF, D])
    router_w = din("router_w", [D, NEXP])
    moe_w1 = din("moe_w1", [NEXP, D, DFF])
    moe_w3 = din("moe_w3", [NEXP, D, DFF])
    moe_w2 = din("moe_w2", [NEXP, DFF, D])
    final_norm_g = din("final_norm_g", [D])
    c_ident = din("c_ident", [128, 128])
    c_rope_c = din("c_rope_c", [128, T])
    c_rope_s = din("c_rope_s", [128, T])
    c_dmask = din("c_dmask", [128, 512])
    c_dmask_s = din("c_dmask_s", [64, 256])
    c_dq = din("c_dq", [128, 512])
    c_dq_s = din("c_dq_s", [128, 256])
    c_dkv = din("c_dkv", [128, 8])
    c_ind = din("c_ind", [64, NB])
    c_triu = din("c_triu", [128, 128])
    c_bm = din("c_bm", [64, 64])
    c_R = din("c_R", [4, 64])
    c_esel = din("c_esel", [8, 8 * 128])
    c_s0mask = din("c_s0mask", [128, 1])

    y_out = dout("y_out", [T, D])
    ret_p = dout("ret_p", [2, 4, 128, 128])
    ret_s = dout("ret_s", [2, NB, 4, 128, 128])
    c31_p = dout("c31_p", [2, 30, 512])
    c31_s = dout("c31_s", [2, NB, 30, 512])
    c3_p = dout("c3_p", [2, 2, 512])
    c3_s = dout("c3_s", [2, NB, 2, 512])
    vs_out = dout("vs_out", [2, NST, 512])

    xi = [nc.dram_tensor(f"xch_in{l}", [1024, 128], F32).ap() for l in range(2)]
    xg = [nc.dram_tensor(f"xch_all{l}", [2048, 128], F32).ap() for l in range(2)]

    es = ExitStack()
    with es:
        def sb(name, shape, dt=F32):
            return es.enter_context(nc.sbuf_tensor(name, list(shape), dt))

        xT = sb("xT", [128, 16, T])
        hT = sb("hT", [128, 16, T], BF16)
        mixT = sb("mixT", [128, 4, T], BF16)
        wsl = sb("wsl", [128, NBUF, SLOT], BF16)
        identf = sb("identf", [128, 128])
        identb = sb("identb", [128, 128], BF16)
        onesb = sb("onesb", [128, 128], BF16)
        rope_c = sb("rope_c", [128, T])
        rope_s = sb("rope_s", [128, T])
        dmask = sb("dmask", [128, 512])
        dmask_s = sb("dmask_s", [64, 256])
        dq = sb("dq", [128, 512])
        dq_s = sb("dq_s", [128, 256])
        dkv = sb("dkv", [128, 8])
        ind = sb("ind", [64, NB])
        triu = sb("triu", [128, 128])
        bm = sb("bm", [64, 64])
        Rm = sb("Rm", [4, 64])
        esel = sb("esel", [8, 8 * 128])
        s0mask = sb("s0mask", [128, 1])
        colT = sb("colT", [128, 512])
        epsc = sb("epsc", [128, 1])
        hx31 = sb("hx31", [128, 4, 30])
        hx3 = sb("hx3", [128, 4, 2])
        S0all = sb("S0all", [128, 4, 128])
        ARENA = 9728
        arena = sb("arena", [128, ARENA])
        arena_b = arena[:].bitcast(BF16)

        psA = es.enter_context(nc.psum_tensor("psA", [128, 3072], F32))
        psS = es.enter_context(nc.psum_tensor("psS", [128, 512], F32))
        psB = es.enter_context(nc.psum_tensor("psB", [128, 1024], BF16))

        S = Sync(nc, es)
        ROWBASE = {"RA": 0, "RB": 1536}

        def fa(off, n):
            assert off + n <= ARENA
            return arena[:, off:off + n]

        def ba(off_words, n):
            assert off_words * 2 + n <= 2 * ARENA
            return arena_b[:, off_words * 2: off_words * 2 + n]

        class WStream:
            def __init__(self):
                self.blocks = []
                self.i = 0
                self.issued = 0

            def reset(self):
                self.i = 0
                self.issued = 0

            def _issue(self, j):
                slot = j % NBUF
                for (a, b, c0, n, src) in self.blocks[j]:
                    dst = wsl[:, slot, 0:a * b].rearrange("p (a b) -> p a b", a=a)[:, :, c0:c0 + n]
                    S.dma("pool", dst, src, reads=[], writes=[("w", slot)])

            def get(self, parts):
                if S.plan:
                    self.blocks.append(parts)
                    self.i += 1
                    return (self.i - 1) % NBUF
                j = self.i
                while self.issued < min(len(self.blocks), j + NBUF - 1):
                    self._issue(self.issued)
                    self.issued += 1
                self.i += 1
                return j % NBUF

        W = WStream()

        def wview(slot, off, a, b):
            return wsl[:, slot, off:off + a * b].rearrange("p (a b) -> p a b", a=a)

        def win_cols(l, c0, n):
            return w_in[l, :, c0:c0 + n].rearrange("(kc p) n -> p kc n", p=128)

        def proj_fm(slot, coff, row, ncols=128, kdim=16, act=None):
            base = ROWBASE[row]
            src = act if act is not None else hT
            wv = wview(slot, 0, 16, 256) if kdim == 16 else None
            fns = []
            for kc in range(kdim):
                for (t0, tn) in TT:
                    fns.append(lambda kc=kc, t0=t0, tn=tn: nc.tensor.matmul(
                        psA[0:ncols, base + t0: base + t0 + tn], lhsT=wv[:, kc, coff:coff + ncols],
                        rhs=src[:, kc, t0:t0 + tn], start=(kc == 0), stop=(kc == kdim - 1)))
            S.op("pe", fns, reads=[("w", slot), "hT"], writes=[row])

        def prow(row, n=T, p=128):
            base = ROWBASE[row]
            return psA[0:p, base:base + n]

        def stat_bcast(src_bf_rows, keys, row):
            base = ROWBASE[row]
            fns = []
            n = len(src_bf_rows)
            for k, r in enumerate(src_bf_rows):
                for (t0, tn) in TT:
                    fns.append(lambda k=k, r=r, t0=t0, tn=tn: nc.tensor.matmul(
                        psA[:, base + t0: base + t0 + tn], lhsT=onesb[:], rhs=r[:, t0:t0 + tn],
                        start=(k == 0), stop=(k == n - 1)))
            S.op("pe", fns, reads=list(keys), writes=[row])

        def rsqrt_row(dst, src, scale, key_dst, key_src):
            S.op("act", lambda: nc.scalar.activation(out=dst, in_=src, func=AF.Sqrt, bias=epsc[:], scale=scale),
                 reads=[key_src], writes=[key_dst])
            S.op("dve", lambda: nc.vector.reciprocal(out=dst, in_=dst), reads=[key_dst], writes=[key_dst])

        def load_consts():
            for dst, src in [(identf, c_ident), (rope_c, c_rope_c), (rope_s, c_rope_s), (dmask, c_dmask),
                             (dmask_s, c_dmask_s), (dq, c_dq), (dq_s, c_dq_s), (dkv, c_dkv), (ind, c_ind),
                             (triu, c_triu), (bm, c_bm), (Rm, c_R), (esel, c_esel), (s0mask, c_s0mask)]:
                S.dma("sp", dst[:], src, writes=["const"])
            S.op("dve", lambda: nc.vector.tensor_copy(out=identb[:], in_=identf[:]), reads=["const"], writes=["identb"])
            S.op("dve", lambda: nc.vector.memset(onesb[:], 1.0), writes=["onesb"])
            S.op("dve", lambda: nc.vector.memset(epsc[:], EPS), writes=["epsc"])
            stg = fa(0, 512).rearrange("p (a b) -> p a b", a=4)
            S.op("dve", lambda: nc.vector.memset(fa(0, 512), 0.0), writes=["stg"])
            rows = {}

            def put(tile, r0, name, src_rows):
                n = src_rows.shape[0]
                S.dma("sp", stg[r0:r0 + n, tile, :], src_rows, reads=[], writes=["stg"])
                rows[name] = tile * 128 + r0
                return r0 + n

            r = 0
            for l in range(2):
                tile = l
                r = 0
                r = put(tile, r, ("mixg", l), mix_norm_g[l].rearrange("(c p) -> c p", p=128))
                r = put(tile, r, ("ffng", l), ffn_norm_g[l].rearrange("(c p) -> c p", p=128))
                r = put(tile, r, ("retg", l), ret_norm_g[l].rearrange("(c p) -> c p", p=128))
                r = put(tile, r, ("c31b", l), conv31_b[l].rearrange("(c p) -> c p", p=128))
                r = put(tile, r, ("clng", l), conv_ln_g[l].rearrange("(c p) -> c p", p=128))
                r = put(tile, r, ("clnb", l), conv_ln_b[l].rearrange("(c p) -> c p", p=128))
                r = put(tile, r, ("scw", l), sconv_w[l].rearrange("j (c p) -> (j c) p", p=128))
                if l == 0:
                    r = put(tile, r, ("fing",), final_norm_g.rearrange("(c p) -> c p", p=128))
            for l in range(2):
                put(2 + l, 0, ("c31w", l), conv31_w[l].rearrange("j (c p) -> (j c) p", p=128))
            fns = [lambda t=t: nc.tensor.transpose(psS[:, t * 128:(t + 1) * 128], stg[:, t, :], identf[:])
                   for t in range(4)]
            S.op("pe", fns, reads=["stg", "const"], writes=["psS"])
            S.op("dve", lambda: nc.vector.tensor_copy(out=colT[:], in_=psS[:]), reads=["psS"], writes=["colT"])
            return rows

        def col(rows, name, i=0):
            j = rows[name] + i
            return colT[:, j:j + 1]

        def load_x():
            for i in range(9):
                n = 128 if i < 8 else 64
                stage = fa(512 + (i % 2) * 2048, 2048)
                S.dma("sp", stage[0:n, :], xin[i * 128:i * 128 + n, :], writes=[("xst", i % 2)])
                for g in range(4):
                    bank = ROWBASE["RA"] + (g % 2) * 512
                    key = "pb%d" % (g % 2)
                    fns = [lambda j=j, g=g: nc.tensor.transpose(
                        psA[:, bank + j * 128: bank + j * 128 + n], stage[0:n, (g * 4 + j) * 128:(g * 4 + j + 1) * 128],
                        identf[0:n, 0:n]) for j in range(4)]
                    S.op("pe", fns, reads=[("xst", i % 2), "const"], writes=[key])
                    src = psA[:, bank:bank + 512].rearrange("p (a b) -> p a b", a=4)[:, :, 0:n]
                    S.op("dve" if g % 2 == 0 else "act",
                         (lambda src=src, g=g: nc.vector.tensor_copy(out=xT[:, g * 4:(g + 1) * 4, i * 128:i * 128 + n], in_=src))
                         if g % 2 == 0 else
                         (lambda src=src, g=g: nc.scalar.copy(out=xT[:, g * 4:(g + 1) * 4, i * 128:i * 128 + n], in_=src)),
                         reads=[key], writes=["xT"])

        def rmsnorm_to_hT(gname, rows, gi=0):
            sq = [ba(0, T), ba(T // 2, T)]
            rstd = fa(T, T)
            for c in range(16):
                b = sq[c % 2]
                S.op("act", lambda c=c, b=b: nc.scalar.activation(out=b, in_=xT[:, c, :], func=AF.Square),
                     reads=[("xT", c)], writes=[("sq", c % 2)])
                base = ROWBASE["RA"]
                fns = [lambda c=c, b=b, t0=t0, tn=tn: nc.tensor.matmul(
                    psA[:, base + t0: base + t0 + tn], lhsT=onesb[:], rhs=b[:, t0:t0 + tn],
                    start=(c == 0), stop=(c == 15)) for (t0, tn) in TT]
                S.op("pe", fns, reads=[("sq", c % 2), "onesb"], writes=["RA"])
            dbg_stop("n_a")
            rsqrt_row(rstd, prow("RA"), 1.0 / D, "rstd", "RA")
            dbg_stop("n_b")
            for c in range(16):
                S.op("dve", lambda c=c: nc.vector.scalar_tensor_tensor(
                    out=hT[:, c, :], in0=xT[:, c, :], scalar=col(rows, gname, gi + c), in1=rstd,
                    op0=ALU.mult, op1=ALU.mult), reads=[("xT", c), "rstd", "colT"], writes=[("hT", c)])

        def accum_out(slots, src_rows, src_key, kdim_per_slot=2):
            nk = len(slots) * kdim_per_slot
            for dm in range(16):
                row = "RA" if dm % 2 == 0 else "RB"
                base = ROWBASE[row]
                fns = []
                for k in range(nk):
                    wv = wview(slots[k // kdim_per_slot], 0, kdim_per_slot, 2048)
                    for (t0, tn) in TT:
                        fns.append(lambda k=k, wv=wv, t0=t0, tn=tn, dm=dm, base=base: nc.tensor.matmul(
                            psA[:, base + t0: base + t0 + tn],
                            lhsT=wv[:, k % kdim_per_slot, dm * 128:(dm + 1) * 128],
                            rhs=src_rows[k][:, t0:t0 + tn], start=(k == 0), stop=(k == nk - 1)))
                wkeys = [("w", s) for s in slots]
                if isinstance(src_key, list) and dm == 0:
                    nsplit = 3 * (nk - 1)
                    S.op("pe", fns[:nsplit], reads=wkeys + src_key[:nk - 1], writes=[row])
                    S.op("pe", fns[nsplit:], reads=wkeys + [src_key[nk - 1]], writes=[row])
                else:
                    S.op("pe", fns, reads=wkeys + (src_key if isinstance(src_key, list) else [src_key]), writes=[row])
                S.op("dve", lambda dm=dm, row=row: nc.vector.tensor_tensor(
                    out=xT[:, dm, :], in0=prow(row), in1=xT[:, dm, :], op=ALU.add),
                    reads=[row, ("xT", dm)], writes=[("xT", dm)])

        def wout_slots(l, m):
            sl = []
            for s in range(2):
                r0 = m * 512 + s * 256
                src = w_out[l, r0:r0 + 256, :].rearrange("(kc p) n -> p kc n", p=128)
                sl.append(W.get([(2, 2048, 0, 2048, src)]))
            return sl

        def rope_proj(slot, dst, dkey, f1, oT):
            wall = wview(slot, 0, 16, 256)
            wx = wall[:, :, 0:128]
            wlo = wall[:, :, 128:192]
            whi = wall[:, :, 192:256]
            for row, parts in (("RA", [(wx, 0, 128)]), ("RB", [(wlo, 0, 64), (whi, 64, 64)])):
                base = ROWBASE[row]
                fns = []
                for (wv, m0, mn) in parts:
                    for kc in range(16):
                        for (t0, tn) in TT:
                            fns.append(lambda wv=wv, m0=m0, mn=mn, kc=kc, t0=t0, tn=tn, base=base: nc.tensor.matmul(
                                psA[m0:m0 + mn, base + t0: base + t0 + tn], lhsT=wv[:, kc, :],
                                rhs=hT[:, kc, t0:t0 + tn], start=(kc == 0), stop=(kc == 15)))
                S.op("pe", fns, reads=[("w", slot), "hT"], writes=[row])
            S.op("dve", lambda: nc.vector.tensor_tensor(out=f1, in0=prow("RA"), in1=rope_c[:], op=ALU.mult),
                 reads=["RA", "const"], writes=["f1"])
            S.op("dve", lambda: nc.vector.tensor_tensor(out=oT, in0=prow("RB"), in1=rope_s[:], op=ALU.mult),
                 reads=["RB", "const"], writes=["oTtmp"])
            S.op("dve", lambda: nc.vector.tensor_tensor(out=dst, in0=f1, in1=oT, op=ALU.add),
                 reads=["f1", "oTtmp"], writes=[dkey])

        def pre_pass(l):
            f1 = fa(0, T)
            oT = fa(T, T)
            kT = ba(2 * T, T)
            ktok = ba(2 * T + T // 2, 1152).rearrange("p (a b) -> p a b", a=9)
            vtt = ba(2 * T + T // 2 + 576, 1152).rearrange("p (a b) -> p a b", a=9)
            o_sm = 2 * T + T // 2 + 1152
            Sx = fa(o_sm, 128)
            gl = fa(o_sm + 128, 128)
            gd = fa(o_sm + 256, 128)
            t1 = fa(o_sm + 384, 128)
            t2 = fa(o_sm + 512, 128)
            S.barrier()
            for h in range(4):
                kc_ = 1 * 512 + h * 128
                vc = 2 * 512 + h * 128
                s_k = W.get([(16, 256, 0, 128, win_cols(l, kc_, 128)),
                             (16, 256, 128, 64, win_cols(l, kc_ + 64, 64)),
                             (16, 256, 192, 64, win_cols(l, kc_, 64))])
                rope_proj(s_k, kT, "kT", f1, oT)
                s_v = W.get([(16, 256, 0, 128, win_cols(l, vc, 128))])
                wv_v = wview(s_v, 0, 16, 256)
                for grp in range(2):
                    base = ROWBASE["RB"]
                    fns = []
                    for j in range(4):
                        i = grp * 4 + j
                        for kc in range(16):
                            fns.append(lambda j=j, i=i, kc=kc: nc.tensor.matmul(
                                psA[:, base + j * 128: base + (j + 1) * 128], lhsT=hT[:, kc, i * 128:(i + 1) * 128],
                                rhs=wv_v[:, kc, 0:128], start=(kc == 0), stop=(kc == 15)))
                    S.op("pe", fns, reads=[("w", s_v), "hT"], writes=["RB"])
                    for j in range(4):
                        i = grp * 4 + j
                        S.op("dve", lambda j=j, i=i: nc.vector.tensor_scalar(
                            out=vtt[:, i, :], in0=psA[:, base + j * 128: base + (j + 1) * 128], scalar1=dkv[:, h:h + 1],
                            scalar2=float(GAM[h] ** (128 * (7 - i))), op0=ALU.mult, op1=ALU.mult),
                            reads=["RB", "const"], writes=["vtt"])
                    fns = [lambda j=j, grp=grp: nc.tensor.transpose(
                        psB[:, j * 128:(j + 1) * 128], kT[:, (grp * 4 + j) * 128:(grp * 4 + j + 1) * 128], identb[:])
                        for j in range(4)]
                    S.op("pe", fns, reads=["kT", "identb"], writes=["psB"])
                    S.op("act", lambda grp=grp: nc.scalar.copy(
                        out=ktok[:, grp * 4:grp * 4 + 4, :], in_=psB[:, 0:512].rearrange("p (a b) -> p a b", a=4)),
                        reads=["psB"], writes=["ktok"])
                fns = [lambda c=c: nc.tensor.matmul(psS[:, 0:128], lhsT=ktok[:, c, :], rhs=vtt[:, c, :],
                                                    start=(c == 0), stop=(c == 7)) for c in range(8)]
                S.op("pe", fns, reads=["ktok", "vtt"], writes=["psS"])
                S.op("act", lambda: nc.scalar.copy(out=Sx, in_=psS[:, 0:128]), reads=["psS"], writes=["Sx"])
                S.dma("sp", xi[l][h * 128:(h + 1) * 128, :], Sx, reads=["Sx"], writes=[("xi", l)])
            lc = slice(NPT - 128, NPT)
            for ct in range(4):
                s_b = W.get([(16, 256, 0, 128, win_cols(l, 2048 + ct * 128, 128)),
                             (16, 256, 128, 128, win_cols(l, 2560 + ct * 128, 128))])
                s_c = W.get([(16, 256, 0, 128, win_cols(l, 3584 + ct * 128, 128)),
                             (16, 256, 128, 128, win_cols(l, 4096 + ct * 128, 128))])
                fns = []
                for q_, sl_ in enumerate((s_b, s_b, s_c, s_c)):
                    wv = wview(sl_, 0, 16, 256)
                    for kc in range(16):
                        fns.append(lambda q_=q_, wv=wv, kc=kc: nc.tensor.matmul(
                            psS[:, q_ * 128:(q_ + 1) * 128], lhsT=wv[:, kc, (q_ % 2) * 128:(q_ % 2) * 128 + 128],
                            rhs=hT[:, kc, lc], start=(kc == 0), stop=(kc == 15)))
                S.op("pe", fns, reads=[("w", s_b), ("w", s_c), "hT"], writes=["psS"])
                S.op("act", lambda: nc.scalar.activation(out=t1, in_=psS[:, 128:256], func=AF.Sigmoid), reads=["psS"], writes=["t1"])
                S.op("act", lambda: nc.scalar.copy(out=t2, in_=psS[:, 256:384]), reads=["psS"], writes=["t2"])
                S.op("dve", lambda: nc.vector.tensor_tensor(out=gl, in0=psS[:, 0:128], in1=t1, op=ALU.mult),
                     reads=["psS", "t1"], writes=["gl"])
                S.op("dve", lambda: nc.vector.tensor_tensor(out=gd, in0=psS[:, 384:512], in1=t2, op=ALU.mult),
                     reads=["psS", "t2"], writes=["gd"])
                S.dma("sp", xi[l][512 + ct * 128:512 + (ct + 1) * 128, 0:30], gl[:, 98:128], reads=["gl"], writes=[("xi", l)])
                S.dma("sp", xi[l][512 + ct * 128:512 + (ct + 1) * 128, 32:34], gd[:, 126:128], reads=["gd"], writes=[("xi", l)])
            S.coll(l, lambda: nc.gpsimd.collective_compute(
                "AllGather", ALU.bypass, replica_groups=[[0, 1], [2, 3], [4, 5], [6, 7]], ins=[xi[l]], outs=[xg[l]]),
                reads=[("xi", l)], writes=[("xg", l)])

        def load_xch(l):
            if XCH:
                S.dma("sp", S0all[:], xg[l][0:512, :].rearrange("(h p) e -> p h e", p=128), reads=[("xg", l)], writes=["S0all"])
                S.dma("sp", hx31[:], xg[l][512:1024, 0:30].rearrange("(c p) r -> p c r", p=128), reads=[("xg", l)], writes=["hx31"])
                S.dma("sp", hx3[:], xg[l][512:1024, 32:34].rearrange("(c p) r -> p c r", p=128), reads=[("xg", l)], writes=["hx3"])
                for t_, k_ in ((S0all, "S0all"), (hx31, "hx31"), (hx3, "hx3")):
                    S.op("dve", lambda t_=t_: nc.vector.tensor_scalar(out=t_[:], in0=t_[:], scalar1=s0mask[:, 0:1], scalar2=None, op0=ALU.mult),
                         reads=[k_, "const"], writes=[k_])
            else:
                for t_, k_ in ((S0all, "S0all"), (hx31, "hx31"), (hx3, "hx3")):
                    S.op("dve", lambda t_=t_: nc.vector.memset(t_[:], 0.0), writes=[k_])

        def mixer_A(l, rows):
            o_f1 = 0
            o_oT = T
            o_q = 2 * T
            o_k = o_q + T // 2
            o_g = o_k + T // 2
            o_kt = o_g + T // 2
            o_v = o_kt + 576
            o_vt = o_v + 576
            o_sm = o_vt + 576
            f1 = fa(o_f1, T)
            oT = fa(o_oT, T)
            qT = ba(o_q, T)
            kT = ba(o_k, T)
            gS = ba(o_g, T)
            ktok = ba(o_kt, 1152).rearrange("p (a b) -> p a b", a=9)
            vtk = ba(o_v, 1152).rearrange("p (a b) -> p a b", a=9)
            vtt = ba(o_vt, 1152).rearrange("p (a b) -> p a b", a=9)
            PT = ba(o_sm, 128)
            Sb = ba(o_sm + 64, 128)
            Sf = fa(o_sm + 128, 128)
            crt = fa(o_sm + 256, 128)
            Ss16 = fa(o_sm + 384, 2048).rearrange("p (a b) -> p a b", a=16)
            Ssb = ba(o_sm + 2432, 512).rearrange("p (a b) -> p a b", a=4)
            Sso2 = [fa(o_sm + 2688 + k * 512, 512).rearrange("p (a b) -> p a b", a=4) for k in range(2)]
            vm = ba(o_sm + 3712, 512).rearrange("p (a b) -> p a b", a=4)
            assert o_sm + 3968 <= ARENA
            S.barrier()
            for h in range(4):
                qc = 0 * 512 + h * 128
                kc_ = 1 * 512 + h * 128
                vc = 2 * 512 + h * 128
                gc = 3 * 512 + h * 128
                for bg in range(4):
                    S.dma("sp", Ss16[:, bg * 4:(bg + 1) * 4, :], st_ret[l, bg * 4:(bg + 1) * 4, h].rearrange("b d e -> d b e"),
                          writes=[("Ss", bg)])

                def proj_qk(slot, dst, dkey):
                    wall = wview(slot, 0, 16, 256)
                    wx = wall[:, :, 0:128]
                    wlo = wall[:, :, 128:192]
                    whi = wall[:, :, 192:256]
                    for row, parts in (("RA", [(wx, 0, 128)]), ("RB", [(wlo, 0, 64), (whi, 64, 64)])):
                        base = ROWBASE[row]
                        fns = []
                        for (wv, m0, mn) in parts:
                            for kc in range(16):
                                for (t0, tn) in TT:
                                    fns.append(lambda wv=wv, m0=m0, mn=mn, kc=kc, t0=t0, tn=tn, base=base: nc.tensor.matmul(
                                        psA[m0:m0 + mn, base + t0: base + t0 + tn], lhsT=wv[:, kc, :],
                                        rhs=hT[:, kc, t0:t0 + tn], start=(kc == 0), stop=(kc == 15)))
                        S.op("pe", fns, reads=[("w", slot), "hT"], writes=[row])
                    S.op("dve", lambda: nc.vector.tensor_tensor(out=f1, in0=prow("RA"), in1=rope_c[:], op=ALU.mult),
                         reads=["RA", "const"], writes=["f1"])
                    S.op("dve", lambda: nc.vector.tensor_tensor(out=oT, in0=prow("RB"), in1=rope_s[:], op=ALU.mult),
                         reads=["RB", "const"], writes=["oTtmp"])
                    S.op("dve", lambda: nc.vector.tensor_tensor(out=dst, in0=f1, in1=oT, op=ALU.add),
                         reads=["f1", "oTtmp"], writes=[dkey])

                s_q = W.get([(16, 256, 0, 128, win_cols(l, qc, 128)),
                             (16, 256, 128, 64, win_cols(l, qc + 64, 64)),
                             (16, 256, 192, 64, win_cols(l, qc, 64))])
                proj_qk(s_q, qT, "qT")
                s_k = W.get([(16, 256, 0, 128, win_cols(l, kc_, 128)),
                             (16, 256, 128, 64, win_cols(l, kc_ + 64, 64)),
                             (16, 256, 192, 64, win_cols(l, kc_, 64))])
                proj_qk(s_k, kT, "kT")
                s_vg = W.get([(16, 256, 0, 128, win_cols(l, vc, 128)), (16, 256, 128, 128, win_cols(l, gc, 128))])
                wv_vg = wview(s_vg, 0, 16, 256)
                base = ROWBASE["RA"]
                fns = []
                for kc in range(16):
                    for (t0, tn) in TT:
                        fns.append(lambda kc=kc, t0=t0, tn=tn: nc.tensor.matmul(
                            psA[:, base + t0: base + t0 + tn], lhsT=wv_vg[:, kc, 128:256],
                            rhs=hT[:, kc, t0:t0 + tn], start=(kc == 0), stop=(kc == 15)))
                S.op("pe", fns, reads=[("w", s_vg), "hT"], writes=["RA"])
                S.op("act", lambda: nc.scalar.activation(out=gS, in_=prow("RA"), func=AF.Silu),
                     reads=["RA"], writes=["gS"])
                for grp in range(3):
                    tiles = [i for i in range(grp * 4, min(9, grp * 4 + 4))]
                    base = ROWBASE["RB"]
                    fns = []
                    for j, i in enumerate(tiles):
                        n = 128 if i < 8 else 64
                        for kc in range(16):
                            fns.append(lambda j=j, i=i, n=n, kc=kc: nc.tensor.matmul(
                                psA[0:n, base + j * 128: base + (j + 1) * 128], lhsT=hT[:, kc, i * 128:i * 128 + n],
                                rhs=wv_vg[:, kc, 0:128], start=(kc == 0), stop=(kc == 15)))
                    S.op("pe", fns, reads=[("w", s_vg), "hT"], writes=["RB"])
                    nt = len(tiles)
                    n = 128 if grp < 2 else 64
                    src = psA[0:n, base:base + nt * 128].rearrange("p (a b) -> p a b", a=nt)
                    S.op("act", lambda src=src, n=n, grp=grp, nt=nt: nc.scalar.copy(
                        out=vtk[0:n, grp * 4:grp * 4 + nt, :], in_=src), reads=["RB"], writes=["vtk"])
                    dcol = dkv[0:n, h:h + 1] if grp < 2 else dkv[0:n, 4 + h:5 + h]
                    S.op("dve", lambda src=src, n=n, grp=grp, nt=nt, dcol=dcol: nc.vector.tensor_scalar(
                        out=vtt[0:n, grp * 4:grp * 4 + nt, :], in0=src, scalar1=dcol, scalar2=None, op0=ALU.mult),
                        reads=["RB", "const"], writes=["vtt"])
                for grp in range(3):
                    tiles = [i for i in range(grp * 4, min(9, grp * 4 + 4))]
                    n = 128 if grp < 2 else 64
                    fns = [lambda j=j, i=i, n=n: nc.tensor.transpose(
                        psB[0:n, j * 128:(j + 1) * 128], kT[:, i * 128:i * 128 + n], identb[:])
                        for j, i in enumerate(tiles)]
                    S.op("pe", fns, reads=["kT", "identb"], writes=["psB"])
                    nt = len(tiles)
                    src = psB[0:n, 0:nt * 128].rearrange("p (a b) -> p a b", a=nt)
                    S.op("act", lambda src=src, n=n, grp=grp, nt=nt: nc.scalar.copy(
                        out=ktok[0:n, grp * 4:grp * 4 + nt, :], in_=src), reads=["psB"], writes=["ktok"])
                dbg_stop("A_proj")
                S.op("dve", lambda: nc.vector.tensor_copy(out=Sf, in_=S0all[:, h, :]), reads=["S0all"], writes=["Sf"])
                S.op("act", lambda: nc.scalar.copy(out=Sb, in_=Sf), reads=["Sf"], writes=["Sb"])
                gC = GAM[h] ** 128
                for c in range(8):
                    cs = slice(c * 128, (c + 1) * 128)
                    S.op("pe", lambda cs=cs: nc.tensor.matmul(psS[:, 0:128], lhsT=kT[:, cs], rhs=qT[:, cs],
                                                              start=True, stop=True),
                         reads=["kT", "qT"], writes=["psS"])
                    S.op("dve", lambda: nc.vector.tensor_tensor(out=PT, in0=psS[:, 0:128], in1=dmask[:, h * 128:(h + 1) * 128],
                                                                op=ALU.mult), reads=["psS", "const"], writes=["PT"])
                    S.op("pe", lambda c=c: nc.tensor.matmul(psA[:, 1536:1664], lhsT=vtk[:, c, :], rhs=PT, start=True, stop=True),
                         reads=["vtk", "PT"], writes=["pb3"])
                    S.op("pe", lambda cs=cs: nc.tensor.matmul(psA[:, 2048:2176], lhsT=Sb, rhs=qT[:, cs], start=True, stop=True),
                         reads=["Sb", "qT"], writes=["pb4"])
                    S.op("pe", lambda c=c: nc.tensor.matmul(psA[:, 2560:2688], lhsT=ktok[:, c, :], rhs=vtt[:, c, :],
                                                            start=True, stop=True),
                         reads=["ktok", "vtt"], writes=["pb5"])
                    S.op("dve", lambda: nc.vector.tensor_tensor(out=crt, in0=psA[:, 2048:2176], in1=dq[:, h * 128:(h + 1) * 128],
                                                                op=ALU.mult), reads=["pb4", "const"], writes=["crt"])
                    S.op("dve", lambda cs=cs: nc.vector.tensor_tensor(out=oT[:, cs], in0=psA[:, 1536:1664], in1=crt, op=ALU.add),
                         reads=["pb3", "crt", "oTtmp"], writes=["oT"])
                    S.op("dve", lambda: nc.vector.scalar_tensor_tensor(out=Sf, in0=Sf, scalar=float(gC), in1=psA[:, 2560:2688],
                                                                       op0=ALU.mult, op1=ALU.add),
                         reads=["pb5", "Sf"], writes=["Sf"])
                    S.op("act", lambda: nc.scalar.copy(out=Sb, in_=Sf), reads=["Sf"], writes=["Sb"])
                S.dma("sp", ret_p[l, h], Sf, reads=["Sf"], writes=[("o_retp", l, h)])
                sc = slice(NPT, T)
                S.op("pe", lambda: nc.tensor.matmul(psS[0:64, 0:64], lhsT=kT[:, sc], rhs=qT[:, sc], start=True, stop=True),
                     reads=["kT", "qT"], writes=["psS"])
                S.op("dve", lambda: nc.vector.tensor_tensor(out=PT[0:64, 0:64], in0=psS[0:64, 0:64],
                                                            in1=dmask_s[:, h * 64:(h + 1) * 64], op=ALU.mult),
                     reads=["psS", "const"], writes=["PT"])
                S.op("pe", lambda: nc.tensor.matmul(psA[:, 1536:1600], lhsT=vtk[0:64, 8, :], rhs=PT[0:64, 0:64],
                                                    start=True, stop=True), reads=["vtk", "PT"], writes=["pb3"])
                g4 = GAM[h] ** 4
                for bg in range(4):
                    Ss = Ss16[:, bg * 4:(bg + 1) * 4, :]
                    Sso = Sso2[bg % 2]
                    S.op("act", lambda Ss=Ss: nc.scalar.copy(out=Ssb[:], in_=Ss), reads=[("Ss", bg)], writes=["Ssb"])
                    fns = [lambda j=j, bg=bg: nc.tensor.matmul(
                        psA[:, 2048 + (bg * 4 + j) * 4: 2048 + (bg * 4 + j) * 4 + 4], lhsT=Ssb[:, j, :],
                        rhs=qT[:, NPT + (bg * 4 + j) * 4: NPT + (bg * 4 + j) * 4 + 4], start=True, stop=True)
                        for j in range(4)]
                    S.op("pe", fns, reads=["Ssb", "qT"], writes=["pb4"])
                    for j in range(4):
                        b = bg * 4 + j
                        S.op("dve", lambda j=j, b=b: nc.vector.tensor_scalar(
                            out=vm[0:64, j, :], in0=vtt[0:64, 8, :], scalar1=ind[:, b:b + 1], scalar2=None, op0=ALU.mult),
                            reads=["vtt", "const"], writes=["vm"])
                    base = ROWBASE["RA"]
                    fns = [lambda j=j: nc.tensor.matmul(psA[:, base + j * 128: base + (j + 1) * 128],
                                                        lhsT=ktok[0:64, 8, :], rhs=vm[0:64, j, :], start=True, stop=True)
                           for j in range(4)]
                    S.op("pe", fns, reads=["ktok", "vm"], writes=["RA"])
                    S.op("dve", lambda Ss=Ss, Sso=Sso: nc.vector.scalar_tensor_tensor(
                        out=Sso[:], in0=Ss, scalar=float(g4),
                        in1=psA[:, base:base + 512].rearrange("p (a b) -> p a b", a=4), op0=ALU.mult, op1=ALU.add),
                        reads=["RA", ("Ss", bg)], writes=[("Sso", bg % 2)])
                    S.dma("sp", ret_s[l, bg * 4:(bg + 1) * 4, h].rearrange("b d e -> d b e"), Sso[:],
                          reads=[("Sso", bg % 2)], writes=[("o_rets", l, h, bg)])
                S.op("dve", lambda: nc.vector.tensor_tensor(out=crt[:, 0:64], in0=psA[:, 2048:2112],
                                                            in1=dq_s[:, h * 64:(h + 1) * 64], op=ALU.mult),
                     reads=["pb4", "const"], writes=["crt"])
                S.op("dve", lambda: nc.vector.tensor_tensor(out=oT[:, sc], in0=psA[:, 1536:1600], in1=crt[:, 0:64], op=ALU.add),
                     reads=["pb3", "crt"], writes=["oT"])
                dbg_stop("A_chunks")
                osq = qT
                S.op("act", lambda: nc.scalar.activation(out=osq, in_=oT, func=AF.Square), reads=["oT", "qT"], writes=["qT"])
                stat_bcast([osq], ["qT", "onesb"], "RB")
                rsqrt_row(f1, prow("RB"), 1.0 / 128, "f1", "RB")
                S.op("dve", lambda: nc.vector.scalar_tensor_tensor(out=oT, in0=oT, scalar=col(rows, ("retg", l), h), in1=f1,
                                                                   op0=ALU.mult, op1=ALU.mult),
                     reads=["oT", "f1", "colT"], writes=["oT"])
                S.op("dve", lambda: nc.vector.tensor_tensor(out=mixT[:, h, :], in0=oT, in1=gS, op=ALU.mult),
                     reads=["oT", "gS"], writes=["mixT", "oTtmp"])
            sl = wout_slots(l, 0)
            accum_out(sl, [mixT[:, k, :] for k in range(4)], "mixT")

        def tm_rows(srcT_fn, ct, tstage, key):
            for which, (c0, n) in enumerate([(NPT - 128, 128), (NPT, 64)]):
                S.op("pe", lambda c0=c0, n=n, which=which: nc.tensor.transpose(
                    psS[0:n, which * 128: which * 128 + 128], srcT_fn(c0, n), identf[:]),
                    reads=[key, "const"], writes=["psS"])
                S.op("act", lambda n=n, which=which: nc.scalar.copy(
                    out=tstage[0:n, which, ct * 128:(ct + 1) * 128], in_=psS[0:n, which * 128: which * 128 + 128]),
                    reads=["psS"], writes=["tstage"])

        def mixer_B(l, rows):
            EXT = 30 + NPT + NB * 34
            o_ext = 0
            o_conv = 1600
            o_f1 = o_conv + 4 * T
            o_b1 = o_f1 + T
            o_hst = o_b1 + T
            ext = fa(o_ext, EXT)
            conv = fa(o_conv, 4 * T).rearrange("p (a b) -> p a b", a=4)
            f1 = fa(o_f1, T)
            b1 = ba(o_b1, T)
            b2 = ba(o_b1 + T // 2, T)
            hst = fa(o_hst, 512)
            tstage = fa(o_hst + 512, 1024).rearrange("p (a b) -> p a b", a=2)
            assert o_hst + 1536 <= ARENA
            S.barrier()
            ext_p = ext[:, 0:30 + NPT]
            ext_s = ext[:, 30 + NPT:EXT].rearrange("p (b r) -> p b r", b=NB)
            S.dma("act", c31_s[l, :, 0:26, :], st_c31[l, :, 4:30, :], writes=[("o_c31s_h", l)])
            for ct in range(4):
                s_b = W.get([(16, 256, 0, 128, win_cols(l, 2048 + ct * 128, 128)),
                             (16, 256, 128, 128, win_cols(l, 2560 + ct * 128, 128))])
                proj_fm(s_b, 0, "RA")
                proj_fm(s_b, 128, "RB")
                S.op("act", lambda: nc.scalar.activation(out=f1, in_=prow("RB"), func=AF.Sigmoid), reads=["RB"], writes=["f1"])
                S.op("dve", lambda: nc.vector.tensor_copy(out=ext[:, 0:30], in_=hx31[:, ct, :]), reads=["hx31"], writes=["ext"])
                hst4 = hst.rearrange("p (a b) -> p a b", a=4)
                for q4 in range(4):
                    S.dma("sp", hst4[0:120, q4, :],
                          st_c31[l, q4 * 4:(q4 + 1) * 4].rearrange("b r c -> (b r) c")[:, ct * 128:(ct + 1) * 128],
                          writes=[("hst", q4)])
                for q4 in range(4):
                    S.op("pe", lambda q4=q4: nc.tensor.transpose(psS[:, 256:376], hst4[0:120, q4, :], identf[0:120, 0:120]),
                         reads=[("hst", q4), "const"], writes=["psS"])
                    S.op("act", lambda q4=q4: nc.scalar.copy(
                        out=ext_s[:, q4 * 4:(q4 + 1) * 4, 0:30], in_=psS[:, 256:376].rearrange("p (b r) -> p b r", b=4)),
                        reads=["psS"], writes=["ext"])
                S.op("dve", lambda: nc.vector.tensor_tensor(out=ext_p[:, 30:30 + NPT], in0=prow("RA", NPT), in1=f1[:, 0:NPT], op=ALU.mult),
                     reads=["RA", "f1"], writes=["ext"])
                S.op("dve", lambda: nc.vector.tensor_tensor(
                    out=ext_s[:, :, 30:34], in0=psA[:, NPT:T].rearrange("p (b r) -> p b r", b=NB),
                    in1=f1[:, NPT:T].rearrange("p (b r) -> p b r", b=NB), op=ALU.mult),
                    reads=["RA", "f1"], writes=["ext"])
                def srcT(c0, n):
                    if c0 < NPT:
                        return ext_p[:, 30 + c0:30 + c0 + n]
                    return ext_s[:, :, 30:34]
                S.op("dve", lambda: nc.vector.tensor_copy(out=f1[:, 0:64].rearrange("p (b r) -> p b r", b=NB), in_=ext_s[:, :, 30:34]),
                     reads=["ext", "f1"], writes=["f1"])
                tm_rows(lambda c0, n: (ext_p[:, 30 + c0:30 + c0 + n] if c0 < NPT else f1[:, 0:64]), ct, tstage, "f1")
                wbase = rows[("c31w", l)]
                accp = [conv[:, ct, 0:NPT], f1[:, 0:NPT]]
                accs = [conv[:, ct, NPT:T].rearrange("p (b r) -> p b r", b=NB), f1[:, NPT:T].rearrange("p (b r) -> p b r", b=NB)]
                for j in range(31):
                    wc = colT[:, wbase + j * 4 + ct: wbase + j * 4 + ct + 1]
                    a_ = j % 2
                    kp, ks = ("cv", a_, "p"), ("cv", a_, "s")
                    if j == 0:
                        S.op("dve", lambda wc=wc: nc.vector.tensor_scalar(
                            out=accp[0], in0=ext_p[:, 0:NPT], scalar1=wc, scalar2=col(rows, ("c31b", l), ct),
                            op0=ALU.mult, op1=ALU.add), reads=["ext", "colT"], writes=[kp, "conv"])
                        S.op("dve", lambda wc=wc: nc.vector.tensor_scalar(
                            out=accs[0], in0=ext_s[:, :, 0:4], scalar1=wc,
                            scalar2=col(rows, ("c31b", l), ct), op0=ALU.mult, op1=ALU.add),
                            reads=["ext", "colT"], writes=[ks])
                    elif j == 1:
                        S.op("dve", lambda wc=wc: nc.vector.tensor_scalar(
                            out=accp[1], in0=ext_p[:, 1:1 + NPT], scalar1=wc, scalar2=None, op0=ALU.mult),
                            reads=["ext", "colT"], writes=[kp, "f1"])
                        S.op("dve", lambda wc=wc: nc.vector.tensor_scalar(
                            out=accs[1], in0=ext_s[:, :, 1:5], scalar1=wc, scalar2=None, op0=ALU.mult),
                            reads=["ext", "colT"], writes=[ks])
                    else:
                        S.op("dve", lambda wc=wc, j=j, a_=a_: nc.vector.scalar_tensor_tensor(
                            out=accp[a_], in0=ext_p[:, j:j + NPT], scalar=wc, in1=accp[a_],
                            op0=ALU.mult, op1=ALU.add), reads=["ext", "colT", kp], writes=[kp])
                        S.op("dve", lambda wc=wc, j=j, a_=a_: nc.vector.scalar_tensor_tensor(
                            out=accs[a_], in0=ext_s[:, :, j:j + 4], scalar=wc, in1=accs[a_],
                            op0=ALU.mult, op1=ALU.add), reads=["ext", "colT", ks], writes=[ks])
                S.op("dve", lambda: nc.vector.tensor_tensor(out=conv[:, ct, :], in0=conv[:, ct, :], in1=f1, op=ALU.add),
                     reads=[("cv", 0, "p"), ("cv", 0, "s"), ("cv", 1, "p"), ("cv", 1, "s"), "f1", "conv"],
                     writes=["conv", ("cv", 0, "p"), ("cv", 0, "s")])
            S.dma("sp", c31_p[l], tstage[98:128, 0, :], reads=["tstage"], writes=[("o_c31p", l)])
            for s in range(4):
                S.dma("sp", c31_s[l, :, 26 + s, :], tstage[s:64:4, 1, :], reads=["tstage"], writes=[("o_c31s", l, s)])
            for ct in range(4):
                b = b1 if ct % 2 == 0 else b2
                S.op("act", lambda ct=ct, b=b: nc.scalar.copy(out=b, in_=conv[:, ct, :]), reads=["conv"], writes=[("b12", ct % 2)])
                base = ROWBASE["RA"]
                fns = [lambda ct=ct, b=b, t0=t0, tn=tn: nc.tensor.matmul(
                    psA[:, base + t0: base + t0 + tn], lhsT=onesb[:], rhs=b[:, t0:t0 + tn], start=(ct == 0), stop=(ct == 3))
                    for (t0, tn) in TT]
                S.op("pe", fns, reads=[("b12", ct % 2), "onesb"], writes=["RA"])
            S.op("act", lambda: nc.scalar.mul(out=f1, in_=prow("RA"), mul=1.0 / 512), reads=["RA"], writes=["f1"])
            for ct in range(4):
                S.op("dve", lambda ct=ct: nc.vector.tensor_tensor(out=conv[:, ct, :], in0=conv[:, ct, :], in1=f1, op=ALU.subtract),
                     reads=["conv", "f1"], writes=["conv"])
            for ct in range(4):
                b = b1 if ct % 2 == 0 else b2
                S.op("act", lambda ct=ct, b=b: nc.scalar.activation(out=b, in_=conv[:, ct, :], func=AF.Square),
                     reads=["conv"], writes=[("b12", ct % 2)])
                base = ROWBASE["RB"]
                fns = [lambda ct=ct, b=b, t0=t0, tn=tn: nc.tensor.matmul(
                    psA[:, base + t0: base + t0 + tn], lhsT=onesb[:], rhs=b[:, t0:t0 + tn], start=(ct == 0), stop=(ct == 3))
                    for (t0, tn) in TT]
                S.op("pe", fns, reads=[("b12", ct % 2), "onesb"], writes=["RB"])
            rsqrt_row(f1, prow("RB"), 1.0 / 512, "f1", "RB")
            for ct in range(4):
                S.op("dve", lambda ct=ct: nc.vector.scalar_tensor_tensor(
                    out=conv[:, ct, :], in0=conv[:, ct, :], scalar=col(rows, ("clng", l), ct), in1=f1,
                    op0=ALU.mult, op1=ALU.mult), reads=["conv", "f1", "colT"], writes=["conv"])
                S.op("act", lambda ct=ct: nc.scalar.activation(out=mixT[:, ct, :], in_=conv[:, ct, :], func=AF.Silu,
                                                              bias=col(rows, ("clnb", l), ct), scale=1.0),
                     reads=["conv", "colT"], writes=["mixT"])
            sl = wout_slots(l, 1)
            accum_out(sl, [mixT[:, k, :] for k in range(4)], "mixT")

        def mixer_C(l, rows):
            EXT = 2 + NPT + NB * 6
            o_ext = 0
            o_f1 = 1124
            o_cv = o_f1 + T
            o_hst = o_cv + T
            ext = fa(o_ext, EXT)
            f1 = fa(o_f1, T)
            cv = fa(o_cv, T)
            hst = fa(o_hst, 512)
            tstage = fa(o_hst + 512, 1024).rearrange("p (a b) -> p a b", a=2)
            S.barrier()
            ext_p = ext[:, 0:2 + NPT]
            ext_s = ext[:, 2 + NPT:EXT].rearrange("p (b r) -> p b r", b=NB)
            S.dma("sp", hst[0:32, :], st_c3[l].rearrange("b r c -> (b r) c"), writes=["hst"])
            for ct in range(4):
                s_c = W.get([(16, 256, 0, 128, win_cols(l, 3584 + ct * 128, 128)),
                             (16, 256, 128, 128, win_cols(l, 4096 + ct * 128, 128))])
                proj_fm(s_c, 0, "RA")
                proj_fm(s_c, 128, "RB")
                S.op("act", lambda: nc.scalar.copy(out=f1, in_=prow("RA")), reads=["RA"], writes=["f1"])
                S.op("dve", lambda: nc.vector.tensor_copy(out=ext[:, 0:2], in_=hx3[:, ct, :]), reads=["hx3"], writes=["ext"])
                S.op("pe", lambda: nc.tensor.transpose(psS[:, 256:288], hst[0:32, ct * 128:(ct + 1) * 128], identf[0:32, 0:32]),
                     reads=["hst", "const"], writes=["psS"])
                S.op("act", lambda: nc.scalar.copy(out=ext_s[:, :, 0:2], in_=psS[:, 256:288].rearrange("p (b r) -> p b r", b=NB)),
                     reads=["psS"], writes=["ext"])
                S.op("dve", lambda: nc.vector.tensor_tensor(out=ext_p[:, 2:2 + NPT], in0=prow("RB", NPT), in1=f1[:, 0:NPT], op=ALU.mult),
                     reads=["RB", "f1"], writes=["ext"])
                S.op("dve", lambda: nc.vector.tensor_tensor(
                    out=ext_s[:, :, 2:6], in0=psA[:, 1536 + NPT:1536 + T].rearrange("p (b r) -> p b r", b=NB),
                    in1=f1[:, NPT:T].rearrange("p (b r) -> p b r", b=NB), op=ALU.mult), reads=["RB", "f1"], writes=["ext"])
                S.op("dve", lambda: nc.vector.tensor_copy(out=f1[:, 0:64].rearrange("p (b r) -> p b r", b=NB), in_=ext_s[:, :, 2:6]),
                     reads=["ext", "f1"], writes=["f1"])
                tm_rows(lambda c0, n: (ext_p[:, 2 + c0:2 + c0 + n] if c0 < NPT else f1[:, 0:64]), ct, tstage, "f1")
                wbase = rows[("scw", l)]
                for j in range(3):
                    wc = colT[:, wbase + j * 4 + ct: wbase + j * 4 + ct + 1]
                    if j == 0:
                        S.op("dve", lambda wc=wc: nc.vector.tensor_scalar(out=cv[:, 0:NPT], in0=ext_p[:, 0:NPT], scalar1=wc, scalar2=None,
                                                                          op0=ALU.mult), reads=["ext", "colT"], writes=["cv"])
                        S.op("dve", lambda wc=wc: nc.vector.tensor_scalar(
                            out=cv[:, NPT:T].rearrange("p (b r) -> p b r", b=NB), in0=ext_s[:, :, 0:4], scalar1=wc, scalar2=None,
                            op0=ALU.mult), reads=["ext", "colT"], writes=["cv"])
                    else:
                        S.op("dve", lambda wc=wc, j=j: nc.vector.scalar_tensor_tensor(
                            out=cv[:, 0:NPT], in0=ext_p[:, j:j + NPT], scalar=wc, in1=cv[:, 0:NPT], op0=ALU.mult, op1=ALU.add),
                            reads=["ext", "colT", "cv"], writes=["cv"])
                        S.op("dve", lambda wc=wc, j=j: nc.vector.scalar_tensor_tensor(
                            out=cv[:, NPT:T].rearrange("p (b r) -> p b r", b=NB), in0=ext_s[:, :, j:j + 4], scalar=wc,
                            in1=cv[:, NPT:T].rearrange("p (b r) -> p b r", b=NB), op0=ALU.mult, op1=ALU.add),
                            reads=["ext", "colT", "cv"], writes=["cv"])
                if ct % 2 == 0:
                    s_cb = W.get([(16, 256, 0, 256, win_cols(l, 3072 + ct * 128, 256))])
                proj_fm(s_cb, (ct % 2) * 128, "RA")
                S.op("dve", lambda ct=ct: nc.vector.tensor_tensor(out=mixT[:, ct, :], in0=prow("RA"), in1=cv, op=ALU.mult),
                     reads=["RA", "cv"], writes=["mixT"])
            S.dma("sp", c3_p[l], tstage[126:128, 0, :], reads=["tstage"], writes=[("o_c3p", l)])
            for s in range(2):
                S.dma("sp", c3_s[l, :, s, :], tstage[2 + s:64:4, 1, :], reads=["tstage"], writes=[("o_c3s", l, s)])
            sl = wout_slots(l, 2)
            accum_out(sl, [mixT[:, k, :] for k in range(4)], "mixT")

        def mixer_D(l, rows):
            o_lng = 0
            o_lnb = 512
            o_sgb = 1024
            o_vn = 1536
            o_sq = 2048
            o_vb = 2560
            o_wm = o_vb + 2304
            o_wms = o_wm + 256
            o_wst = o_wms + 128
            o_sm = o_wst + 512
            o_u = o_sm + 8
            o_mb = o_u + 64
            lng = fa(o_lng, 512)
            lnb = fa(o_lnb, 512)
            sgb = fa(o_sgb, 512)
            vn = fa(o_vn, 512)
            sqs = fa(o_sq, 512)
            vb = ba(o_vb, 9 * 512).rearrange("p (a b) -> p a b", a=9)
            wm = ba(o_wm, 512).rearrange("p (a b) -> p a b", a=4)
            wms = ba(o_wms, 256).rearrange("p (a b) -> p a b", a=4)
            wst4 = [fa(o_wst + k * 128, 128) for k in range(4)]
            sm = fa(o_sm, 8)
            U = fa(o_u, 64)
            mb = fa(o_mb, T)
            assert o_mb + T <= ARENA
            S.barrier()
            S.dma("sp", lng, sgu_ln_g[l].partition_broadcast(128), writes=["lng"])
            S.dma("sp", lnb, sgu_ln_b[l].partition_broadcast(128), writes=["lnb"])
            S.dma("sp", sgb, sgu_b[l].partition_broadcast(128), writes=["sgb"])
            for g in range(4):
                S.dma("sp", wst4[g], sgu_w[l, g], writes=[("wst", g)])
            for g in range(4):
                wst = wst4[g]
                S.op("pe", lambda wst=wst: nc.tensor.transpose(psS[:, 0:128], wst, identf[:]), reads=[("wst", g), "const"], writes=["psS"])
                S.op("dve", lambda g=g: nc.vector.tensor_tensor(out=wm[:, g, :], in0=psS[:, 0:128], in1=triu[:], op=ALU.mult),
                     reads=["psS", "const"], writes=["wm"])
                S.op("pe", lambda wst=wst: nc.tensor.matmul(psS[0:4, 128:192], lhsT=wst[0:4, 0:4], rhs=Rm[:], start=True, stop=True),
                     reads=[("wst", g), "const"], writes=["psS"])
                S.op("act", lambda: nc.scalar.copy(out=U[0:4, :], in_=psS[0:4, 128:192]), reads=["psS"], writes=["U"])
                S.op("pe", lambda: nc.tensor.matmul(psS[0:64, 256:320], lhsT=Rm[:], rhs=U[0:4, :], start=True, stop=True),
                     reads=["U", "const"], writes=["psS"])
                S.op("dve", lambda g=g: nc.vector.tensor_tensor(out=wms[0:64, g, :], in0=psS[0:64, 256:320], in1=bm[:], op=ALU.mult),
                     reads=["psS", "const"], writes=["wms"])
            dbg_stop("D_w")
            s_v0 = W.get([(16, 256, 0, 256, win_cols(l, 5120, 256))])
            s_v1 = W.get([(16, 256, 0, 256, win_cols(l, 5376, 256))])
            for i in range(9):
                n = 128 if i < 8 else 64
                row = "RA" if i % 2 == 0 else "RB"
                base = ROWBASE[row]
                fns = []
                for half, sl_ in enumerate((s_v0, s_v1)):
                    wv = wview(sl_, 0, 16, 256)
                    for kc in range(16):
                        fns.append(lambda half=half, wv=wv, kc=kc, i=i, n=n, base=base: nc.tensor.matmul(
                            psA[0:n, base + half * 256: base + half * 256 + 256], lhsT=hT[:, kc, i * 128:i * 128 + n],
                            rhs=wv[:, kc, :], start=(kc == 0), stop=(kc == 15)))
                S.op("pe", fns, reads=[("w", s_v0), ("w", s_v1), "hT"], writes=[row])
                dv = psA[0:n, base:base + 512]
                dbg_stop("D_p0")
                S.op("dve", lambda dv=dv, n=n: nc.vector.tensor_reduce(out=sm[0:n, 0:1], in_=dv, axis=AX.X, op=ALU.add),
                     reads=[row], writes=["sm0"])
                dbg_stop("D_r0")
                S.op("act", lambda dv=dv, n=n: nc.scalar.copy(out=sqs[0:n, :], in_=dv), reads=[row, "sm0"], writes=["sqs"])
                S.op("dve", lambda n=n: nc.vector.tensor_tensor(out=sqs[0:n, :], in0=sqs[0:n, :], in1=sqs[0:n, :], op=ALU.mult), reads=["sqs"], writes=["sqs"])
                dbg_stop("D_qa")
                S.op("dve", lambda n=n: nc.vector.tensor_reduce(out=sm[0:n, 1:2], in_=sqs[0:n, :], axis=AX.X, op=ALU.add),
                     reads=["sqs"], writes=["sm1"])
                dbg_stop("D_q0")
                S.op("dve", lambda n=n: nc.vector.tensor_scalar(out=sm[0:n, 2:3], in0=sm[0:n, 0:1], scalar1=1.0 / 512, scalar2=None, op0=ALU.mult),
                     reads=["sm0"], writes=["sm2"])
                S.op("dve", lambda n=n: nc.vector.tensor_tensor(out=sm[0:n, 3:4], in0=sm[0:n, 2:3], in1=sm[0:n, 2:3], op=ALU.mult),
                     reads=["sm2"], writes=["sm3"])
                S.op("dve", lambda n=n: nc.vector.scalar_tensor_tensor(out=sm[0:n, 4:5], in0=sm[0:n, 1:2], scalar=1.0 / 512, in1=sm[0:n, 3:4],
                                                                       op0=ALU.mult, op1=ALU.subtract), reads=["sm1", "sm3"], writes=["sm4"])
                dbg_stop("D_t0")
                S.op("act", lambda n=n: nc.scalar.activation(out=sm[0:n, 5:6], in_=sm[0:n, 4:5], func=AF.Sqrt, bias=epsc[0:n, :], scale=1.0),
                     reads=["sm4", "epsc"], writes=["sm5"])
                S.op("dve", lambda n=n: nc.vector.reciprocal(out=sm[0:n, 6:7], in_=sm[0:n, 5:6]), reads=["sm5"], writes=["sm6"])
                dbg_stop("D_s0")
                S.op("dve", lambda dv=dv, n=n: nc.vector.tensor_scalar(out=vn[0:n, :], in0=dv, scalar1=sm[0:n, 2:3], scalar2=sm[0:n, 6:7],
                                                                       op0=ALU.subtract, op1=ALU.mult),
                     reads=[row, "sm2", "sm6"], writes=["vn"])
                S.op("dve", lambda n=n: nc.vector.tensor_tensor(out=vn[0:n, :], in0=vn[0:n, :], in1=lng[0:n, :], op=ALU.mult),
                     reads=["vn", "lng"], writes=["vn"])
                S.op("dve", lambda n=n: nc.vector.tensor_tensor(out=vn[0:n, :], in0=vn[0:n, :], in1=lnb[0:n, :], op=ALU.add),
                     reads=["vn", "lnb"], writes=["vn"])
                S.op("act", lambda n=n, i=i: nc.scalar.copy(out=vb[0:n, i, :], in_=vn[0:n, :]), reads=["vn"], writes=["vb"])
                dbg_stop("D_v0")
                if i == 8:
                    S.dma("sp", vs_out[l], vn[0:64, :], reads=["vn"], writes=[("o_vs", l)])
            dbg_stop("D_ln")
            for g in range(4):
                if g % 2 == 0:
                    s_u = W.get([(16, 256, 0, 256, win_cols(l, 4608 + g * 128, 256))])
                base = ROWBASE["RA"]
                fns = [lambda c=c, g=g: nc.tensor.matmul(psA[:, base + c * 128: base + (c + 1) * 128],
                                                         lhsT=vb[:, c, g * 128:(g + 1) * 128], rhs=wm[:, g, :], start=True, stop=True)
                       for c in range(8)]
                fns.append(lambda g=g: nc.tensor.matmul(psA[:, base + NPT: base + T], lhsT=vb[0:64, 8, g * 128:(g + 1) * 128],
                                                        rhs=wms[0:64, g, :], start=True, stop=True))
                S.op("pe", fns, reads=["vb", "wm", "wms"], writes=["RA"])
                S.op("dve", lambda g=g: nc.vector.tensor_tensor(
                    out=mb[:, 0:NPT].rearrange("p (c t) -> p c t", c=8), in0=psA[:, base:base + NPT].rearrange("p (c t) -> p c t", c=8),
                    in1=sgb[:, g * 128:(g + 1) * 128].unsqueeze(1).to_broadcast([128, 8, 128]), op=ALU.add),
                    reads=["RA", "sgb"], writes=["mb"])
                S.op("dve", lambda g=g: nc.vector.tensor_tensor(
                    out=mb[:, NPT:T].rearrange("p (b r) -> p b r", b=NB), in0=psA[:, base + NPT:base + T].rearrange("p (b r) -> p b r", b=NB),
                    in1=sgb[:, g * 128:g * 128 + 4].unsqueeze(1).to_broadcast([128, NB, 4]), op=ALU.add),
                    reads=["RA", "sgb"], writes=["mb"])
                proj_fm(s_u, (g % 2) * 128, "RB")
                S.op("dve", lambda g=g: nc.vector.tensor_tensor(out=mixT[:, g, :], in0=prow("RB"), in1=mb, op=ALU.mult),
                     reads=["RB", "mb"], writes=["mixT"])
            dbg_stop("D_mix")
            sl = wout_slots(l, 3)
            accum_out(sl, [mixT[:, k, :] for k in range(4)], "mixT")

        def ffn_pass(w1, w3, w2, gm=None):
            o_act = 0
            o_s = 2 * T
            act = ba(o_act, 4 * T).rearrange("p (a b) -> p a b", a=4)
            sbuf = [fa(o_s, T), fa(o_s + T, T)]
            for grp in range(DFF // 512):
                f0 = grp * 512
                for half in range(2):
                    c0 = f0 + half * 256
                    s1 = W.get([(16, 256, 0, 256, w1[:, c0:c0 + 256].rearrange("(kc p) n -> p kc n", p=128))])
                    s3 = W.get([(16, 256, 0, 256, w3[:, c0:c0 + 256].rearrange("(kc p) n -> p kc n", p=128))])
                    for t in range(2):
                        ti = half * 2 + t
                        sb_ = sbuf[ti % 2]
                        skey = ("fs", ti % 2)
                        proj_fm(s1, t * 128, "RA")
                        S.op("act", lambda sb_=sb_: nc.scalar.activation(out=sb_, in_=prow("RA"), func=AF.Silu),
                             reads=["RA"], writes=[skey])
                        proj_fm(s3, t * 128, "RB")
                        if gm is not None:
                            S.op("dve", lambda sb_=sb_: nc.vector.tensor_tensor(out=sb_, in0=sb_, in1=gm, op=ALU.mult),
                                 reads=[skey, "gm"], writes=[skey])
                        S.op("dve", lambda sb_=sb_, ti=ti: nc.vector.tensor_tensor(out=act[:, ti, :], in0=prow("RB"), in1=sb_, op=ALU.mult),
                             reads=["RB", skey], writes=[("actT", ti)])
                sl = []
                for half in range(2):
                    r0 = f0 + half * 256
                    sl.append(W.get([(2, 2048, 0, 2048, w2[r0:r0 + 256, :].rearrange("(kc p) n -> p kc n", p=128))]))
                accum_out(sl, [act[:, k, :] for k in range(4)], [("actT", k) for k in range(4)])

        def moe(rows):
            o_gm = 4 * T
            o_rw = o_gm + T
            o_lg = o_rw + 128
            gm = fa(o_gm, T)
            rw = fa(o_rw, 128).rearrange("p (a b) -> p a b", a=16)
            lg = fa(o_lg, 72).rearrange("p (a b) -> p a b", a=9)
            m1 = fa(o_lg + 72, 9)
            m2 = fa(o_lg + 84, 9)
            oh1 = fa(o_lg + 96, 72).rearrange("p (a b) -> p a b", a=9)
            oh2 = fa(o_lg + 168, 72).rearrange("p (a b) -> p a b", a=9)
            tmp = fa(o_lg + 240, 72).rearrange("p (a b) -> p a b", a=9)
            g1 = fa(o_lg + 312, 9)
            g2 = fa(o_lg + 324, 9)
            G = fa(o_lg + 336, 72).rearrange("p (a b) -> p a b", a=9)
            GT = fa(o_lg + 408, T)
            assert o_lg + 408 + T <= ARENA
            S.barrier()
            S.op("dve", lambda: nc.vector.memset(lg, -1e30), writes=["lg"])
            S.dma("sp", rw, router_w.rearrange("(c p) e -> p c e", p=128), writes=["rw"])
            for c in range(16):
                S.op("dve", lambda c=c: nc.vector.tensor_scalar(out=rw[:, c, :], in0=rw[:, c, :], scalar1=col(rows, ("ffng", 1), c),
                                                                scalar2=None, op0=ALU.mult), reads=["rw", "colT"], writes=["rw"])
            for i in range(9):
                n = 128 if i < 8 else 64
                fns = [lambda c=c, i=i, n=n: nc.tensor.matmul(psS[0:n, i * 8:(i + 1) * 8], lhsT=xT[:, c, i * 128:i * 128 + n],
                                                              rhs=rw[:, c, :], start=(c == 0), stop=(c == 15)) for c in range(16)]
                S.op("pe", fns, reads=["xT", "rw"], writes=["psS"])
            rstd_row = fa(T, T)
            onesf = fa(o_lg + 348, 1)
            S.op("dve", lambda: nc.vector.memset(onesf, 1.0), writes=["onesf"])
            fns = [lambda i=i: nc.tensor.matmul(psS[0:(128 if i < 8 else 64), 128 + i:129 + i],
                                                lhsT=rstd_row[0:1, i * 128:i * 128 + (128 if i < 8 else 64)], rhs=onesf[0:1, 0:1],
                                                start=True, stop=True) for i in range(9)]
            S.op("pe", fns, reads=["rstd", "onesf"], writes=["psS"])
            S.op("act", lambda: nc.scalar.copy(out=g2, in_=psS[:, 128:137]), reads=["psS"], writes=["g2"])
            for i in range(9):
                n = 128 if i < 8 else 64
                S.op("dve", lambda i=i, n=n: nc.vector.tensor_scalar(out=lg[0:n, i, :], in0=psS[0:n, i * 8:(i + 1) * 8], scalar1=g2[0:n, i:i + 1],
                                                                     scalar2=None, op0=ALU.mult), reads=["psS", "g2", "lg"], writes=["lg"])
            S.op("dve", lambda: nc.vector.tensor_reduce(out=m1, in_=lg, axis=AX.X, op=ALU.max), reads=["lg"], writes=["m1"])
            S.op("dve", lambda: nc.vector.tensor_tensor(out=oh1, in0=lg, in1=m1.unsqueeze(2).to_broadcast([128, 9, 8]), op=ALU.is_equal),
                 reads=["lg", "m1"], writes=["oh1"])
            S.op("dve", lambda: nc.vector.scalar_tensor_tensor(out=tmp, in0=oh1, scalar=-1e30, in1=lg, op0=ALU.mult, op1=ALU.add),
                 reads=["oh1", "lg"], writes=["tmp"])
            S.op("dve", lambda: nc.vector.tensor_reduce(out=m2, in_=tmp, axis=AX.X, op=ALU.max), reads=["tmp"], writes=["m2"])
            S.op("dve", lambda: nc.vector.tensor_tensor(out=oh2, in0=tmp, in1=m2.unsqueeze(2).to_broadcast([128, 9, 8]), op=ALU.is_equal),
                 reads=["tmp", "m2"], writes=["oh2"])
            S.op("dve", lambda: nc.vector.tensor_tensor(out=g1, in0=m1, in1=m2, op=ALU.subtract), reads=["m1", "m2"], writes=["g1"])
            S.op("act", lambda: nc.scalar.activation(out=g1, in_=g1, func=AF.Sigmoid), reads=["g1"], writes=["g1"])
            S.op("dve", lambda: nc.vector.tensor_scalar(out=g2, in0=g1, scalar1=-1.0, scalar2=1.0, op0=ALU.mult, op1=ALU.add),
                 reads=["g1", "g2"], writes=["g2"])
            S.op("dve", lambda: nc.vector.tensor_tensor(out=G, in0=oh1, in1=g1.unsqueeze(2).to_broadcast([128, 9, 8]), op=ALU.mult),
                 reads=["oh1", "g1"], writes=["G"])
            S.op("dve", lambda: nc.vector.tensor_tensor(out=tmp, in0=oh2, in1=g2.unsqueeze(2).to_broadcast([128, 9, 8]), op=ALU.mult),
                 reads=["oh2", "g2", "tmp"], writes=["tmp"])
            S.op("dve", lambda: nc.vector.tensor_tensor(out=G, in0=G, in1=tmp, op=ALU.add), reads=["G", "tmp"], writes=["G"])
            for i in range(9):
                n = 128 if i < 8 else 64
                S.op("pe", lambda i=i, n=n: nc.tensor.transpose(psA[0:8, i * 128:i * 128 + n], G[0:n, i, :], identf[0:n, 0:n]),
                     reads=["G", "const"], writes=["RA"])
            S.op("act", lambda: nc.scalar.copy(out=GT[0:8, :], in_=psA[0:8, 0:T]), reads=["RA"], writes=["GT"])
            for e in range(NEXP):
                base = ROWBASE["RB"]
                fns = [lambda e=e, t0=t0, tn=tn: nc.tensor.matmul(psA[:, base + t0: base + t0 + tn], lhsT=esel[:, e * 128:(e + 1) * 128],
                                                                  rhs=GT[0:8, t0:t0 + tn], start=True, stop=True) for (t0, tn) in TT]
                S.op("pe", fns, reads=["GT", "const"], writes=["RB"])
                S.op("act", lambda: nc.scalar.copy(out=gm, in_=prow("RB")), reads=["RB"], writes=["gm"])
                ffn_pass(moe_w1[e], moe_w3[e], moe_w2[e], gm=gm)

        def final_out(rows, norm=True):
            S.barrier()
            rstd = fa(0, T)
            sq = [ba(T, T), ba(T + T // 2, T)]
            for c in range(16 if norm else 0):
                b = sq[c % 2]
                S.op("act", lambda c=c, b=b: nc.scalar.activation(out=b, in_=xT[:, c, :], func=AF.Square),
                     reads=["xT"], writes=[("sq", c % 2)])
                base = ROWBASE["RA"]
                fns = [lambda c=c, b=b, t0=t0, tn=tn: nc.tensor.matmul(
                    psA[:, base + t0: base + t0 + tn], lhsT=onesb[:], rhs=b[:, t0:t0 + tn], start=(c == 0), stop=(c == 15))
                    for (t0, tn) in TT]
                S.op("pe", fns, reads=[("sq", c % 2), "onesb"], writes=["RA"])
            if norm:
                rsqrt_row(rstd, prow("RA"), 1.0 / D, "rstd", "RA")
            for c in range(16 if norm else 0):
                S.op("dve", lambda c=c: nc.vector.scalar_tensor_tensor(
                    out=xT[:, c, :], in0=xT[:, c, :], scalar=col(rows, ("fing",), c), in1=rstd, op0=ALU.mult, op1=ALU.mult),
                    reads=["xT", "rstd", "colT"], writes=["xT"])
            ost = [fa(2 * T, 2048), fa(2 * T + 2048, 2048)]
            for i in range(9):
                n = 128 if i < 8 else 64
                o = ost[i % 2]
                for g in range(4):
                    bank = ROWBASE["RA"] + (g % 2) * 512 if g < 2 else ROWBASE["RB"] + (g % 2) * 512
                    key = "pb%d" % (bank // 512)
                    fns = [lambda j=j, g=g, bank=bank: nc.tensor.transpose(
                        psA[0:n, bank + j * 128: bank + (j + 1) * 128], xT[:, g * 4 + j, i * 128:i * 128 + n], identf[:])
                        for j in range(4)]
                    S.op("pe", fns, reads=["xT", "const"], writes=[key])
                    if g % 2 == 0:
                        S.op("dve", lambda g=g, bank=bank, o=o: nc.vector.tensor_copy(out=o[0:n, g * 512:(g + 1) * 512], in_=psA[0:n, bank:bank + 512]),
                             reads=[key], writes=[("ost", i % 2)])
                    else:
                        S.op("act", lambda g=g, bank=bank, o=o: nc.scalar.copy(out=o[0:n, g * 512:(g + 1) * 512], in_=psA[0:n, bank:bank + 512]),
                             reads=[key], writes=[("ost", i % 2)])
                S.dma("sp", y_out[i * 128:i * 128 + n, :], o[0:n, :], reads=[("ost", i % 2)], writes=[("o_y", i)])

        def emit_all():
            st = [0]

            def cut():
                st[0] += 1
                if STAGE == st[0]:
                    final_out(rows, norm=False)
                    S.finish()
                    return True
                return False

            rows = load_consts()
            load_x()
            if cut():
                return
            for l in range(2):
                rmsnorm_to_hT(("mixg", l), rows)
                dbg_stop("norm0")
                if XCH:
                    pre_pass(l)
                mixer_D(l, rows)
                load_xch(l)
                if cut():
                    return
                mixer_A(l, rows)
                if cut():
                    return
                mixer_B(l, rows)
                if cut():
                    return
                mixer_C(l, rows)
                if cut():
                    return
                S.barrier()
                rmsnorm_to_hT(("ffng", l), rows)
                if l == 0:
                    ffn_pass(dense_w1[0], dense_w3[0], dense_w2[0])
                else:
                    moe(rows)
                if cut():
                    return
            final_out(rows)
            S.finish()

        S.plan = True
        try:
            emit_all()
        except StopEmit:
            pass
        S.plan = False
        S.reset()
        W.reset()
        try:
            emit_all()
        except StopEmit:
            S.finish()
    return nc


def _consts(half):
    c = {}
    c["c_ident"] = np.eye(128, dtype=np.float32)
    pos = np.concatenate([np.arange(NPT, dtype=np.float32) + np.float32(half * NPT),
                          np.tile(np.arange(4, dtype=np.float32) + np.float32(16384.0), NB)]).astype(np.float32)
    inv = (np.float32(10000.0) ** (-np.arange(64, dtype=np.float32) / np.float32(64))).astype(np.float32)
    ang = (pos[None, :] * inv[:, None]).astype(np.float32)
    cs = np.cos(ang).astype(np.float32)
    sn = np.sin(ang).astype(np.float32)
    c["c_rope_c"] = np.concatenate([cs, cs], 0)
    c["c_rope_s"] = np.concatenate([-sn, sn], 0)
    scale = 128.0 ** -0.5
    logg = [math.log1p(-2.0 ** (-5.0 - h)) for h in range(4)]
    j = np.arange(128)[:, None]
    i = np.arange(128)[None, :]
    dm = np.zeros((128, 4, 128), np.float64)
    dms = np.zeros((64, 4, 64), np.float64)
    dqv = np.zeros((128, 4, 128), np.float64)
    dqs = np.zeros((128, 4, 64), np.float64)
    dkv = np.zeros((128, 8), np.float64)
    js = np.arange(64)[:, None]
    is_ = np.arange(64)[None, :]
    for h in range(4):
        dm[:, h, :] = np.where(i >= j, np.exp(logg[h] * np.maximum(i - j, 0)), 0.0) * scale
        same = (js // 4) == (is_ // 4)
        dms[:, h, :] = np.where(same & (is_ >= js), np.exp(logg[h] * np.maximum(is_ - js, 0)), 0.0) * scale
        dqv[:, h, :] = np.exp(logg[h] * (np.arange(128) + 1.0))[None, :]
        dqs[:, h, :] = np.exp(logg[h] * ((np.arange(64) % 4) + 1.0))[None, :]
        dkv[:, h] = np.exp(logg[h] * (127.0 - np.arange(128))) * scale
        dkv[:64, 4 + h] = np.exp(logg[h] * (3.0 - (np.arange(64) % 4))) * scale
    c["c_dmask"] = dm.reshape(128, 512).astype(np.float32)
    c["c_dmask_s"] = dms.reshape(64, 256).astype(np.float32)
    c["c_dq"] = dqv.reshape(128, 512).astype(np.float32)
    c["c_dq_s"] = dqs.reshape(128, 256).astype(np.float32)
    c["c_dkv"] = dkv.astype(np.float32)
    c["c_ind"] = ((np.arange(64)[:, None] // 4) == np.arange(NB)[None, :]).astype(np.float32)
    c["c_triu"] = (j <= i).astype(np.float32)
    c["c_bm"] = (((js // 4) == (is_ // 4)) & (js <= is_)).astype(np.float32)
    c["c_R"] = ((np.arange(64)[None, :] % 4) == np.arange(4)[:, None]).astype(np.float32)
    es = np.zeros((8, 8, 128), np.float32)
    for e in range(8):
        es[e, e, :] = 1.0
    c["c_esel"] = es.reshape(8, 1024)
    c["c_s0mask"] = np.full((128, 1), float(half), np.float32)
    return c


_NC_CACHE = {}


def kernel(**inputs):
    f = lambda k: np.ascontiguousarray(np.asarray(inputs[k], dtype=np.float32))
    x_prompt = f("x_prompt")
    x_sample = f("x_sample")
    shared = {
        "mix_norm_g": f("mix_norm_g"), "w_in": f("w_in"), "ret_norm_g": f("ret_norm_g"), "conv31_w": f("conv31_w"),
        "conv31_b": f("conv31_b"), "conv_ln_g": f("conv_ln_g"), "conv_ln_b": f("conv_ln_b"), "sconv_w": f("sconv_w"),
        "sgu_ln_g": f("sgu_ln_g"), "sgu_ln_b": f("sgu_ln_b"), "sgu_w": f("sgu_w"),
        "sgu_b": f("sgu_b").reshape(2, 512), "w_out": f("w_out"), "ffn_norm_g": f("ffn_norm_g"),
        "dense_w1": f("dense_w1"), "dense_w3": f("dense_w3"), "dense_w2": f("dense_w2"),
        "router_w": f("router_w")[0], "moe_w1": f("moe_w1")[0], "moe_w3": f("moe_w3")[0], "moe_w2": f("moe_w2")[0],
        "final_norm_g": f("final_norm_g"),
    }
    st_ret = f("state_ret")
    st_c31 = f("state_conv31")
    st_c3 = f("state_conv3")
    in_maps = []
    for c in range(NCORES):
        b, half = c // 2, c % 2
        m = dict(shared)
        m["xin"] = np.ascontiguousarray(np.concatenate(
            [x_prompt[b, half * NPT:(half + 1) * NPT], x_sample[c * NB:(c + 1) * NB].reshape(NST, D)], 0))
        m["st_ret"] = np.ascontiguousarray(st_ret[:, c * NB:(c + 1) * NB])
        m["st_c31"] = np.ascontiguousarray(st_c31[:, c * NB:(c + 1) * NB])
        m["st_c3"] = np.ascontiguousarray(st_c3[:, c * NB:(c + 1) * NB])
        m.update(_consts(half))
        in_maps.append(m)
    if "nc" not in _NC_CACHE:
        _NC_CACHE["nc"] = build_program()
    res = run_bass_kernel_spmd(_NC_CACHE["nc"], in_maps, core_ids=list(range(NCORES)))
    R = res.results
    y_prompt = np.zeros((4, 2048, D), np.float32)
    y_sample = np.zeros((128, 4, D), np.float32)
    ret_p = np.zeros((2, 4, 4, 128, 128), np.float32)
    ret_s = np.zeros((2, 128, 4, 128, 128), np.float32)
    c31_p = np.zeros((2, 4, 30, 512), np.float32)
    c31_s = np.zeros((2, 128, 30, 512), np.float32)
    c3_p = np.zeros((2, 4, 2, 512), np.float32)
    c3_s = np.zeros((2, 128, 2, 512), np.float32)
    vs = np.zeros((2, 128, 4, 512), np.float32)
    for c in range(NCORES):
        b, half = c // 2, c % 2
        r = R[c]
        y_prompt[b, half * NPT:(half + 1) * NPT] = r["y_out"][:NPT]
        y_sample[c * NB:(c + 1) * NB] = r["y_out"][NPT:].reshape(NB, 4, D)
        ret_s[:, c * NB:(c + 1) * NB] = r["ret_s"]
        c31_s[:, c * NB:(c + 1) * NB] = r["c31_s"]
        c3_s[:, c * NB:(c + 1) * NB] = r["c3_s"]
        vs[:, c * NB:(c + 1) * NB] = r["vs_out"].reshape(2, NB, 4, 512)
        if half == 1:
            ret_p[:, b] = r["ret_p"]
            c31_p[:, b] = r["c31_p"]
            c3_p[:, b] = r["c3_p"]
    return (y_prompt, y_sample, ret_p, ret_s, c31_p, c31_s, c3_p, c3_s, vs)
```

```python
import math
from contextlib import ExitStack

import numpy as np
import concourse.bass as bass
import concourse.mybir as mybir
from concourse.bass_utils import run_bass_kernel_spmd

F32 = mybir.dt.float32
BF16 = mybir.dt.bfloat16
ALU = mybir.AluOpType
AF = mybir.ActivationFunctionType
AX = mybir.AxisListType

NCORES = 8
D = 2048
DIN = 5632
DFF = 5632
NPT = 1024
NST = 64
T = NPT + NST
TT = [(0, 512), (512, 512), (1024, 64)]
NB = 16
EPS = 1e-6
NBUF = 4
SLOT = 4096
NEXP = 8
GAM = [1.0 - 2.0 ** (-5.0 - h) for h in range(4)]
SAME_SYNC = True
DBG = None
LITE = False
XCH = True


class StopEmit(Exception):
    pass


def dbg_stop(tag):
    if DBG == tag:
        raise StopEmit()


STAGE = 0


class Sync:
    def __init__(self, nc, es):
        self.nc = nc
        self.plan = False
        self.engs = {}
        for name, obj in [("pe", nc.tensor), ("dve", nc.vector), ("act", nc.scalar),
                          ("pool", nc.gpsimd), ("sp", nc.sync)]:
            self.engs[name] = dict(obj=obj, sem=es.enter_context(nc.semaphore("sem_" + name)), cnt=0, waited={})
        self.dpools = {}
        for q, n in [("sp", 8), ("pool", 6), ("act", 4)]:
            self.dpools[q] = dict(i=0, sems=[dict(sem=es.enter_context(nc.semaphore(f"d_{q}{i}")), cnt=0)
                                             for i in range(n)])
        self.lastw = {}
        self.readers = {}
        self.cc = [dict(sem=es.enter_context(nc.semaphore(f"cc{i}")), cnt=0) for i in range(2)]

    def coll(self, i, fn, reads=(), writes=()):
        if self.plan:
            return
        reads, writes = self._x(reads), self._x(writes)
        self._need("pool", self._deps(reads, writes))
        ins = fn()
        self.cc[i]["cnt"] += 1
        ins.then_inc(self.cc[i]["sem"])
        self._mark(reads, writes, (("cc", i), self.cc[i]["cnt"]))

    def reset(self):
        for c in self.cc:
            c["cnt"] = 0
        for e in self.engs.values():
            e["cnt"] = 0
            e["waited"] = {}
        for p in self.dpools.values():
            p["i"] = 0
            for s in p["sems"]:
                s["cnt"] = 0
        self.lastw = {}
        self.readers = {}

    def semh(self, sk):
        if isinstance(sk, str):
            return self.engs[sk]["sem"]
        if sk[0] == "cc":
            return self.cc[sk[1]]["sem"]
        return self.dpools[sk[0]]["sems"][sk[1]]["sem"]

    def _need(self, eng, deps):
        e = self.engs[eng]
        best = {}
        for sk, v in deps:
            if sk == eng and not SAME_SYNC:
                continue
            if v > best.get(sk, 0):
                best[sk] = v
        for sk, v in best.items():
            if e["waited"].get(sk, 0) >= v:
                continue
            e["obj"].wait_ge(self.semh(sk), v)
            e["waited"][sk] = v

    PSUM_KEYS = ("pb0", "pb1", "pb2", "pb3", "pb4", "pb5", "psS", "psB")

    def _deps(self, reads, writes):
        d = []
        for k in reads:
            if k in self.lastw:
                d.append(self.lastw[k])
            if k in self.PSUM_KEYS:
                d.extend(self.readers.get(k, {}).items())
        for k in writes:
            if k in self.lastw:
                d.append(self.lastw[k])
            d.extend(self.readers.get(k, {}).items())
        return d

    def _mark(self, reads, writes, dep):
        for k in reads:
            r = self.readers.setdefault(k, {})
            r[dep[0]] = max(r.get(dep[0], 0), dep[1])
        for k in writes:
            self.lastw[k] = dep
            self.readers[k] = {}

    ALIAS = {"RA": ("pb0", "pb1", "pb2"), "RB": ("pb3", "pb4", "pb5")}
    ALIAS.update({("xT", c): ("xT",) for c in range(16)})
    ALIAS.update({("hT", c): ("hT",) for c in range(16)})

    def _x(self, keys):
        out = []
        for k in keys:
            out.extend(self.ALIAS.get(k, (k,)))
        return out

    def op(self, eng, fns, reads=(), writes=()):
        if self.plan:
            return
        reads, writes = self._x(reads), self._x(writes)
        self._need(eng, self._deps(reads, writes))
        e = self.engs[eng]
        if callable(fns):
            fns = [fns]
        for f in fns[:-1]:
            f()
        ins = fns[-1]()
        e["cnt"] += 1
        ins.then_inc(e["sem"], 1)
        self._mark(reads, writes, (eng, e["cnt"]))

    def dma(self, q, out, in_, reads=(), writes=()):
        if self.plan:
            return
        reads, writes = self._x(reads), self._x(writes)
        p = self.dpools[q]
        idx = p["i"] % len(p["sems"])
        p["i"] += 1
        ds = p["sems"][idx]
        deps = self._deps(reads, writes)
        if ds["cnt"] > 0:
            deps.append(((q, idx), 16 * ds["cnt"]))
        self._need(q, deps)
        ins = self.engs[q]["obj"].dma_start(out=out, in_=in_)
        ds["cnt"] += 1
        ins.then_inc(ds["sem"], 16)
        self._mark(reads, writes, ((q, idx), 16 * ds["cnt"]))

    def barrier(self, full=False):
        if self.plan:
            return
        deps = [(n, e["cnt"]) for n, e in self.engs.items() if e["cnt"] > 0]
        for q, p in self.dpools.items():
            if q == "pool" and not full:
                continue
            for i, s in enumerate(p["sems"]):
                if s["cnt"] > 0:
                    deps.append(((q, i), 16 * s["cnt"]))
        if full:
            for i, c in enumerate(self.cc):
                if c["cnt"] > 0:
                    deps.append((("cc", i), c["cnt"]))
        for n in self.engs:
            if n == "pool" and not full:
                continue
            self._need(n, [d for d in deps if d[0] != n])

    def finish(self):
        self.barrier(full=True)


def build_program():
    nc = bass.Bass("TRN2", target_bir_lowering=False)

    def din(name, shape):
        if LITE and name in ("w_in", "w_out", "dense_w1", "dense_w3", "dense_w2", "moe_w1", "moe_w3", "moe_w2"):
            return nc.dram_tensor(name, [1] * (len(shape) - 2) + list(shape[-2:]), F32, kind="ExternalInput").ap()
        return nc.dram_tensor(name, list(shape), F32, kind="ExternalInput").ap()

    def dout(name, shape):
        return nc.dram_tensor(name, list(shape), F32, kind="ExternalOutput").ap()

    xin = din("xin", [T, D])
    st_ret = din("st_ret", [2, NB, 4, 128, 128])
    st_c31 = din("st_c31", [2, NB, 30, 512])
    st_c3 = din("st_c3", [2, NB, 2, 512])
    mix_norm_g = din("mix_norm_g", [2, D])
    w_in = din("w_in", [2, D, DIN])
    ret_norm_g = din("ret_norm_g", [2, 512])
    conv31_w = din("conv31_w", [2, 31, 512])
    conv31_b = din("conv31_b", [2, 512])
    conv_ln_g = din("conv_ln_g", [2, 512])
    conv_ln_b = din("conv_ln_b", [2, 512])
    sconv_w = din("sconv_w", [2, 3, 512])
    sgu_ln_g = din("sgu_ln_g", [2, 512])
    sgu_ln_b = din("sgu_ln_b", [2, 512])
    sgu_w = din("sgu_w", [2, 4, 128, 128])
    sgu_b = din("sgu_b", [2, 512])
    w_out = din("w_out", [2, D, D])
    ffn_norm_g = din("ffn_norm_g", [2, D])
    dense_w1 = din("dense_w1", [1, D, DFF])
    dense_w3 = din("dense_w3", [1, D, DFF])
    dense_w2 = din("dense_w2", [1, DFF, D])
    router_w = din("router_w", [D, NEXP])
    moe_w1 = din("moe_w1", [NEXP, D, DFF])
    moe_w3 = din("moe_w3", [NEXP, D, DFF])
    moe_w2 = din("moe_w2", [NEXP, DFF, D])
    final_norm_g = din("final_norm_g", [D])
    c_ident = din("c_ident", [128, 128])
    c_rope_c = din("c_rope_c", [128, T])
    c_rope_s = din("c_rope_s", [128, T])
    c_dmask = din("c_dmask", [128, 512])
    c_dmask_s = din("c_dmask_s", [64, 256])
    c_dq = din("c_dq", [128, 512])
    c_dq_s = din("c_dq_s", [128, 256])
    c_dkv = din("c_dkv", [128, 8])
    c_ind = din("c_ind", [64, NB])
    c_triu = din("c_triu", [128, 128])
    c_bm = din("c_bm", [64, 64])
    c_R = din("c_R", [4, 64])
    c_esel = din("c_esel", [8, 8 * 128])
    c_s0mask = din("c_s0mask", [128, 1])

    y_out = dout("y_out", [T, D])
    ret_p = dout("ret_p", [2, 4, 128, 128])
    ret_s = dout("ret_s", [2, NB, 4, 128, 128])
    c31_p = dout("c31_p", [2, 30, 512])
    c31_s = dout("c31_s", [2, NB, 30, 512])
    c3_p = dout("c3_p", [2, 2, 512])
    c3_s = dout("c3_s", [2, NB, 2, 512])
    vs_out = dout("vs_out", [2, NST, 512])

    xi = [nc.dram_tensor(f"xch_in{l}", [1024, 128], F32).ap() for l in range(2)]
    xg = [nc.dram_tensor(f"xch_all{l}", [2048, 128], F32).ap() for l in range(2)]

    es = ExitStack()
    with es:
        def sb(name, shape, dt=F32):
            return es.enter_context(nc.sbuf_tensor(name, list(shape), dt))

        xT = sb("xT", [128, 16, T])
        hT = sb("hT", [128, 16, T], BF16)
        mixT = sb("mixT", [128, 4, T], BF16)
        wsl = sb("wsl", [128, NBUF, SLOT], BF16)
        identf = sb("identf", [128, 128])
        identb = sb("identb", [128, 128], BF16)
        onesb = sb("onesb", [128, 128], BF16)
        rope_c = sb("rope_c", [128, T])
        rope_s = sb("rope_s", [128, T])
        dmask = sb("dmask", [128, 512])
        dmask_s = sb("dmask_s", [64, 256])
        dq = sb("dq", [128, 512])
        dq_s = sb("dq_s", [128, 256])
        dkv = sb("dkv", [128, 8])
        ind = sb("ind", [64, NB])
        triu = sb("triu", [128, 128])
        bm = sb("bm", [64, 64])
        Rm = sb("Rm", [4, 64])
        esel = sb("esel", [8, 8 * 128])
        s0mask = sb("s0mask", [128, 1])
        colT = sb("colT", [128, 512])
        epsc = sb("epsc", [128, 1])
        rw_t = sb("rw_t", [128, 16, 8])
        hx31 = sb("hx31", [128, 4, 30])
        hx3 = sb("hx3", [128, 4, 2])
        S0all = sb("S0all", [128, 4, 128])
        ARENA = 9728
        arena = sb("arena", [128, ARENA])
        arena_b = arena[:].bitcast(BF16)

        psA = es.enter_context(nc.psum_tensor("psA", [128, 3072], F32))
        psS = es.enter_context(nc.psum_tensor("psS", [128, 512], F32))
        psB = es.enter_context(nc.psum_tensor("psB", [128, 1024], BF16))

        S = Sync(nc, es)
        ROWBASE = {"RA": 0, "RB": 1536}

        def fa(off, n):
            assert off + n <= ARENA
            return arena[:, off:off + n]

        def ba(off_words, n):
            assert off_words * 2 + n <= 2 * ARENA
            return arena_b[:, off_words * 2: off_words * 2 + n]

        class WStream:
            def __init__(self):
                self.blocks = []
                self.i = 0
                self.issued = 0

            def reset(self):
                self.i = 0
                self.issued = 0

            def _issue(self, j):
                slot = j % NBUF
                for (a, b, c0, n, src) in self.blocks[j]:
                    dst = wsl[:, slot, 0:a * b].rearrange("p (a b) -> p a b", a=a)[:, :, c0:c0 + n]
                    S.dma("pool", dst, src, reads=[], writes=[("w", slot)])

            def get(self, parts):
                if S.plan:
                    self.blocks.append(parts)
                    self.i += 1
                    return (self.i - 1) % NBUF
                j = self.i
                while self.issued < min(len(self.blocks), j + NBUF - 1):
                    self._issue(self.issued)
                    self.issued += 1
                self.i += 1
                return j % NBUF

        W = WStream()

        def wview(slot, off, a, b):
            return wsl[:, slot, off:off + a * b].rearrange("p (a b) -> p a b", a=a)

        def win_cols(l, c0, n):
            return w_in[l, :, c0:c0 + n].rearrange("(kc p) n -> p kc n", p=128)

        def proj_fm(slot, coff, row, ncols=128, kdim=16, act=None):
            base = ROWBASE[row]
            src = act if act is not None else hT
            wv = wview(slot, 0, 16, 256) if kdim == 16 else None
            fns = []
            for kc in range(kdim):
                for (t0, tn) in TT:
                    fns.append(lambda kc=kc, t0=t0, tn=tn: nc.tensor.matmul(
                        psA[0:ncols, base + t0: base + t0 + tn], lhsT=wv[:, kc, coff:coff + ncols],
                        rhs=src[:, kc, t0:t0 + tn], start=(kc == 0), stop=(kc == kdim - 1)))
            S.op("pe", fns, reads=[("w", slot), "hT"], writes=[row])

        def prow(row, n=T, p=128):
            base = ROWBASE[row]
            return psA[0:p, base:base + n]

        def stat_bcast(src_bf_rows, keys, row):
            base = ROWBASE[row]
            fns = []
            n = len(src_bf_rows)
            for k, r in enumerate(src_bf_rows):
                for (t0, tn) in TT:
                    fns.append(lambda k=k, r=r, t0=t0, tn=tn: nc.tensor.matmul(
                        psA[:, base + t0: base + t0 + tn], lhsT=onesb[:], rhs=r[:, t0:t0 + tn],
                        start=(k == 0), stop=(k == n - 1)))
            S.op("pe", fns, reads=list(keys), writes=[row])

        def rsqrt_row(dst, src, scale, key_dst, key_src):
            S.op("act", lambda: nc.scalar.activation(out=dst, in_=src, func=AF.Sqrt, bias=epsc[:], scale=scale),
                 reads=[key_src], writes=[key_dst])
            S.op("dve", lambda: nc.vector.reciprocal(out=dst, in_=dst), reads=[key_dst], writes=[key_dst])

        def load_consts():
            for dst, src in [(identf, c_ident), (rope_c, c_rope_c), (rope_s, c_rope_s), (dmask, c_dmask),
                             (dmask_s, c_dmask_s), (dq, c_dq), (dq_s, c_dq_s), (dkv, c_dkv), (ind, c_ind),
                             (triu, c_triu), (bm, c_bm), (Rm, c_R), (esel, c_esel), (s0mask, c_s0mask)]:
                S.dma("sp", dst[:], src, writes=["const"])
            S.op("dve", lambda: nc.vector.tensor_copy(out=identb[:], in_=identf[:]), reads=["const"], writes=["identb"])
            S.op("dve", lambda: nc.vector.memset(onesb[:], 1.0), writes=["onesb"])
            S.op("dve", lambda: nc.vector.memset(epsc[:], EPS), writes=["epsc"])
            stg = fa(0, 512).rearrange("p (a b) -> p a b", a=4)
            S.op("dve", lambda: nc.vector.memset(fa(0, 512), 0.0), writes=["stg"])
            rows = {}

            def put(tile, r0, name, src_rows):
                n = src_rows.shape[0]
                S.dma("sp", stg[r0:r0 + n, tile, :], src_rows, reads=[], writes=["stg"])
                rows[name] = tile * 128 + r0
                return r0 + n

            r = 0
            for l in range(2):
                tile = l
                r = 0
                r = put(tile, r, ("mixg", l), mix_norm_g[l].rearrange("(c p) -> c p", p=128))
                r = put(tile, r, ("ffng", l), ffn_norm_g[l].rearrange("(c p) -> c p", p=128))
                r = put(tile, r, ("retg", l), ret_norm_g[l].rearrange("(c p) -> c p", p=128))
                r = put(tile, r, ("c31b", l), conv31_b[l].rearrange("(c p) -> c p", p=128))
                r = put(tile, r, ("clng", l), conv_ln_g[l].rearrange("(c p) -> c p", p=128))
                r = put(tile, r, ("clnb", l), conv_ln_b[l].rearrange("(c p) -> c p", p=128))
                r = put(tile, r, ("scw", l), sconv_w[l].rearrange("j (c p) -> (j c) p", p=128))
                if l == 0:
                    r = put(tile, r, ("fing",), final_norm_g.rearrange("(c p) -> c p", p=128))
            for l in range(2):
                put(2 + l, 0, ("c31w", l), conv31_w[l].rearrange("j (c p) -> (j c) p", p=128))
            fns = [lambda t=t: nc.tensor.transpose(psS[:, t * 128:(t + 1) * 128], stg[:, t, :], identf[:])
                   for t in range(4)]
            S.op("pe", fns, reads=["stg", "const"], writes=["psS"])
            S.op("dve", lambda: nc.vector.tensor_copy(out=colT[:], in_=psS[:]), reads=["psS"], writes=["colT"])
            S.dma("sp", rw_t[:], router_w.rearrange("(c p) e -> p c e", p=128), writes=["rw"])
            for c in range(16):
                j = rows[("ffng", 1)] + c
                S.op("dve", lambda c=c, j=j: nc.vector.tensor_scalar(out=rw_t[:, c, :], in0=rw_t[:, c, :], scalar1=colT[:, j:j + 1],
                                                                     scalar2=None, op0=ALU.mult), reads=["rw", "colT"], writes=["rw"])
            return rows

        def col(rows, name, i=0):
            j = rows[name] + i
            return colT[:, j:j + 1]

        def load_x():
            for i in range(9):
                n = 128 if i < 8 else 64
                stage = fa(512 + (i % 2) * 2048, 2048)
                S.dma("sp", stage[0:n, :], xin[i * 128:i * 128 + n, :], writes=[("xst", i % 2)])
                for g in range(4):
                    bank = ROWBASE["RA"] + (g % 2) * 512
                    key = "pb%d" % (g % 2)
                    fns = [lambda j=j, g=g: nc.tensor.transpose(
                        psA[:, bank + j * 128: bank + j * 128 + n], stage[0:n, (g * 4 + j) * 128:(g * 4 + j + 1) * 128],
                        identf[0:n, 0:n]) for j in range(4)]
                    S.op("pe", fns, reads=[("xst", i % 2), "const"], writes=[key])
                    src = psA[:, bank:bank + 512].rearrange("p (a b) -> p a b", a=4)[:, :, 0:n]
                    S.op("dve" if g % 2 == 0 else "act",
                         (lambda src=src, g=g: nc.vector.tensor_copy(out=xT[:, g * 4:(g + 1) * 4, i * 128:i * 128 + n], in_=src))
                         if g % 2 == 0 else
                         (lambda src=src, g=g: nc.scalar.copy(out=xT[:, g * 4:(g + 1) * 4, i * 128:i * 128 + n], in_=src)),
                         reads=[key], writes=["xT"])

        def rmsnorm_to_hT(gname, rows, gi=0):
            sq = [ba(0, T), ba(T // 2, T)]
            rstd = fa(T, T)
            for c in range(16):
                b = sq[c % 2]
                S.op("act", lambda c=c, b=b: nc.scalar.activation(out=b, in_=xT[:, c, :], func=AF.Square),
                     reads=[("xT", c)], writes=[("sq", c % 2)])
                base = ROWBASE["RA"]
                fns = [lambda c=c, b=b, t0=t0, tn=tn: nc.tensor.matmul(
                    psA[:, base + t0: base + t0 + tn], lhsT=onesb[:], rhs=b[:, t0:t0 + tn],
                    start=(c == 0), stop=(c == 15)) for (t0, tn) in TT]
                S.op("pe", fns, reads=[("sq", c % 2), "onesb"], writes=["RA"])
            dbg_stop("n_a")
            rsqrt_row(rstd, prow("RA"), 1.0 / D, "rstd", "RA")
            dbg_stop("n_b")
            for c in range(16):
                S.op("dve", lambda c=c: nc.vector.scalar_tensor_tensor(
                    out=hT[:, c, :], in0=xT[:, c, :], scalar=col(rows, gname, gi + c), in1=rstd,
                    op0=ALU.mult, op1=ALU.mult), reads=[("xT", c), "rstd", "colT"], writes=[("hT", c)])

        def accum_out(slots, src_rows, src_key, kdim_per_slot=2):
            nk = len(slots) * kdim_per_slot
            for dm in range(16):
                row = "RA" if dm % 2 == 0 else "RB"
                base = ROWBASE[row]
                fns = []
                for k in range(nk):
                    wv = wview(slots[k // kdim_per_slot], 0, kdim_per_slot, 2048)
                    for (t0, tn) in TT:
                        fns.append(lambda k=k, wv=wv, t0=t0, tn=tn, dm=dm, base=base: nc.tensor.matmul(
                            psA[:, base + t0: base + t0 + tn],
                            lhsT=wv[:, k % kdim_per_slot, dm * 128:(dm + 1) * 128],
                            rhs=src_rows[k][:, t0:t0 + tn], start=(k == 0), stop=(k == nk - 1)))
                wkeys = [("w", s) for s in slots]
                if isinstance(src_key, list) and dm == 0:
                    nsplit = 3 * (nk - 1)
                    S.op("pe", fns[:nsplit], reads=wkeys + src_key[:nk - 1], writes=[row])
                    S.op("pe", fns[nsplit:], reads=wkeys + [src_key[nk - 1]], writes=[row])
                else:
                    S.op("pe", fns, reads=wkeys + (src_key if isinstance(src_key, list) else [src_key]), writes=[row])
                S.op("dve", lambda dm=dm, row=row: nc.vector.tensor_tensor(
                    out=xT[:, dm, :], in0=prow(row), in1=xT[:, dm, :], op=ALU.add),
                    reads=[row, ("xT", dm)], writes=[("xT", dm)])

        def wout_slots(l, m):
            sl = []
            for s in range(2):
                r0 = m * 512 + s * 256
                src = w_out[l, r0:r0 + 256, :].rearrange("(kc p) n -> p kc n", p=128)
                sl.append(W.get([(2, 2048, 0, 2048, src)]))
            return sl

        def rope_proj(slot, dst, dkey, f1, oT):
            wall = wview(slot, 0, 16, 256)
            wx = wall[:, :, 0:128]
            wlo = wall[:, :, 128:192]
            whi = wall[:, :, 192:256]
            for row, parts in (("RA", [(wx, 0, 128)]), ("RB", [(wlo, 0, 64), (whi, 64, 64)])):
                base = ROWBASE[row]
                fns = []
                for (wv, m0, mn) in parts:
                    for kc in range(16):
                        for (t0, tn) in TT:
                            fns.append(lambda wv=wv, m0=m0, mn=mn, kc=kc, t0=t0, tn=tn, base=base: nc.tensor.matmul(
                                psA[m0:m0 + mn, base + t0: base + t0 + tn], lhsT=wv[:, kc, :],
                                rhs=hT[:, kc, t0:t0 + tn], start=(kc == 0), stop=(kc == 15)))
                S.op("pe", fns, reads=[("w", slot), "hT"], writes=[row])
            S.op("dve", lambda: nc.vector.tensor_tensor(out=f1, in0=prow("RA"), in1=rope_c[:], op=ALU.mult),
                 reads=["RA", "const"], writes=["f1"])
            S.op("dve", lambda: nc.vector.tensor_tensor(out=oT, in0=prow("RB"), in1=rope_s[:], op=ALU.mult),
                 reads=["RB", "const"], writes=["oTtmp"])
            S.op("dve", lambda: nc.vector.tensor_tensor(out=dst, in0=f1, in1=oT, op=ALU.add),
                 reads=["f1", "oTtmp"], writes=[dkey])

        def pre_pass(l):
            f1 = fa(0, T)
            oT = fa(T, T)
            kT = ba(2 * T, T)
            ktok = ba(2 * T + T // 2, 1152).rearrange("p (a b) -> p a b", a=9)
            vtt = ba(2 * T + T // 2 + 576, 1152).rearrange("p (a b) -> p a b", a=9)
            o_sm = 2 * T + T // 2 + 1152
            Sx = fa(o_sm, 128)
            gl = fa(o_sm + 128, 128)
            gd = fa(o_sm + 256, 128)
            t1 = fa(o_sm + 384, 128)
            t2 = fa(o_sm + 512, 128)
            S.barrier()
            for h in range(4):
                kc_ = 1 * 512 + h * 128
                vc = 2 * 512 + h * 128
                s_k = W.get([(16, 256, 0, 128, win_cols(l, kc_, 128)),
                             (16, 256, 128, 64, win_cols(l, kc_ + 64, 64)),
                             (16, 256, 192, 64, win_cols(l, kc_, 64))])
                rope_proj(s_k, kT, "kT", f1, oT)
                s_v = W.get([(16, 256, 0, 128, win_cols(l, vc, 128))])
                wv_v = wview(s_v, 0, 16, 256)
                for grp in range(2):
                    base = ROWBASE["RB"]
                    fns = []
                    for j in range(4):
                        i = grp * 4 + j
                        for kc in range(16):
                            fns.append(lambda j=j, i=i, kc=kc: nc.tensor.matmul(
                                psA[:, base + j * 128: base + (j + 1) * 128], lhsT=hT[:, kc, i * 128:(i + 1) * 128],
                                rhs=wv_v[:, kc, 0:128], start=(kc == 0), stop=(kc == 15)))
                    S.op("pe", fns, reads=[("w", s_v), "hT"], writes=["RB"])
                    for j in range(4):
                        i = grp * 4 + j
                        S.op("dve", lambda j=j, i=i: nc.vector.tensor_scalar(
                            out=vtt[:, i, :], in0=psA[:, base + j * 128: base + (j + 1) * 128], scalar1=dkv[:, h:h + 1],
                            scalar2=float(GAM[h] ** (128 * (7 - i))), op0=ALU.mult, op1=ALU.mult),
                            reads=["RB", "const"], writes=["vtt"])
                    fns = [lambda j=j, grp=grp: nc.tensor.transpose(
                        psB[:, j * 128:(j + 1) * 128], kT[:, (grp * 4 + j) * 128:(grp * 4 + j + 1) * 128], identb[:])
                        for j in range(4)]
                    S.op("pe", fns, reads=["kT", "identb"], writes=["psB"])
                    S.op("act", lambda grp=grp: nc.scalar.copy(
                        out=ktok[:, grp * 4:grp * 4 + 4, :], in_=psB[:, 0:512].rearrange("p (a b) -> p a b", a=4)),
                        reads=["psB"], writes=["ktok"])
                fns = [lambda c=c: nc.tensor.matmul(psS[:, 0:128], lhsT=ktok[:, c, :], rhs=vtt[:, c, :],
                                                    start=(c == 0), stop=(c == 7)) for c in range(8)]
                S.op("pe", fns, reads=["ktok", "vtt"], writes=["psS"])
                S.op("act", lambda: nc.scalar.copy(out=Sx, in_=psS[:, 0:128]), reads=["psS"], writes=["Sx"])
                S.dma("sp", xi[l][h * 128:(h + 1) * 128, :], Sx, reads=["Sx"], writes=[("xi", l)])
            lc = slice(NPT - 128, NPT)
            for ct in range(4):
                s_b = W.get([(16, 256, 0, 128, win_cols(l, 2048 + ct * 128, 128)),
                             (16, 256, 128, 128, win_cols(l, 2560 + ct * 128, 128))])
                s_c = W.get([(16, 256, 0, 128, win_cols(l, 3584 + ct * 128, 128)),
                             (16, 256, 128, 128, win_cols(l, 4096 + ct * 128, 128))])
                fns = []
                for q_, sl_ in enumerate((s_b, s_b, s_c, s_c)):
                    wv = wview(sl_, 0, 16, 256)
                    for kc in range(16):
                        fns.append(lambda q_=q_, wv=wv, kc=kc: nc.tensor.matmul(
                            psS[:, q_ * 128:(q_ + 1) * 128], lhsT=wv[:, kc, (q_ % 2) * 128:(q_ % 2) * 128 + 128],
                            rhs=hT[:, kc, lc], start=(kc == 0), stop=(kc == 15)))
                S.op("pe", fns, reads=[("w", s_b), ("w", s_c), "hT"], writes=["psS"])
                S.op("act", lambda: nc.scalar.activation(out=t1, in_=psS[:, 128:256], func=AF.Sigmoid), reads=["psS"], writes=["t1"])
                S.op("act", lambda: nc.scalar.copy(out=t2, in_=psS[:, 256:384]), reads=["psS"], writes=["t2"])
                S.op("dve", lambda: nc.vector.tensor_tensor(out=gl, in0=psS[:, 0:128], in1=t1, op=ALU.mult),
                     reads=["psS", "t1"], writes=["gl"])
                S.op("dve", lambda: nc.vector.tensor_tensor(out=gd, in0=psS[:, 384:512], in1=t2, op=ALU.mult),
                     reads=["psS", "t2"], writes=["gd"])
                S.dma("sp", xi[l][512 + ct * 128:512 + (ct + 1) * 128, 0:30], gl[:, 98:128], reads=["gl"], writes=[("xi", l)])
                S.dma("sp", xi[l][512 + ct * 128:512 + (ct + 1) * 128, 32:34], gd[:, 126:128], reads=["gd"], writes=[("xi", l)])
            S.coll(l, lambda: nc.gpsimd.collective_compute(
                "AllGather", ALU.bypass, replica_groups=[[0, 1], [2, 3], [4, 5], [6, 7]], ins=[xi[l]], outs=[xg[l]]),
                reads=[("xi", l)], writes=[("xg", l)])

        def load_xch(l):
            if XCH:
                S.dma("sp", S0all[:], xg[l][0:512, :].rearrange("(h p) e -> p h e", p=128), reads=[("xg", l)], writes=["S0all"])
                S.dma("sp", hx31[:], xg[l][512:1024, 0:30].rearrange("(c p) r -> p c r", p=128), reads=[("xg", l)], writes=["hx31"])
                S.dma("sp", hx3[:], xg[l][512:1024, 32:34].rearrange("(c p) r -> p c r", p=128), reads=[("xg", l)], writes=["hx3"])
                for t_, k_ in ((S0all, "S0all"), (hx31, "hx31"), (hx3, "hx3")):
                    S.op("dve", lambda t_=t_: nc.vector.tensor_scalar(out=t_[:], in0=t_[:], scalar1=s0mask[:, 0:1], scalar2=None, op0=ALU.mult),
                         reads=[k_, "const"], writes=[k_])
            else:
                for t_, k_ in ((S0all, "S0all"), (hx31, "hx31"), (hx3, "hx3")):
                    S.op("dve", lambda t_=t_: nc.vector.memset(t_[:], 0.0), writes=[k_])

        def mixer_A(l, rows):
            o_f1 = 0
            o_oT = T
            o_q = 2 * T
            o_k = o_q + T // 2
            o_g = o_k + T // 2
            o_kt = o_g + T // 2
            o_v = o_kt + 576
            o_vt = o_v + 576
            o_sm = o_vt + 576
            f1 = fa(o_f1, T)
            oT = fa(o_oT, T)
            qT = ba(o_q, T)
            kT = ba(o_k, T)
            gS = ba(o_g, T)
            ktok = ba(o_kt, 1152).rearrange("p (a b) -> p a b", a=9)
            vtk = ba(o_v, 1152).rearrange("p (a b) -> p a b", a=9)
            vtt = ba(o_vt, 1152).rearrange("p (a b) -> p a b", a=9)
            PT = ba(o_sm, 128)
            Sb = ba(o_sm + 64, 128)
            Sf = fa(o_sm + 128, 128)
            crt = fa(o_sm + 256, 128)
            Ss16 = fa(o_sm + 384, 2048).rearrange("p (a b) -> p a b", a=16)
            Ssb = ba(o_sm + 2432, 512).rearrange("p (a b) -> p a b", a=4)
            Sso2 = [fa(o_sm + 2688 + k * 512, 512).rearrange("p (a b) -> p a b", a=4) for k in range(2)]
            vm = ba(o_sm + 3712, 512).rearrange("p (a b) -> p a b", a=4)
            assert o_sm + 3968 <= ARENA
            S.barrier()
            for h in range(4):
                qc = 0 * 512 + h * 128
                kc_ = 1 * 512 + h * 128
                vc = 2 * 512 + h * 128
                gc = 3 * 512 + h * 128
                for bg in range(4):
                    S.dma("sp", Ss16[:, bg * 4:(bg + 1) * 4, :], st_ret[l, bg * 4:(bg + 1) * 4, h].rearrange("b d e -> d b e"),
                          writes=[("Ss", bg)])

                def proj_qk(slot, dst, dkey):
                    wall = wview(slot, 0, 16, 256)
                    wx = wall[:, :, 0:128]
                    wlo = wall[:, :, 128:192]
                    whi = wall[:, :, 192:256]
                    for row, parts in (("RA", [(wx, 0, 128)]), ("RB", [(wlo, 0, 64), (whi, 64, 64)])):
                        base = ROWBASE[row]
                        fns = []
                        for (wv, m0, mn) in parts:
                            for kc in range(16):
                                for (t0, tn) in TT:
                                    fns.append(lambda wv=wv, m0=m0, mn=mn, kc=kc, t0=t0, tn=tn, base=base: nc.tensor.matmul(
                                        psA[m0:m0 + mn, base + t0: base + t0 + tn], lhsT=wv[:, kc, :],
                                        rhs=hT[:, kc, t0:t0 + tn], start=(kc == 0), stop=(kc == 15)))
                        S.op("pe", fns, reads=[("w", slot), "hT"], writes=[row])
                    S.op("dve", lambda: nc.vector.tensor_tensor(out=f1, in0=prow("RA"), in1=rope_c[:], op=ALU.mult),
                         reads=["RA", "const"], writes=["f1"])
                    S.op("dve", lambda: nc.vector.tensor_tensor(out=oT, in0=prow("RB"), in1=rope_s[:], op=ALU.mult),
                         reads=["RB", "const"], writes=["oTtmp"])
                    S.op("dve", lambda: nc.vector.tensor_tensor(out=dst, in0=f1, in1=oT, op=ALU.add),
                         reads=["f1", "oTtmp"], writes=[dkey])

                s_q = W.get([(16, 256, 0, 128, win_cols(l, qc, 128)),
                             (16, 256, 128, 64, win_cols(l, qc + 64, 64)),
                             (16, 256, 192, 64, win_cols(l, qc, 64))])
                proj_qk(s_q, qT, "qT")
                s_k = W.get([(16, 256, 0, 128, win_cols(l, kc_, 128)),
                             (16, 256, 128, 64, win_cols(l, kc_ + 64, 64)),
                             (16, 256, 192, 64, win_cols(l, kc_, 64))])
                proj_qk(s_k, kT, "kT")
                s_vg = W.get([(16, 256, 0, 128, win_cols(l, vc, 128)), (16, 256, 128, 128, win_cols(l, gc, 128))])
                wv_vg = wview(s_vg, 0, 16, 256)
                base = ROWBASE["RA"]
                fns = []
                for kc in range(16):
                    for (t0, tn) in TT:
                        fns.append(lambda kc=kc, t0=t0, tn=tn: nc.tensor.matmul(
                            psA[:, base + t0: base + t0 + tn], lhsT=wv_vg[:, kc, 128:256],
                            rhs=hT[:, kc, t0:t0 + tn], start=(kc == 0), stop=(kc == 15)))
                S.op("pe", fns, reads=[("w", s_vg), "hT"], writes=["RA"])
                S.op("act", lambda: nc.scalar.activation(out=gS, in_=prow("RA"), func=AF.Silu),
                     reads=["RA"], writes=["gS"])
                for grp in range(3):
                    tiles = [i for i in range(grp * 4, min(9, grp * 4 + 4))]
                    base = ROWBASE["RB"]
                    fns = []
                    for j, i in enumerate(tiles):
                        n = 128 if i < 8 else 64
                        for kc in range(16):
                            fns.append(lambda j=j, i=i, n=n, kc=kc: nc.tensor.matmul(
                                psA[0:n, base + j * 128: base + (j + 1) * 128], lhsT=hT[:, kc, i * 128:i * 128 + n],
                                rhs=wv_vg[:, kc, 0:128], start=(kc == 0), stop=(kc == 15)))
                    S.op("pe", fns, reads=[("w", s_vg), "hT"], writes=["RB"])
                    nt = len(tiles)
                    n = 128 if grp < 2 else 64
                    src = psA[0:n, base:base + nt * 128].rearrange("p (a b) -> p a b", a=nt)
                    S.op("act", lambda src=src, n=n, grp=grp, nt=nt: nc.scalar.copy(
                        out=vtk[0:n, grp * 4:grp * 4 + nt, :], in_=src), reads=["RB"], writes=["vtk"])
                    dcol = dkv[0:n, h:h + 1] if grp < 2 else dkv[0:n, 4 + h:5 + h]
                    S.op("dve", lambda src=src, n=n, grp=grp, nt=nt, dcol=dcol: nc.vector.tensor_scalar(
                        out=vtt[0:n, grp * 4:grp * 4 + nt, :], in0=src, scalar1=dcol, scalar2=None, op0=ALU.mult),
                        reads=["RB", "const"], writes=["vtt"])
                for grp in range(3):
                    tiles = [i for i in range(grp * 4, min(9, grp * 4 + 4))]
                    n = 128 if grp < 2 else 64
                    fns = [lambda j=j, i=i, n=n: nc.tensor.transpose(
                        psB[0:n, j * 128:(j + 1) * 128], kT[:, i * 128:i * 128 + n], identb[:])
                        for j, i in enumerate(tiles)]
                    S.op("pe", fns, reads=["kT", "identb"], writes=["psB"])
                    nt = len(tiles)
                    src = psB[0:n, 0:nt * 128].rearrange("p (a b) -> p a b", a=nt)
                    S.op("act", lambda src=src, n=n, grp=grp, nt=nt: nc.scalar.copy(
                        out=ktok[0:n, grp * 4:grp * 4 + nt, :], in_=src), reads=["psB"], writes=["ktok"])
                dbg_stop("A_proj")
                S.op("dve", lambda: nc.vector.tensor_copy(out=Sf, in_=S0all[:, h, :]), reads=["S0all"], writes=["Sf"])
                S.op("act", lambda: nc.scalar.copy(out=Sb, in_=Sf), reads=["Sf"], writes=["Sb"])
                gC = GAM[h] ** 128
                for c in range(8):
                    cs = slice(c * 128, (c + 1) * 128)
                    S.op("pe", lambda cs=cs: nc.tensor.matmul(psS[:, 0:128], lhsT=kT[:, cs], rhs=qT[:, cs],
                                                              start=True, stop=True),
                         reads=["kT", "qT"], writes=["psS"])
                    S.op("dve", lambda: nc.vector.tensor_tensor(out=PT, in0=psS[:, 0:128], in1=dmask[:, h * 128:(h + 1) * 128],
                                                                op=ALU.mult), reads=["psS", "const"], writes=["PT"])
                    S.op("pe", lambda c=c: nc.tensor.matmul(psA[:, 1536:1664], lhsT=vtk[:, c, :], rhs=PT, start=True, stop=True),
                         reads=["vtk", "PT"], writes=["pb3"])
                    S.op("pe", lambda cs=cs: nc.tensor.matmul(psA[:, 2048:2176], lhsT=Sb, rhs=qT[:, cs], start=True, stop=True),
                         reads=["Sb", "qT"], writes=["pb4"])
                    S.op("pe", lambda c=c: nc.tensor.matmul(psA[:, 2560:2688], lhsT=ktok[:, c, :], rhs=vtt[:, c, :],
                                                            start=True, stop=True),
                         reads=["ktok", "vtt"], writes=["pb5"])
                    S.op("dve", lambda: nc.vector.tensor_tensor(out=crt, in0=psA[:, 2048:2176], in1=dq[:, h * 128:(h + 1) * 128],
                                                                op=ALU.mult), reads=["pb4", "const"], writes=["crt"])
                    S.op("dve", lambda cs=cs: nc.vector.tensor_tensor(out=oT[:, cs], in0=psA[:, 1536:1664], in1=crt, op=ALU.add),
                         reads=["pb3", "crt", "oTtmp"], writes=["oT"])
                    S.op("dve", lambda: nc.vector.scalar_tensor_tensor(out=Sf, in0=Sf, scalar=float(gC), in1=psA[:, 2560:2688],
                                                                       op0=ALU.mult, op1=ALU.add),
                         reads=["pb5", "Sf"], writes=["Sf"])
                    S.op("act", lambda: nc.scalar.copy(out=Sb, in_=Sf), reads=["Sf"], writes=["Sb"])
                S.dma("sp", ret_p[l, h], Sf, reads=["Sf"], writes=[("o_retp", l, h)])
                sc = slice(NPT, T)
                S.op("pe", lambda: nc.tensor.matmul(psS[0:64, 0:64], lhsT=kT[:, sc], rhs=qT[:, sc], start=True, stop=True),
                     reads=["kT", "qT"], writes=["psS"])
                S.op("dve", lambda: nc.vector.tensor_tensor(out=PT[0:64, 0:64], in0=psS[0:64, 0:64],
                                                            in1=dmask_s[:, h * 64:(h + 1) * 64], op=ALU.mult),
                     reads=["psS", "const"], writes=["PT"])
                S.op("pe", lambda: nc.tensor.matmul(psA[:, 1536:1600], lhsT=vtk[0:64, 8, :], rhs=PT[0:64, 0:64],
                                                    start=True, stop=True), reads=["vtk", "PT"], writes=["pb3"])
                g4 = GAM[h] ** 4
                for bg in range(4):
                    Ss = Ss16[:, bg * 4:(bg + 1) * 4, :]
                    Sso = Sso2[bg % 2]
                    S.op("act", lambda Ss=Ss: nc.scalar.copy(out=Ssb[:], in_=Ss), reads=[("Ss", bg)], writes=["Ssb"])
                    fns = [lambda j=j, bg=bg: nc.tensor.matmul(
                        psA[:, 2048 + (bg * 4 + j) * 4: 2048 + (bg * 4 + j) * 4 + 4], lhsT=Ssb[:, j, :],
                        rhs=qT[:, NPT + (bg * 4 + j) * 4: NPT + (bg * 4 + j) * 4 + 4], start=True, stop=True)
                        for j in range(4)]
                    S.op("pe", fns, reads=["Ssb", "qT"], writes=["pb4"])
                    for j in range(4):
                        b = bg * 4 + j
                        S.op("dve", lambda j=j, b=b: nc.vector.tensor_scalar(
                            out=vm[0:64, j, :], in0=vtt[0:64, 8, :], scalar1=ind[:, b:b + 1], scalar2=None, op0=ALU.mult),
                            reads=["vtt", "const"], writes=["vm"])
                    base = ROWBASE["RA"]
                    fns = [lambda j=j: nc.tensor.matmul(psA[:, base + j * 128: base + (j + 1) * 128],
                                                        lhsT=ktok[0:64, 8, :], rhs=vm[0:64, j, :], start=True, stop=True)
                           for j in range(4)]
                    S.op("pe", fns, reads=["ktok", "vm"], writes=["RA"])
                    S.op("dve", lambda Ss=Ss, Sso=Sso: nc.vector.scalar_tensor_tensor(
                        out=Sso[:], in0=Ss, scalar=float(g4),
                        in1=psA[:, base:base + 512].rearrange("p (a b) -> p a b", a=4), op0=ALU.mult, op1=ALU.add),
                        reads=["RA", ("Ss", bg)], writes=[("Sso", bg % 2)])
                    S.dma("sp", ret_s[l, bg * 4:(bg + 1) * 4, h].rearrange("b d e -> d b e"), Sso[:],
                          reads=[("Sso", bg % 2)], writes=[("o_rets", l, h, bg)])
                S.op("dve", lambda: nc.vector.tensor_tensor(out=crt[:, 0:64], in0=psA[:, 2048:2112],
                                                            in1=dq_s[:, h * 64:(h + 1) * 64], op=ALU.mult),
                     reads=["pb4", "const"], writes=["crt"])
                S.op("dve", lambda: nc.vector.tensor_tensor(out=oT[:, sc], in0=psA[:, 1536:1600], in1=crt[:, 0:64], op=ALU.add),
                     reads=["pb3", "crt"], writes=["oT"])
                dbg_stop("A_chunks")
                osq = qT
                S.op("act", lambda: nc.scalar.activation(out=osq, in_=oT, func=AF.Square), reads=["oT", "qT"], writes=["qT"])
                stat_bcast([osq], ["qT", "onesb"], "RB")
                rsqrt_row(f1, prow("RB"), 1.0 / 128, "f1", "RB")
                S.op("dve", lambda: nc.vector.scalar_tensor_tensor(out=oT, in0=oT, scalar=col(rows, ("retg", l), h), in1=f1,
                                                                   op0=ALU.mult, op1=ALU.mult),
                     reads=["oT", "f1", "colT"], writes=["oT"])
                S.op("dve", lambda: nc.vector.tensor_tensor(out=mixT[:, h, :], in0=oT, in1=gS, op=ALU.mult),
                     reads=["oT", "gS"], writes=["mixT", "oTtmp"])
            sl = wout_slots(l, 0)
            accum_out(sl, [mixT[:, k, :] for k in range(4)], "mixT")

        def tm_rows(srcT_fn, ct, tstage, key):
            for which, (c0, n) in enumerate([(NPT - 128, 128), (NPT, 64)]):
                S.op("pe", lambda c0=c0, n=n, which=which: nc.tensor.transpose(
                    psS[0:n, which * 128: which * 128 + 128], srcT_fn(c0, n), identf[:]),
                    reads=[key, "const"], writes=["psS"])
                S.op("act", lambda n=n, which=which: nc.scalar.copy(
                    out=tstage[0:n, which, ct * 128:(ct + 1) * 128], in_=psS[0:n, which * 128: which * 128 + 128]),
                    reads=["psS"], writes=["tstage"])

        def mixer_B(l, rows):
            EXT = 30 + NPT + NB * 34
            o_ext = 0
            o_conv = 1600
            o_f1 = o_conv + 4 * T
            o_b1 = o_f1 + T
            o_hst = o_b1 + T
            ext = fa(o_ext, EXT)
            conv = fa(o_conv, 4 * T).rearrange("p (a b) -> p a b", a=4)
            f1 = fa(o_f1, T)
            b1 = ba(o_b1, T)
            b2 = ba(o_b1 + T // 2, T)
            hst = fa(o_hst, 512)
            tstage = fa(o_hst + 512, 1024).rearrange("p (a b) -> p a b", a=2)
            assert o_hst + 1536 <= ARENA
            S.barrier()
            ext_p = ext[:, 0:30 + NPT]
            ext_s = ext[:, 30 + NPT:EXT].rearrange("p (b r) -> p b r", b=NB)
            S.dma("act", c31_s[l, :, 0:26, :], st_c31[l, :, 4:30, :], writes=[("o_c31s_h", l)])
            for ct in range(4):
                s_b = W.get([(16, 256, 0, 128, win_cols(l, 2048 + ct * 128, 128)),
                             (16, 256, 128, 128, win_cols(l, 2560 + ct * 128, 128))])
                proj_fm(s_b, 0, "RA")
                proj_fm(s_b, 128, "RB")
                S.op("act", lambda: nc.scalar.activation(out=f1, in_=prow("RB"), func=AF.Sigmoid), reads=["RB"], writes=["f1"])
                S.op("dve", lambda: nc.vector.tensor_copy(out=ext[:, 0:30], in_=hx31[:, ct, :]), reads=["hx31"], writes=["ext"])
                hst4 = hst.rearrange("p (a b) -> p a b", a=4)
                for q4 in range(4):
                    S.dma("sp", hst4[0:120, q4, :],
                          st_c31[l, q4 * 4:(q4 + 1) * 4].rearrange("b r c -> (b r) c")[:, ct * 128:(ct + 1) * 128],
                          writes=[("hst", q4)])
                for q4 in range(4):
                    S.op("pe", lambda q4=q4: nc.tensor.transpose(psS[:, 256:376], hst4[0:120, q4, :], identf[0:120, 0:120]),
                         reads=[("hst", q4), "const"], writes=["psS"])
                    S.op("act", lambda q4=q4: nc.scalar.copy(
                        out=ext_s[:, q4 * 4:(q4 + 1) * 4, 0:30], in_=psS[:, 256:376].rearrange("p (b r) -> p b r", b=4)),
                        reads=["psS"], writes=["ext"])
                S.op("dve", lambda: nc.vector.tensor_tensor(out=ext_p[:, 30:30 + NPT], in0=prow("RA", NPT), in1=f1[:, 0:NPT], op=ALU.mult),
                     reads=["RA", "f1"], writes=["ext"])
                S.op("dve", lambda: nc.vector.tensor_tensor(
                    out=ext_s[:, :, 30:34], in0=psA[:, NPT:T].rearrange("p (b r) -> p b r", b=NB),
                    in1=f1[:, NPT:T].rearrange("p (b r) -> p b r", b=NB), op=ALU.mult),
                    reads=["RA", "f1"], writes=["ext"])
                def srcT(c0, n):
                    if c0 < NPT:
                        return ext_p[:, 30 + c0:30 + c0 + n]
                    return ext_s[:, :, 30:34]
                S.op("dve", lambda: nc.vector.tensor_copy(out=f1[:, 0:64].rearrange("p (b r) -> p b r", b=NB), in_=ext_s[:, :, 30:34]),
                     reads=["ext", "f1"], writes=["f1"])
                tm_rows(lambda c0, n: (ext_p[:, 30 + c0:30 + c0 + n] if c0 < NPT else f1[:, 0:64]), ct, tstage, "f1")
                wbase = rows[("c31w", l)]
                accp = [conv[:, ct, 0:NPT], f1[:, 0:NPT]]
                accs = [conv[:, ct, NPT:T].rearrange("p (b r) -> p b r", b=NB), f1[:, NPT:T].rearrange("p (b r) -> p b r", b=NB)]
                for j in range(31):
                    wc = colT[:, wbase + j * 4 + ct: wbase + j * 4 + ct + 1]
                    a_ = j % 2
                    kp, ks = ("cv", a_, "p"), ("cv", a_, "s")
                    if j == 0:
                        S.op("dve", lambda wc=wc: nc.vector.tensor_scalar(
                            out=accp[0], in0=ext_p[:, 0:NPT], scalar1=wc, scalar2=col(rows, ("c31b", l), ct),
                            op0=ALU.mult, op1=ALU.add), reads=["ext", "colT"], writes=[kp, "conv"])
                        S.op("dve", lambda wc=wc: nc.vector.tensor_scalar(
                            out=accs[0], in0=ext_s[:, :, 0:4], scalar1=wc,
                            scalar2=col(rows, ("c31b", l), ct), op0=ALU.mult, op1=ALU.add),
                            reads=["ext", "colT"], writes=[ks])
                    elif j == 1:
                        S.op("dve", lambda wc=wc: nc.vector.tensor_scalar(
                            out=accp[1], in0=ext_p[:, 1:1 + NPT], scalar1=wc, scalar2=None, op0=ALU.mult),
                            reads=["ext", "colT"], writes=[kp, "f1"])
                        S.op("dve", lambda wc=wc: nc.vector.tensor_scalar(
                            out=accs[1], in0=ext_s[:, :, 1:5], scalar1=wc, scalar2=None, op0=ALU.mult),
                            reads=["ext", "colT"], writes=[ks])
                    else:
                        S.op("dve", lambda wc=wc, j=j, a_=a_: nc.vector.scalar_tensor_tensor(
                            out=accp[a_], in0=ext_p[:, j:j + NPT], scalar=wc, in1=accp[a_],
                            op0=ALU.mult, op1=ALU.add), reads=["ext", "colT", kp], writes=[kp])
                        S.op("dve", lambda wc=wc, j=j, a_=a_: nc.vector.scalar_tensor_tensor(
                            out=accs[a_], in0=ext_s[:, :, j:j + 4], scalar=wc, in1=accs[a_],
                            op0=ALU.mult, op1=ALU.add), reads=["ext", "colT", ks], writes=[ks])
                S.op("dve", lambda: nc.vector.tensor_tensor(out=conv[:, ct, :], in0=conv[:, ct, :], in1=f1, op=ALU.add),
                     reads=[("cv", 0, "p"), ("cv", 0, "s"), ("cv", 1, "p"), ("cv", 1, "s"), "f1", "conv"],
                     writes=["conv", ("cv", 0, "p"), ("cv", 0, "s")])
            S.dma("sp", c31_p[l], tstage[98:128, 0, :], reads=["tstage"], writes=[("o_c31p", l)])
            for s in range(4):
                S.dma("sp", c31_s[l, :, 26 + s, :], tstage[s:64:4, 1, :], reads=["tstage"], writes=[("o_c31s", l, s)])
            for ct in range(4):
                b = b1 if ct % 2 == 0 else b2
                S.op("act", lambda ct=ct, b=b: nc.scalar.copy(out=b, in_=conv[:, ct, :]), reads=["conv"], writes=[("b12", ct % 2)])
                base = ROWBASE["RA"]
                fns = [lambda ct=ct, b=b, t0=t0, tn=tn: nc.tensor.matmul(
                    psA[:, base + t0: base + t0 + tn], lhsT=onesb[:], rhs=b[:, t0:t0 + tn], start=(ct == 0), stop=(ct == 3))
                    for (t0, tn) in TT]
                S.op("pe", fns, reads=[("b12", ct % 2), "onesb"], writes=["RA"])
            S.op("act", lambda: nc.scalar.mul(out=f1, in_=prow("RA"), mul=1.0 / 512), reads=["RA"], writes=["f1"])
            for ct in range(4):
                S.op("dve", lambda ct=ct: nc.vector.tensor_tensor(out=conv[:, ct, :], in0=conv[:, ct, :], in1=f1, op=ALU.subtract),
                     reads=["conv", "f1"], writes=["conv"])
            for ct in range(4):
                b = b1 if ct % 2 == 0 else b2
                S.op("act", lambda ct=ct, b=b: nc.scalar.activation(out=b, in_=conv[:, ct, :], func=AF.Square),
                     reads=["conv"], writes=[("b12", ct % 2)])
                base = ROWBASE["RB"]
                fns = [lambda ct=ct, b=b, t0=t0, tn=tn: nc.tensor.matmul(
                    psA[:, base + t0: base + t0 + tn], lhsT=onesb[:], rhs=b[:, t0:t0 + tn], start=(ct == 0), stop=(ct == 3))
                    for (t0, tn) in TT]
                S.op("pe", fns, reads=[("b12", ct % 2), "onesb"], writes=["RB"])
            rsqrt_row(f1, prow("RB"), 1.0 / 512, "f1", "RB")
            for ct in range(4):
                S.op("dve", lambda ct=ct: nc.vector.scalar_tensor_tensor(
                    out=conv[:, ct, :], in0=conv[:, ct, :], scalar=col(rows, ("clng", l), ct), in1=f1,
                    op0=ALU.mult, op1=ALU.mult), reads=["conv", "f1", "colT"], writes=["conv"])
                S.op("act", lambda ct=ct: nc.scalar.activation(out=mixT[:, ct, :], in_=conv[:, ct, :], func=AF.Silu,
                                                              bias=col(rows, ("clnb", l), ct), scale=1.0),
                     reads=["conv", "colT"], writes=["mixT"])
            sl = wout_slots(l, 1)
            accum_out(sl, [mixT[:, k, :] for k in range(4)], "mixT")

        def mixer_C(l, rows):
            EXT = 2 + NPT + NB * 6
            o_ext = 0
            o_f1 = 1124
            o_cv = o_f1 + T
            o_hst = o_cv + T
            ext = fa(o_ext, EXT)
            f1 = fa(o_f1, T)
            cv = fa(o_cv, T)
            hst = fa(o_hst, 512)
            tstage = fa(o_hst + 512, 1024).rearrange("p (a b) -> p a b", a=2)
            S.barrier()
            ext_p = ext[:, 0:2 + NPT]
            ext_s = ext[:, 2 + NPT:EXT].rearrange("p (b r) -> p b r", b=NB)
            S.dma("sp", hst[0:32, :], st_c3[l].rearrange("b r c -> (b r) c"), writes=["hst"])
            for ct in range(4):
                s_c = W.get([(16, 256, 0, 128, win_cols(l, 3584 + ct * 128, 128)),
                             (16, 256, 128, 128, win_cols(l, 4096 + ct * 128, 128))])
                proj_fm(s_c, 0, "RA")
                proj_fm(s_c, 128, "RB")
                S.op("act", lambda: nc.scalar.copy(out=f1, in_=prow("RA")), reads=["RA"], writes=["f1"])
                S.op("dve", lambda: nc.vector.tensor_copy(out=ext[:, 0:2], in_=hx3[:, ct, :]), reads=["hx3"], writes=["ext"])
                S.op("pe", lambda: nc.tensor.transpose(psS[:, 256:288], hst[0:32, ct * 128:(ct + 1) * 128], identf[0:32, 0:32]),
                     reads=["hst", "const"], writes=["psS"])
                S.op("act", lambda: nc.scalar.copy(out=ext_s[:, :, 0:2], in_=psS[:, 256:288].rearrange("p (b r) -> p b r", b=NB)),
                     reads=["psS"], writes=["ext"])
                S.op("dve", lambda: nc.vector.tensor_tensor(out=ext_p[:, 2:2 + NPT], in0=prow("RB", NPT), in1=f1[:, 0:NPT], op=ALU.mult),
                     reads=["RB", "f1"], writes=["ext"])
                S.op("dve", lambda: nc.vector.tensor_tensor(
                    out=ext_s[:, :, 2:6], in0=psA[:, 1536 + NPT:1536 + T].rearrange("p (b r) -> p b r", b=NB),
                    in1=f1[:, NPT:T].rearrange("p (b r) -> p b r", b=NB), op=ALU.mult), reads=["RB", "f1"], writes=["ext"])
                S.op("dve", lambda: nc.vector.tensor_copy(out=f1[:, 0:64].rearrange("p (b r) -> p b r", b=NB), in_=ext_s[:, :, 2:6]),
                     reads=["ext", "f1"], writes=["f1"])
                tm_rows(lambda c0, n: (ext_p[:, 2 + c0:2 + c0 + n] if c0 < NPT else f1[:, 0:64]), ct, tstage, "f1")
                wbase = rows[("scw", l)]
                for j in range(3):
                    wc = colT[:, wbase + j * 4 + ct: wbase + j * 4 + ct + 1]
                    if j == 0:
                        S.op("dve", lambda wc=wc: nc.vector.tensor_scalar(out=cv[:, 0:NPT], in0=ext_p[:, 0:NPT], scalar1=wc, scalar2=None,
                                                                          op0=ALU.mult), reads=["ext", "colT"], writes=["cv"])
                        S.op("dve", lambda wc=wc: nc.vector.tensor_scalar(
                            out=cv[:, NPT:T].rearrange("p (b r) -> p b r", b=NB), in0=ext_s[:, :, 0:4], scalar1=wc, scalar2=None,
                            op0=ALU.mult), reads=["ext", "colT"], writes=["cv"])
                    else:
                        S.op("dve", lambda wc=wc, j=j: nc.vector.scalar_tensor_tensor(
                            out=cv[:, 0:NPT], in0=ext_p[:, j:j + NPT], scalar=wc, in1=cv[:, 0:NPT], op0=ALU.mult, op1=ALU.add),
                            reads=["ext", "colT", "cv"], writes=["cv"])
                        S.op("dve", lambda wc=wc, j=j: nc.vector.scalar_tensor_tensor(
                            out=cv[:, NPT:T].rearrange("p (b r) -> p b r", b=NB), in0=ext_s[:, :, j:j + 4], scalar=wc,
                            in1=cv[:, NPT:T].rearrange("p (b r) -> p b r", b=NB), op0=ALU.mult, op1=ALU.add),
                            reads=["ext", "colT", "cv"], writes=["cv"])
                if ct % 2 == 0:
                    s_cb = W.get([(16, 256, 0, 256, win_cols(l, 3072 + ct * 128, 256))])
                proj_fm(s_cb, (ct % 2) * 128, "RA")
                S.op("dve", lambda ct=ct: nc.vector.tensor_tensor(out=mixT[:, ct, :], in0=prow("RA"), in1=cv, op=ALU.mult),
                     reads=["RA", "cv"], writes=["mixT"])
            S.dma("sp", c3_p[l], tstage[126:128, 0, :], reads=["tstage"], writes=[("o_c3p", l)])
            for s in range(2):
                S.dma("sp", c3_s[l, :, s, :], tstage[2 + s:64:4, 1, :], reads=["tstage"], writes=[("o_c3s", l, s)])
            sl = wout_slots(l, 2)
            accum_out(sl, [mixT[:, k, :] for k in range(4)], "mixT")

        def mixer_D(l, rows):
            o_lng = 0
            o_lnb = 512
            o_sgb = 1024
            o_vn = 1536
            o_sq = 2048
            o_vb = 2560
            o_wm = o_vb + 2304
            o_wms = o_wm + 256
            o_wst = o_wms + 128
            o_sm = o_wst + 512
            o_u = o_sm + 8
            o_mb = o_u + 64
            lng = fa(o_lng, 512)
            lnb = fa(o_lnb, 512)
            sgb = fa(o_sgb, 512)
            vn = fa(o_vn, 512)
            sqs = fa(o_sq, 512)
            vb = ba(o_vb, 9 * 512).rearrange("p (a b) -> p a b", a=9)
            wm = ba(o_wm, 512).rearrange("p (a b) -> p a b", a=4)
            wms = ba(o_wms, 256).rearrange("p (a b) -> p a b", a=4)
            wst4 = [fa(o_wst + k * 128, 128) for k in range(4)]
            sm = fa(o_sm, 8)
            U = fa(o_u, 64)
            mb = fa(o_mb, T)
            assert o_mb + T <= ARENA
            S.barrier()
            S.dma("sp", lng, sgu_ln_g[l].partition_broadcast(128), writes=["lng"])
            S.dma("sp", lnb, sgu_ln_b[l].partition_broadcast(128), writes=["lnb"])
            S.dma("sp", sgb, sgu_b[l].partition_broadcast(128), writes=["sgb"])
            for g in range(4):
                S.dma("sp", wst4[g], sgu_w[l, g], writes=[("wst", g)])
            for g in range(4):
                wst = wst4[g]
                S.op("pe", lambda wst=wst: nc.tensor.transpose(psS[:, 0:128], wst, identf[:]), reads=[("wst", g), "const"], writes=["psS"])
                S.op("dve", lambda g=g: nc.vector.tensor_tensor(out=wm[:, g, :], in0=psS[:, 0:128], in1=triu[:], op=ALU.mult),
                     reads=["psS", "const"], writes=["wm"])
                S.op("pe", lambda wst=wst: nc.tensor.matmul(psS[0:4, 128:192], lhsT=wst[0:4, 0:4], rhs=Rm[:], start=True, stop=True),
                     reads=[("wst", g), "const"], writes=["psS"])
                S.op("act", lambda: nc.scalar.copy(out=U[0:4, :], in_=psS[0:4, 128:192]), reads=["psS"], writes=["U"])
                S.op("pe", lambda: nc.tensor.matmul(psS[0:64, 256:320], lhsT=Rm[:], rhs=U[0:4, :], start=True, stop=True),
                     reads=["U", "const"], writes=["psS"])
                S.op("dve", lambda g=g: nc.vector.tensor_tensor(out=wms[0:64, g, :], in0=psS[0:64, 256:320], in1=bm[:], op=ALU.mult),
                     reads=["psS", "const"], writes=["wms"])
            dbg_stop("D_w")
            s_v0 = W.get([(16, 256, 0, 256, win_cols(l, 5120, 256))])
            s_v1 = W.get([(16, 256, 0, 256, win_cols(l, 5376, 256))])
            for i in range(9):
                n = 128 if i < 8 else 64
                row = "RA" if i % 2 == 0 else "RB"
                base = ROWBASE[row]
                fns = []
                for half, sl_ in enumerate((s_v0, s_v1)):
                    wv = wview(sl_, 0, 16, 256)
                    for kc in range(16):
                        fns.append(lambda half=half, wv=wv, kc=kc, i=i, n=n, base=base: nc.tensor.matmul(
                            psA[0:n, base + half * 256: base + half * 256 + 256], lhsT=hT[:, kc, i * 128:i * 128 + n],
                            rhs=wv[:, kc, :], start=(kc == 0), stop=(kc == 15)))
                S.op("pe", fns, reads=[("w", s_v0), ("w", s_v1), "hT"], writes=[row])
                dv = psA[0:n, base:base + 512]
                dbg_stop("D_p0")
                S.op("dve", lambda dv=dv, n=n: nc.vector.tensor_reduce(out=sm[0:n, 0:1], in_=dv, axis=AX.X, op=ALU.add),
                     reads=[row], writes=["sm0"])
                dbg_stop("D_r0")
                S.op("act", lambda dv=dv, n=n: nc.scalar.copy(out=sqs[0:n, :], in_=dv), reads=[row, "sm0"], writes=["sqs"])
                S.op("dve", lambda n=n: nc.vector.tensor_tensor(out=sqs[0:n, :], in0=sqs[0:n, :], in1=sqs[0:n, :], op=ALU.mult), reads=["sqs"], writes=["sqs"])
                dbg_stop("D_qa")
                S.op("dve", lambda n=n: nc.vector.tensor_reduce(out=sm[0:n, 1:2], in_=sqs[0:n, :], axis=AX.X, op=ALU.add),
                     reads=["sqs"], writes=["sm1"])
                dbg_stop("D_q0")
                S.op("dve", lambda n=n: nc.vector.tensor_scalar(out=sm[0:n, 2:3], in0=sm[0:n, 0:1], scalar1=1.0 / 512, scalar2=None, op0=ALU.mult),
                     reads=["sm0"], writes=["sm2"])
                S.op("dve", lambda n=n: nc.vector.tensor_tensor(out=sm[0:n, 3:4], in0=sm[0:n, 2:3], in1=sm[0:n, 2:3], op=ALU.mult),
                     reads=["sm2"], writes=["sm3"])
                S.op("dve", lambda n=n: nc.vector.scalar_tensor_tensor(out=sm[0:n, 4:5], in0=sm[0:n, 1:2], scalar=1.0 / 512, in1=sm[0:n, 3:4],
                                                                       op0=ALU.mult, op1=ALU.subtract), reads=["sm1", "sm3"], writes=["sm4"])
                dbg_stop("D_t0")
                S.op("act", lambda n=n: nc.scalar.activation(out=sm[0:n, 5:6], in_=sm[0:n, 4:5], func=AF.Sqrt, bias=epsc[0:n, :], scale=1.0),
                     reads=["sm4", "epsc"], writes=["sm5"])
                S.op("dve", lambda n=n: nc.vector.reciprocal(out=sm[0:n, 6:7], in_=sm[0:n, 5:6]), reads=["sm5"], writes=["sm6"])
                dbg_stop("D_s0")
                S.op("dve", lambda dv=dv, n=n: nc.vector.tensor_scalar(out=vn[0:n, :], in0=dv, scalar1=sm[0:n, 2:3], scalar2=sm[0:n, 6:7],
                                                                       op0=ALU.subtract, op1=ALU.mult),
                     reads=[row, "sm2", "sm6"], writes=["vn"])
                S.op("dve", lambda n=n: nc.vector.tensor_tensor(out=vn[0:n, :], in0=vn[0:n, :], in1=lng[0:n, :], op=ALU.mult),
                     reads=["vn", "lng"], writes=["vn"])
                S.op("dve", lambda n=n: nc.vector.tensor_tensor(out=vn[0:n, :], in0=vn[0:n, :], in1=lnb[0:n, :], op=ALU.add),
                     reads=["vn", "lnb"], writes=["vn"])
                S.op("act", lambda n=n, i=i: nc.scalar.copy(out=vb[0:n, i, :], in_=vn[0:n, :]), reads=["vn"], writes=["vb"])
                dbg_stop("D_v0")
                if i == 8:
                    S.dma("sp", vs_out[l], vn[0:64, :], reads=["vn"], writes=[("o_vs", l)])
            dbg_stop("D_ln")
            for g in range(4):
                if g % 2 == 0:
                    s_u = W.get([(16, 256, 0, 256, win_cols(l, 4608 + g * 128, 256))])
                base = ROWBASE["RA"]
                fns = [lambda c=c, g=g: nc.tensor.matmul(psA[:, base + c * 128: base + (c + 1) * 128],
                                                         lhsT=vb[:, c, g * 128:(g + 1) * 128], rhs=wm[:, g, :], start=True, stop=True)
                       for c in range(8)]
                fns.append(lambda g=g: nc.tensor.matmul(psA[:, base + NPT: base + T], lhsT=vb[0:64, 8, g * 128:(g + 1) * 128],
                                                        rhs=wms[0:64, g, :], start=True, stop=True))
                S.op("pe", fns, reads=["vb", "wm", "wms"], writes=["RA"])
                S.op("dve", lambda g=g: nc.vector.tensor_tensor(
                    out=mb[:, 0:NPT].rearrange("p (c t) -> p c t", c=8), in0=psA[:, base:base + NPT].rearrange("p (c t) -> p c t", c=8),
                    in1=sgb[:, g * 128:(g + 1) * 128].unsqueeze(1).to_broadcast([128, 8, 128]), op=ALU.add),
                    reads=["RA", "sgb"], writes=["mb"])
                S.op("dve", lambda g=g: nc.vector.tensor_tensor(
                    out=mb[:, NPT:T].rearrange("p (b r) -> p b r", b=NB), in0=psA[:, base + NPT:base + T].rearrange("p (b r) -> p b r", b=NB),
                    in1=sgb[:, g * 128:g * 128 + 4].unsqueeze(1).to_broadcast([128, NB, 4]), op=ALU.add),
                    reads=["RA", "sgb"], writes=["mb"])
                proj_fm(s_u, (g % 2) * 128, "RB")
                S.op("dve", lambda g=g: nc.vector.tensor_tensor(out=mixT[:, g, :], in0=prow("RB"), in1=mb, op=ALU.mult),
                     reads=["RB", "mb"], writes=["mixT"])
            dbg_stop("D_mix")
            sl = wout_slots(l, 3)
            accum_out(sl, [mixT[:, k, :] for k in range(4)], "mixT")

        def ffn_pass(w1, w3, w2, gm=None):
            o_act = 0
            o_s = 2 * T
            act = ba(o_act, 4 * T).rearrange("p (a b) -> p a b", a=4)
            sbuf = [fa(o_s, T), fa(o_s + T, T)]
            for grp in range(DFF // 512):
                f0 = grp * 512
                for half in range(2):
                    c0 = f0 + half * 256
                    s1 = W.get([(16, 256, 0, 256, w1[:, c0:c0 + 256].rearrange("(kc p) n -> p kc n", p=128))])
                    s3 = W.get([(16, 256, 0, 256, w3[:, c0:c0 + 256].rearrange("(kc p) n -> p kc n", p=128))])
                    for t in range(2):
                        ti = half * 2 + t
                        sb_ = sbuf[ti % 2]
                        skey = ("fs", ti % 2)
                        proj_fm(s1, t * 128, "RA")
                        S.op("act", lambda sb_=sb_: nc.scalar.activation(out=sb_, in_=prow("RA"), func=AF.Silu),
                             reads=["RA"], writes=[skey])
                        proj_fm(s3, t * 128, "RB")
                        if gm is not None:
                            S.op("dve", lambda sb_=sb_: nc.vector.tensor_tensor(out=sb_, in0=sb_, in1=gm, op=ALU.mult),
                                 reads=[skey, "gm"], writes=[skey])
                        S.op("dve", lambda sb_=sb_, ti=ti: nc.vector.tensor_tensor(out=act[:, ti, :], in0=prow("RB"), in1=sb_, op=ALU.mult),
                             reads=["RB", skey], writes=[("actT", ti)])
                sl = []
                for half in range(2):
                    r0 = f0 + half * 256
                    sl.append(W.get([(2, 2048, 0, 2048, w2[r0:r0 + 256, :].rearrange("(kc p) n -> p kc n", p=128))]))
                accum_out(sl, [act[:, k, :] for k in range(4)], [("actT", k) for k in range(4)])

        def moe(rows):
            o_gm = 4 * T
            o_rw = o_gm + T
            o_lg = o_rw + 128
            gm = fa(o_gm, T)
            rw = fa(o_rw, 128).rearrange("p (a b) -> p a b", a=16)
            lg = fa(o_lg, 72).rearrange("p (a b) -> p a b", a=9)
            m1 = fa(o_lg + 72, 9)
            m2 = fa(o_lg + 84, 9)
            oh1 = fa(o_lg + 96, 72).rearrange("p (a b) -> p a b", a=9)
            oh2 = fa(o_lg + 168, 72).rearrange("p (a b) -> p a b", a=9)
            tmp = fa(o_lg + 240, 72).rearrange("p (a b) -> p a b", a=9)
            g1 = fa(o_lg + 312, 9)
            g2 = fa(o_lg + 324, 9)
            G = fa(o_lg + 336, 72).rearrange("p (a b) -> p a b", a=9)
            GT = fa(o_lg + 408, T)
            assert o_lg + 408 + T <= ARENA
            S.barrier()
            S.op("dve", lambda: nc.vector.memset(lg, -1e30), writes=["lg"])
            rw = rw_t
            for i in range(9):
                n = 128 if i < 8 else 64
                fns = [lambda c=c, i=i, n=n: nc.tensor.matmul(psS[0:n, i * 8:(i + 1) * 8], lhsT=xT[:, c, i * 128:i * 128 + n],
                                                              rhs=rw[:, c, :], start=(c == 0), stop=(c == 15)) for c in range(16)]
                S.op("pe", fns, reads=["xT", "rw"], writes=["psS"])
            rstd_row = fa(T, T)
            onesf = fa(o_lg + 348, 1)
            S.op("dve", lambda: nc.vector.memset(onesf, 1.0), writes=["onesf"])
            fns = [lambda i=i: nc.tensor.matmul(psS[0:(128 if i < 8 else 64), 128 + i:129 + i],
                                                lhsT=rstd_row[0:1, i * 128:i * 128 + (128 if i < 8 else 64)], rhs=onesf[0:1, 0:1],
                                                start=True, stop=True) for i in range(9)]
            S.op("pe", fns, reads=["rstd", "onesf"], writes=["psS"])
            S.op("act", lambda: nc.scalar.copy(out=g2, in_=psS[:, 128:137]), reads=["psS"], writes=["g2"])
            for i in range(9):
                n = 128 if i < 8 else 64
                S.op("dve", lambda i=i, n=n: nc.vector.tensor_scalar(out=lg[0:n, i, :], in0=psS[0:n, i * 8:(i + 1) * 8], scalar1=g2[0:n, i:i + 1],
                                                                     scalar2=None, op0=ALU.mult), reads=["psS", "g2", "lg"], writes=["lg"])
            S.op("dve", lambda: nc.vector.tensor_reduce(out=m1, in_=lg, axis=AX.X, op=ALU.max), reads=["lg"], writes=["m1"])
            S.op("dve", lambda: nc.vector.tensor_tensor(out=oh1, in0=lg, in1=m1.unsqueeze(2).to_broadcast([128, 9, 8]), op=ALU.is_equal),
                 reads=["lg", "m1"], writes=["oh1"])
            S.op("dve", lambda: nc.vector.scalar_tensor_tensor(out=tmp, in0=oh1, scalar=-1e30, in1=lg, op0=ALU.mult, op1=ALU.add),
                 reads=["oh1", "lg"], writes=["tmp"])
            S.op("dve", lambda: nc.vector.tensor_reduce(out=m2, in_=tmp, axis=AX.X, op=ALU.max), reads=["tmp"], writes=["m2"])
            S.op("dve", lambda: nc.vector.tensor_tensor(out=oh2, in0=tmp, in1=m2.unsqueeze(2).to_broadcast([128, 9, 8]), op=ALU.is_equal),
                 reads=["tmp", "m2"], writes=["oh2"])
            S.op("dve", lambda: nc.vector.tensor_tensor(out=g1, in0=m1, in1=m2, op=ALU.subtract), reads=["m1", "m2"], writes=["g1"])
            S.op("act", lambda: nc.scalar.activation(out=g1, in_=g1, func=AF.Sigmoid), reads=["g1"], writes=["g1"])
            S.op("dve", lambda: nc.vector.tensor_scalar(out=g2, in0=g1, scalar1=-1.0, scalar2=1.0, op0=ALU.mult, op1=ALU.add),
                 reads=["g1", "g2"], writes=["g2"])
            S.op("dve", lambda: nc.vector.tensor_tensor(out=G, in0=oh1, in1=g1.unsqueeze(2).to_broadcast([128, 9, 8]), op=ALU.mult),
                 reads=["oh1", "g1"], writes=["G"])
            S.op("dve", lambda: nc.vector.tensor_tensor(out=tmp, in0=oh2, in1=g2.unsqueeze(2).to_broadcast([128, 9, 8]), op=ALU.mult),
                 reads=["oh2", "g2", "tmp"], writes=["tmp"])
            S.op("dve", lambda: nc.vector.tensor_tensor(out=G, in0=G, in1=tmp, op=ALU.add), reads=["G", "tmp"], writes=["G"])
            for i in range(9):
                n = 128 if i < 8 else 64
                S.op("pe", lambda i=i, n=n: nc.tensor.transpose(psA[0:8, i * 128:i * 128 + n], G[0:n, i, :], identf[0:n, 0:n]),
                     reads=["G", "const"], writes=["RA"])
            S.op("act", lambda: nc.scalar.copy(out=GT[0:8, :], in_=psA[0:8, 0:T]), reads=["RA"], writes=["GT"])
            for e in range(NEXP):
                base = ROWBASE["RB"]
                fns = [lambda e=e, t0=t0, tn=tn: nc.tensor.matmul(psA[:, base + t0: base + t0 + tn], lhsT=esel[:, e * 128:(e + 1) * 128],
                                                                  rhs=GT[0:8, t0:t0 + tn], start=True, stop=True) for (t0, tn) in TT]
                S.op("pe", fns, reads=["GT", "const"], writes=["RB"])
                S.op("act", lambda: nc.scalar.copy(out=gm, in_=prow("RB")), reads=["RB"], writes=["gm"])
                ffn_pass(moe_w1[e], moe_w3[e], moe_w2[e], gm=gm)

        def final_out(rows, norm=True):
            S.barrier()
            rstd = fa(0, T)
            sq = [ba(T, T), ba(T + T // 2, T)]
            for c in range(16 if norm else 0):
                b = sq[c % 2]
                S.op("act", lambda c=c, b=b: nc.scalar.activation(out=b, in_=xT[:, c, :], func=AF.Square),
                     reads=["xT"], writes=[("sq", c % 2)])
                base = ROWBASE["RA"]
                fns = [lambda c=c, b=b, t0=t0, tn=tn: nc.tensor.matmul(
                    psA[:, base + t0: base + t0 + tn], lhsT=onesb[:], rhs=b[:, t0:t0 + tn], start=(c == 0), stop=(c == 15))
                    for (t0, tn) in TT]
                S.op("pe", fns, reads=[("sq", c % 2), "onesb"], writes=["RA"])
            if norm:
                rsqrt_row(rstd, prow("RA"), 1.0 / D, "rstd", "RA")
            for c in range(16 if norm else 0):
                S.op("dve", lambda c=c: nc.vector.scalar_tensor_tensor(
                    out=xT[:, c, :], in0=xT[:, c, :], scalar=col(rows, ("fing",), c), in1=rstd, op0=ALU.mult, op1=ALU.mult),
                    reads=["xT", "rstd", "colT"], writes=["xT"])
            ost = [fa(2 * T, 2048), fa(2 * T + 2048, 2048)]
            for i in range(9):
                n = 128 if i < 8 else 64
                o = ost[i % 2]
                for g in range(4):
                    bank = ROWBASE["RA"] + (g % 2) * 512 if g < 2 else ROWBASE["RB"] + (g % 2) * 512
                    key = "pb%d" % (bank // 512)
                    fns = [lambda j=j, g=g, bank=bank: nc.tensor.transpose(
                        psA[0:n, bank + j * 128: bank + (j + 1) * 128], xT[:, g * 4 + j, i * 128:i * 128 + n], identf[:])
                        for j in range(4)]
                    S.op("pe", fns, reads=["xT", "const"], writes=[key])
                    if g % 2 == 0:
                        S.op("dve", lambda g=g, bank=bank, o=o: nc.vector.tensor_copy(out=o[0:n, g * 512:(g + 1) * 512], in_=psA[0:n, bank:bank + 512]),
                             reads=[key], writes=[("ost", i % 2)])
                    else:
                        S.op("act", lambda g=g, bank=bank, o=o: nc.scalar.copy(out=o[0:n, g * 512:(g + 1) * 512], in_=psA[0:n, bank:bank + 512]),
                             reads=[key], writes=[("ost", i % 2)])
                S.dma("sp", y_out[i * 128:i * 128 + n, :], o[0:n, :], reads=[("ost", i % 2)], writes=[("o_y", i)])

        def emit_all():
            st = [0]

            def cut():
                st[0] += 1
                if STAGE == st[0]:
                    final_out(rows, norm=False)
                    S.finish()
                    return True
                return False

            rows = load_consts()
            load_x()
            if cut():
                return
            for l in range(2):
                rmsnorm_to_hT(("mixg", l), rows)
                dbg_stop("norm0")
                if XCH:
                    pre_pass(l)
                mixer_D(l, rows)
                load_xch(l)
                if cut():
                    return
                mixer_A(l, rows)
                if cut():
                    return
                mixer_B(l, rows)
                if cut():
                    return
                mixer_C(l, rows)
                if cut():
                    return
                S.barrier()
                rmsnorm_to_hT(("ffng", l), rows)
                if l == 0:
                    ffn_pass(dense_w1[0], dense_w3[0], dense_w2[0])
                else:
                    moe(rows)
                if cut():
                    return
            final_out(rows)
            S.finish()

        S.plan = True
        try:
            emit_all()
        except StopEmit:
            pass
        S.plan = False
        S.reset()
        W.reset()
        try:
            emit_all()
        except StopEmit:
            S.finish()
    return nc


def _consts(half):
    c = {}
    c["c_ident"] = np.eye(128, dtype=np.float32)
    pos = np.concatenate([np.arange(NPT, dtype=np.float32) + np.float32(half * NPT),
                          np.tile(np.arange(4, dtype=np.float32) + np.float32(16384.0), NB)]).astype(np.float32)
    inv = (np.float32(10000.0) ** (-np.arange(64, dtype=np.float32) / np.float32(64))).astype(np.float32)
    ang = (pos[None, :] * inv[:, None]).astype(np.float32)
    cs = np.cos(ang).astype(np.float32)
    sn = np.sin(ang).astype(np.float32)
    c["c_rope_c"] = np.concatenate([cs, cs], 0)
    c["c_rope_s"] = np.concatenate([-sn, sn], 0)
    scale = 128.0 ** -0.5
    logg = [math.log1p(-2.0 ** (-5.0 - h)) for h in range(4)]
    j = np.arange(128)[:, None]
    i = np.arange(128)[None, :]
    dm = np.zeros((128, 4, 128), np.float64)
    dms = np.zeros((64, 4, 64), np.float64)
    dqv = np.zeros((128, 4, 128), np.float64)
    dqs = np.zeros((128, 4, 64), np.float64)
    dkv = np.zeros((128, 8), np.float64)
    js = np.arange(64)[:, None]
    is_ = np.arange(64)[None, :]
    for h in range(4):
        dm[:, h, :] = np.where(i >= j, np.exp(logg[h] * np.maximum(i - j, 0)), 0.0) * scale
        same = (js // 4) == (is_ // 4)
        dms[:, h, :] = np.where(same & (is_ >= js), np.exp(logg[h] * np.maximum(is_ - js, 0)), 0.0) * scale
        dqv[:, h, :] = np.exp(logg[h] * (np.arange(128) + 1.0))[None, :]
        dqs[:, h, :] = np.exp(logg[h] * ((np.arange(64) % 4) + 1.0))[None, :]
        dkv[:, h] = np.exp(logg[h] * (127.0 - np.arange(128))) * scale
        dkv[:64, 4 + h] = np.exp(logg[h] * (3.0 - (np.arange(64) % 4))) * scale
    c["c_dmask"] = dm.reshape(128, 512).astype(np.float32)
    c["c_dmask_s"] = dms.reshape(64, 256).astype(np.float32)
    c["c_dq"] = dqv.reshape(128, 512).astype(np.float32)
    c["c_dq_s"] = dqs.reshape(128, 256).astype(np.float32)
    c["c_dkv"] = dkv.astype(np.float32)
    c["c_ind"] = ((np.arange(64)[:, None] // 4) == np.arange(NB)[None, :]).astype(np.float32)
    c["c_triu"] = (j <= i).astype(np.float32)
    c["c_bm"] = (((js // 4) == (is_ // 4)) & (js <= is_)).astype(np.float32)
    c["c_R"] = ((np.arange(64)[None, :] % 4) == np.arange(4)[:, None]).astype(np.float32)
    es = np.zeros((8, 8, 128), np.float32)
    for e in range(8):
        es[e, e, :] = 1.0
    c["c_esel"] = es.reshape(8, 1024)
    c["c_s0mask"] = np.full((128, 1), float(half), np.float32)
    return c


_NC_CACHE = {}


def kernel(**inputs):
    f = lambda k: np.ascontiguousarray(np.asarray(inputs[k], dtype=np.float32))
    x_prompt = f("x_prompt")
    x_sample = f("x_sample")
    shared = {
        "mix_norm_g": f("mix_norm_g"), "w_in": f("w_in"), "ret_norm_g": f("ret_norm_g"), "conv31_w": f("conv31_w"),
        "conv31_b": f("conv31_b"), "conv_ln_g": f("conv_ln_g"), "conv_ln_b": f("conv_ln_b"), "sconv_w": f("sconv_w"),
        "sgu_ln_g": f("sgu_ln_g"), "sgu_ln_b": f("sgu_ln_b"), "sgu_w": f("sgu_w"),
        "sgu_b": f("sgu_b").reshape(2, 512), "w_out": f("w_out"), "ffn_norm_g": f("ffn_norm_g"),
        "dense_w1": f("dense_w1"), "dense_w3": f("dense_w3"), "dense_w2": f("dense_w2"),
        "router_w": f("router_w")[0], "moe_w1": f("moe_w1")[0], "moe_w3": f("moe_w3")[0], "moe_w2": f("moe_w2")[0],
        "final_norm_g": f("final_norm_g"),
    }
    st_ret = f("state_ret")
    st_c31 = f("state_conv31")
    st_c3 = f("state_conv3")
    in_maps = []
    for c in range(NCORES):
        b, half = c // 2, c % 2
        m = dict(shared)
        m["xin"] = np.ascontiguousarray(np.concatenate(
            [x_prompt[b, half * NPT:(half + 1) * NPT], x_sample[c * NB:(c + 1) * NB].reshape(NST, D)], 0))
        m["st_ret"] = np.ascontiguousarray(st_ret[:, c * NB:(c + 1) * NB])
        m["st_c31"] = np.ascontiguousarray(st_c31[:, c * NB:(c + 1) * NB])
        m["st_c3"] = np.ascontiguousarray(st_c3[:, c * NB:(c + 1) * NB])
        m.update(_consts(half))
        in_maps.append(m)
    if "nc" not in _NC_CACHE:
        _NC_CACHE["nc"] = build_program()
    res = run_bass_kernel_spmd(_NC_CACHE["nc"], in_maps, core_ids=list(range(NCORES)))
    R = res.results
    y_prompt = np.zeros((4, 2048, D), np.float32)
    y_sample = np.zeros((128, 4, D), np.float32)
    ret_p = np.zeros((2, 4, 4, 128, 128), np.float32)
    ret_s = np.zeros((2, 128, 4, 128, 128), np.float32)
    c31_p = np.zeros((2, 4, 30, 512), np.float32)
    c31_s = np.zeros((2, 128, 30, 512), np.float32)
    c3_p = np.zeros((2, 4, 2, 512), np.float32)
    c3_s = np.zeros((2, 128, 2, 512), np.float32)
    vs = np.zeros((2, 128, 4, 512), np.float32)
    for c in range(NCORES):
        b, half = c // 2, c % 2
        r = R[c]
        y_prompt[b, half * NPT:(half + 1) * NPT] = r["y_out"][:NPT]
        y_sample[c * NB:(c + 1) * NB] = r["y_out"][NPT:].reshape(NB, 4, D)
        ret_s[:, c * NB:(c + 1) * NB] = r["ret_s"]
        c31_s[:, c * NB:(c + 1) * NB] = r["c31_s"]
        c3_s[:, c * NB:(c + 1) * NB] = r["c3_s"]
        vs[:, c * NB:(c + 1) * NB] = r["vs_out"].reshape(2, NB, 4, 512)
        if half == 1:
            ret_p[:, b] = r["ret_p"]
            c31_p[:, b] = r["c31_p"]
            c3_p[:, b] = r["c3_p"]
    return (y_prompt, y_sample, ret_p, ret_s, c31_p, c31_s, c3_p, c3_s, vs)
```
